# Optimizing a Trainium2 kernel written in Bass

```python
import jax, jax.numpy as jnp
from jax import lax
import numpy as np

D_MODEL = 2048
BATCH = 1
SEQ = 8192
DEPTH = 2

N_META = 16
EPS = 1e-6
D_FF = -(-(8 * D_MODEL) // (3 * 256)) * 256
N_A_LAYERS = (DEPTH + 1) // 2
N_C_LAYERS = DEPTH // 2
D_A = D_MODEL // 2
HGRN_HEAD = 128
HGRN_HEADS = D_A // HGRN_HEAD
CHUNK = 64
D_B = D_MODEL - D_A
SCONV_W = 3
D_C = D_MODEL // 2
RG_BLOCKS = 8
RG_BLOCK = D_C // RG_BLOCKS
RG_CONV_W = 4
RG_C = 8.0
ATT_HEADS = 8
ATT_HEAD_DIM = 128
D_D = ATT_HEADS * ATT_HEAD_DIM
KV_RANK = 256
IDX_HEADS = 16
IDX_DIM = 64
TOPK_MAX = 256
Q_BLOCK = 128

AB_SIZES = [D_A, D_A, D_A, D_A, D_B, D_B, D_B]
CD_SIZES = [D_C, D_C, D_D, KV_RANK, IDX_HEADS * IDX_DIM, IDX_DIM, IDX_HEADS]

kernel_name = 'hybrid_hgrn2_sconv_rglru_dsa_block'


def rms_norm(x, g):
    xf = x.astype(jnp.float32)
    y = xf * lax.rsqrt(jnp.mean(xf * xf, axis=-1, keepdims=True) + EPS)
    return (y * g.astype(jnp.float32)).astype(x.dtype)


def split_cols(a, sizes):
    idx = [int(v) for v in np.cumsum(sizes)[:-1]]
    return jnp.split(a, idx, axis=-1)


def causal_depthwise_conv(u, w):
    K = w.shape[0]
    return lax.conv_general_dilated(
        u, w[:, None, :].astype(u.dtype), window_strides=(1,), padding=[(K - 1, 0)],
        dimension_numbers=('NWC', 'WIO', 'NWC'), feature_group_count=u.shape[-1])


def hgrn2_chunked(q, f_logit, v, lb):
    B, T, H, dk = q.shape
    dv = v.shape[-1]
    f32 = jnp.float32
    f = lb + (1.0 - lb) * jax.nn.sigmoid(f_logit.astype(f32))
    log_f = jnp.log(f)
    k = 1.0 - f
    pad = (-N_META) % CHUNK
    padf = lambda a: jnp.pad(a.astype(f32), ((0, 0), (pad, 0), (0, 0), (0, 0)))
    q, k, v, log_f = padf(q), padf(k), padf(v), padf(log_f)
    n = (T + pad) // CHUNK
    chunks = lambda a: a.reshape(B, n, CHUNK, H, a.shape[-1]).transpose(1, 0, 3, 2, 4)
    tri = jnp.tril(jnp.ones((CHUNK, CHUNK), bool))[:, :, None]

    def step(S, xs):
        qc, kc, vc, lfc = xs
        b = jnp.cumsum(lfc, axis=2)
        inter = jnp.einsum('bhtk,bhkv->bhtv', qc * jnp.exp(b), S)
        diff = b[:, :, :, None, :] - b[:, :, None, :, :]
        decay = jnp.where(tri, jnp.exp(jnp.where(tri, diff, 0.0)), 0.0)
        att = jnp.einsum('bhtsk,bhsk->bhts', qc[:, :, :, None, :] * decay, kc)
        intra = jnp.einsum('bhts,bhsv->bhtv', att, vc)
        b_last = b[:, :, -1:, :]
        S_new = jnp.exp(b_last[:, :, 0, :])[..., None] * S + jnp.einsum(
            'bhsk,bhsv->bhkv', kc * jnp.exp(b_last - b), vc)
        return S_new, inter + intra

    S0 = jnp.zeros((B, H, dk, dv), f32)
    _, o = lax.scan(step, S0, (chunks(q), chunks(k), chunks(v), chunks(log_f)))
    o = o.transpose(1, 0, 3, 2, 4).reshape(B, n * CHUNK, H, dv)
    return o[:, pad:]


def rg_lru(u, w_a, b_a, w_i, b_i, lam):
    B, T, _ = u.shape
    f32 = jnp.float32
    ub = u.reshape(B, T, RG_BLOCKS, RG_BLOCK)
    r = jax.nn.sigmoid(jnp.einsum('btnc,ncd->btnd', ub, w_a.astype(f32)).reshape(B, T, D_C) + b_a.astype(f32))
    ig = jax.nn.sigmoid(jnp.einsum('btnc,ncd->btnd', ub, w_i.astype(f32)).reshape(B, T, D_C) + b_i.astype(f32))
    log_a = -RG_C * r * jax.nn.softplus(-lam.astype(f32))
    a = jnp.exp(log_a)
    xin = jnp.sqrt(-jnp.expm1(2.0 * log_a)) * (ig * u)

    def comb(left, right):
        a1, b1 = left
        a2, b2 = right
        return a1 * a2, a2 * b1 + b2

    _, h = lax.associative_scan(comb, (a, xin), axis=1)
    return h


def dsa_attention(q, ckv, iq, ik, iw, w_uk, w_uv):
    B, T = q.shape[0], q.shape[1]
    f32 = jnp.float32
    m = N_META
    L = T - m
    k_sel = min(TOPK_MAX, L // 4)
    n_blk = L // Q_BLOCK
    q_lat = jnp.einsum('bthd,rhd->bthr', q, w_uk) * (ATT_HEAD_DIM ** -0.5)
    c_meta, c_real = ckv[:, :m], ckv[:, m:]
    ik_real = ik[:, m:].astype(f32)
    tri = jnp.tril(jnp.ones((m, m), bool))
    s_mm = jnp.einsum('bqhr,bsr->bqhs', q_lat[:, :m], c_meta).astype(f32)
    p_mm = jax.nn.softmax(jnp.where(tri[None, :, None, :], s_mm, -jnp.inf), axis=-1).astype(ckv.dtype)
    o_meta = jnp.einsum('bqhs,bsr->bqhr', p_mm, c_meta)
    s_pos = jnp.arange(L)
    bidx = jnp.arange(B)[:, None, None]

    def to_blocks(a):
        return a[:, m:].reshape((B, n_blk, Q_BLOCK) + a.shape[2:]).swapaxes(0, 1)

    def block(args):
        ql, iqb, iwb, start = args
        t_pos = start + jnp.arange(Q_BLOCK)
        rel = jax.nn.relu(jnp.einsum('bqhd,bsd->bqhs', iqb.astype(f32), ik_real) * (IDX_DIM ** -0.5))
        score = jnp.einsum('bqhs,bqh->bqs', rel, iwb.astype(f32) * (IDX_HEADS ** -0.5))
        score = jnp.where((s_pos[None, :] <= t_pos[:, None])[None], score, -jnp.inf)
        _, idx = lax.top_k(score, k_sel)
        valid = idx <= t_pos[None, :, None]
        c_sel = c_real[bidx, idx]
        s_meta = jnp.einsum('bqhr,bmr->bqhm', ql, c_meta).astype(f32)
        s_sel = jnp.einsum('bqhr,bqkr->bqhk', ql, c_sel).astype(f32)
        s_all = jnp.concatenate([s_meta, jnp.where(valid[:, :, None, :], s_sel, -jnp.inf)], axis=-1)
        p = jax.nn.softmax(s_all, axis=-1).astype(ql.dtype)
        return (jnp.einsum('bqhm,bmr->bqhr', p[..., :m], c_meta)
                + jnp.einsum('bqhk,bqkr->bqhr', p[..., m:], c_sel))

    starts = jnp.arange(n_blk, dtype=jnp.int32) * Q_BLOCK
    o_blk = lax.map(block, (to_blocks(q_lat), to_blocks(iq), to_blocks(iw), starts))
    o_real = o_blk.swapaxes(0, 1).reshape(B, L, ATT_HEADS, KV_RANK)
    o_lat = jnp.concatenate([o_meta, o_real], axis=1)
    return jnp.einsum('bthr,rhd->bthd', o_lat, w_uv)


def mixer_ab(h, w_in, w_out, lb, out_norm_g, sconv_w):
    B, T, _ = h.shape
    q, f, i, g, sx, sb, sc = split_cols(h @ w_in, AB_SIZES)
    hd = lambda a: a.reshape(B, T, HGRN_HEADS, HGRN_HEAD)
    o = hgrn2_chunked(hd(q), hd(f), hd(i), lb.reshape(HGRN_HEADS, HGRN_HEAD))
    o = rms_norm(o, out_norm_g).reshape(B, T, D_A).astype(h.dtype) * jax.nn.silu(g)
    yb = sb * causal_depthwise_conv(sc * sx, sconv_w)
    return jnp.concatenate([o, yb], axis=-1) @ w_out


def mixer_cd(h, w_in, w_out, conv_w, conv_b, w_a, b_a, w_i, b_i, lam, kv_norm_g, w_uk, w_uv):
    B, T, _ = h.shape
    rx, ry, q, ckv, iq, ik, iw = split_cols(h @ w_in, CD_SIZES)
    u = causal_depthwise_conv(rx, conv_w) + conv_b
    hc = rg_lru(u.astype(jnp.float32), w_a, b_a, w_i, b_i, lam).astype(h.dtype) * jax.nn.gelu(ry)
    att = dsa_attention(q.reshape(B, T, ATT_HEADS, ATT_HEAD_DIM), rms_norm(ckv, kv_norm_g),
                        iq.reshape(B, T, IDX_HEADS, IDX_DIM), ik, iw, w_uk, w_uv)
    return jnp.concatenate([hc, att.reshape(B, T, D_D)], axis=-1) @ w_out


def swiglu(h, w1, w3, w2):
    return (jax.nn.silu(h @ w1) * (h @ w3)) @ w2


def setup_inputs(seed: int = 0) -> dict:
    key = jax.random.key(seed)
    ks = iter(jax.random.split(key, 40))
    f32 = jnp.float32
    nrm = lambda shape, scale: jax.random.normal(next(ks), shape, f32) * scale
    gain = lambda shape: 1.0 + 0.05 * jax.random.normal(next(ks), shape, f32)
    u = jax.random.uniform(next(ks), (N_C_LAYERS, D_C), f32, minval=0.9, maxval=0.999)
    s = u ** (1.0 / RG_C)
    return {
        'x': nrm((BATCH, SEQ, D_MODEL), 1.0),
        'meta_tokens': nrm((N_META, D_MODEL), 1.0),
        'ln_mix_pre': gain((DEPTH, D_MODEL)),
        'ln_mix_post': gain((DEPTH, D_MODEL)),
        'ln_ffn_pre': gain((DEPTH, D_MODEL)),
        'ln_ffn_post': gain((DEPTH, D_MODEL)),
        'ffn_w1': nrm((DEPTH, D_MODEL, D_FF), D_MODEL ** -0.5),
        'ffn_w3': nrm((DEPTH, D_MODEL, D_FF), D_MODEL ** -0.5),
        'ffn_w2': nrm((DEPTH, D_FF, D_MODEL), D_FF ** -0.5),
        'ab_w_in': nrm((N_A_LAYERS, D_MODEL, sum(AB_SIZES)), D_MODEL ** -0.5),
        'ab_w_out': nrm((N_A_LAYERS, D_A + D_B, D_MODEL), (D_A + D_B) ** -0.5),
        'hgrn_lb_logits': nrm((N_A_LAYERS + 1, D_A), 0.5),
        'hgrn_out_norm': gain((N_A_LAYERS, HGRN_HEAD)),
        'sconv_w': nrm((N_A_LAYERS, SCONV_W, D_B), SCONV_W ** -0.5),
        'cd_w_in': nrm((N_C_LAYERS, D_MODEL, sum(CD_SIZES)), D_MODEL ** -0.5),
        'cd_w_out': nrm((N_C_LAYERS, D_C + D_D, D_MODEL), (D_C + D_D) ** -0.5),
        'rg_conv_w': nrm((N_C_LAYERS, RG_CONV_W, D_C), RG_CONV_W ** -0.5),
        'rg_conv_b': nrm((N_C_LAYERS, D_C), 0.01),
        'rg_w_a': nrm((N_C_LAYERS, RG_BLOCKS, RG_BLOCK, RG_BLOCK), RG_BLOCK ** -0.5),
        'rg_b_a': nrm((N_C_LAYERS, D_C), 0.01),
        'rg_w_i': nrm((N_C_LAYERS, RG_BLOCKS, RG_BLOCK, RG_BLOCK), RG_BLOCK ** -0.5),
        'rg_b_i': nrm((N_C_LAYERS, D_C), 0.01),
        'rg_lambda': jnp.log(s) - jnp.log1p(-s),
        'mla_kv_norm': gain((N_C_LAYERS, KV_RANK)),
        'mla_w_uk': nrm((N_C_LAYERS, KV_RANK, ATT_HEADS, ATT_HEAD_DIM), KV_RANK ** -0.5),
        'mla_w_uv': nrm((N_C_LAYERS, KV_RANK, ATT_HEADS, ATT_HEAD_DIM), KV_RANK ** -0.5),
    }


def reference(x, meta_tokens, ln_mix_pre, ln_mix_post, ln_ffn_pre, ln_ffn_post, ffn_w1, ffn_w3, ffn_w2,
              ab_w_in, ab_w_out, hgrn_lb_logits, hgrn_out_norm, sconv_w,
              cd_w_in, cd_w_out, rg_conv_w, rg_conv_b, rg_w_a, rg_b_a, rg_w_i, rg_b_i, rg_lambda,
              mla_kv_norm, mla_w_uk, mla_w_uv):
    B = x.shape[0]
    meta = jnp.broadcast_to(meta_tokens.astype(x.dtype)[None], (B, N_META, x.shape[-1]))
    h = jnp.concatenate([meta, x], axis=1)
    lb_all = jnp.cumsum(jax.nn.softmax(hgrn_lb_logits.astype(jnp.float32), axis=0), axis=0)
    for l in range(DEPTH):
        j = l // 2
        hn = rms_norm(h, ln_mix_pre[l])
        if l % 2 == 0:
            mix = mixer_ab(hn, ab_w_in[j], ab_w_out[j], lb_all[j], hgrn_out_norm[j], sconv_w[j])
        else:
            mix = mixer_cd(hn, cd_w_in[j], cd_w_out[j], rg_conv_w[j], rg_conv_b[j], rg_w_a[j], rg_b_a[j],
                           rg_w_i[j], rg_b_i[j], rg_lambda[j], mla_kv_norm[j], mla_w_uk[j], mla_w_uv[j])
        h = h + rms_norm(mix, ln_mix_post[l])
        hn = rms_norm(h, ln_ffn_pre[l])
        h = h + rms_norm(swiglu(hn, ffn_w1[l], ffn_w3[l], ffn_w2[l]), ln_ffn_post[l])
    return h[:, N_META:]
```

```python
import numpy as np
from contextlib import ExitStack
import concourse.bass as bass
import concourse.mybir as mybir
from concourse.bass_utils import run_bass_kernel_spmd

F32 = mybir.dt.float32
BF16 = mybir.dt.bfloat16
AF = mybir.ActivationFunctionType
ALU = mybir.AluOpType
AX = mybir.AxisListType

D = 2048
DFF = 5632
NMETA = 16
SEQ = 8192
EPS = 1e-6
NCORES = 8


class T:
    def __init__(self, ap, name=""):
        self.ap = ap
        self.name = name
        self.w = None
        self.r = []

    def __getitem__(self, k):
        return self.ap[k]


class View(T):
    pass


class Sched:
    def __init__(self, nc, stack):
        self.nc = nc
        self.stack = stack
        self.eng = {"pe": nc.tensor, "act": nc.scalar, "dve": nc.vector, "pool": nc.gpsimd, "sp": nc.sync}
        self.sem = {k: stack.enter_context(nc.semaphore("s_" + k)) for k in self.eng}
        self.cnt = {k: 0 for k in self.eng}
        self.seen = {k: {} for k in self.eng}
        self.nsem = 0
        self.ninst = 0

    def sb(self, name, shape, dt):
        t = self.stack.enter_context(self.nc.sbuf_tensor(name, shape, dt))
        return T(t, name)

    def ps(self, name, shape=(128, 512), dt=F32):
        t = self.stack.enter_context(self.nc.psum_tensor(name, list(shape), dt))
        return T(t, name)

    def dsem(self, name):
        self.nsem += 1
        return [self.stack.enter_context(self.nc.semaphore(name)), 0]

    def _wait(self, e, tok):
        if tok is None:
            return
        sem, val, owner = tok
        if owner == e and e == "pe":
            return
        seen = self.seen[e]
        key = id(sem)
        if seen.get(key, 0) >= val:
            return
        self.eng[e].wait_ge(sem, val)
        seen[key] = val

    def deps(self, e, reads, writes):
        for t in reads:
            self._wait(e, t.w)
        for t in writes:
            self._wait(e, t.w)
            for tok in t.r:
                self._wait(e, tok)

    def done(self, tok, reads, writes):
        for t in reads:
            t.r.append(tok)
            if len(t.r) > 64:
                t.r = t.r[-48:]
        for t in writes:
            t.w = tok
            t.r = []

    def op(self, e, fn, reads=(), writes=()):
        self.deps(e, reads, writes)
        ins = fn()
        self.cnt[e] += 1
        self.ninst += 1
        ins.then_inc(self.sem[e], 1)
        tok = (self.sem[e], self.cnt[e], e)
        self.done(tok, reads, writes)

    def dma(self, q, out, in_, reads=(), writes=(), sem=None, **kw):
        self.deps(q, reads, writes)
        ins = self.eng[q].dma_start(out=out, in_=in_, **kw)
        sem[1] += 16
        self.ninst += 1
        ins.then_inc(sem[0], 16)
        tok = (sem[0], sem[1], "dma")
        self.done(tok, reads, writes)

    def finish(self, tiles):
        for t in tiles:
            self._wait("sp", t.w)
            for tok in t.r:
                self._wait("sp", tok)

    def mm(self, out_t, out_ap, lhsT_t, lhsT_ap, rhs_t, rhs_ap, start, stop, **kw):
        nc = self.nc
        self.op("pe", lambda: nc.tensor.matmul(out_ap, lhsT=lhsT_ap, rhs=rhs_ap, start=start, stop=stop, **kw),
                reads=[lhsT_t, rhs_t], writes=[out_t])


def trim_reads(t):
    pass


CD_M = 4432
CD_MB = 35


DBG = {'ss': True, 'norm': True, 'castpool': True}


def build_rowlocal(layer, stop=99, inproj_mb=None):
    with_cd = (layer == 0)
    CDMB = inproj_mb if inproj_mb else CD_MB
    if layer == 0:
        NT = 1040
        halves = [(0, 528, [(0, 512), (512, 16)]), (528, 512, [(0, 512)])]
    else:
        NT = 1024
        halves = [(0, 512, [(0, 512)]), (512, 512, [(0, 512)])]
    HN = 528
    nc = bass.Bass("TRN2", target_bir_lowering=False)
    hT_in = nc.dram_tensor("hT_in", [128, 16, NT], F32, kind="ExternalInput").ap()
    if not inproj_mb:
        mixT_in = nc.dram_tensor("mixT_in", [128, 16, NT], F32, kind="ExternalInput").ap()
        w_out_t = nc.dram_tensor("w_out_t", [16, 128, 16, 128], F32, kind="ExternalInput").ap()
        w1_t = nc.dram_tensor("w1_t", [44, 128, 16, 128], F32, kind="ExternalInput").ap()
        w3_t = nc.dram_tensor("w3_t", [44, 128, 16, 128], F32, kind="ExternalInput").ap()
        w2_t = nc.dram_tensor("w2_t", [16, 128, 44, 128], F32, kind="ExternalInput").ap()
        hT_out = nc.dram_tensor("hT_out", [128, 16, NT], F32, kind="ExternalOutput").ap()
    NG = 4
    gains = nc.dram_tensor("gains", [128, NG, 16], F32, kind="ExternalInput").ap()
    if with_cd:
        cd_t = nc.dram_tensor("cd_t", [CDMB, 128, 16, 128], F32, kind="ExternalInput").ap()
        proj_out = nc.dram_tensor("proj_out", [CDMB, 128, NT], F32, kind="ExternalOutput").ap()

    with ExitStack() as st:
        S = Sched(nc, st)
        hT = S.sb("hT", [128, 16, HN], F32)
        xb = S.sb("xb", [128, 16, HN], BF16)
        y = S.sb("y", [128, 16, HN], F32)
        m = S.sb("m", [128, 44, HN], BF16)
        g_sb = S.sb("g_sb", [128, NG, 16], F32)
        ones = S.sb("ones", [128, 128], BF16)
        rstd = S.sb("rstd", [128, HN], F32)
        lnt = S.sb("lnt", [128, HN], F32)
        NSTG = 2
        stg = [S.sb(f"stg{i}", [128, 22, 128], F32) for i in range(NSTG)]
        NWB = 3
        wbf = [S.sb(f"wbf{i}", [128, 22, 128], BF16) for i in range(NWB)]
        stg_sem = [S.dsem(f"stgsem{i}") for i in range(NSTG)]
        sq = [S.sb(f"sq{i}", [128, HN], BF16) for i in range(2)]
        tmp = [S.sb(f"tmp{i}", [128, HN], F32) for i in range(2)]
        ostg = [S.sb(f"ostg{i}", [128, HN], F32) for i in range(2)]
        ostg_sem = [S.dsem(f"ostgsem{i}") for i in range(2)]
        accb = [[S.ps(f"acc{w}{b}") for b in range(2)] for w in range(2)]
        small = [S.ps("small0"), S.ps("small1")]
        smallv = [[small[w] for b in range(2)] for w in range(2)]
        ssb = S.ps("ssb")
        sss = S.ps("sss")
        io_sem = [S.dsem("io0"), S.dsem("io1"), S.dsem("io2"), S.dsem("io3")]

        S.dma("sp", g_sb[:], gains[:, :, :], writes=[g_sb], sem=io_sem[2])
        S.op("dve", lambda: nc.vector.memset(ones[:], 1.0), writes=[ones])

        state = {"fill": 0, "wb": 0, "blk": 0, "sq": 0, "tmp": 0, "ostg": 0, "cast": 0}

        def load_w(src_ap, kn):
            i = state["fill"] % NSTG
            state["fill"] += 1
            S.dma("sp", stg[i][:, 0:kn, :], src_ap, writes=[stg[i]], sem=stg_sem[i])
            j = state["wb"] % NWB
            state["wb"] += 1
            c = state["cast"] % 3
            state["cast"] += 1
            if c != 2:
                S.op("act", lambda: nc.scalar.copy(out=wbf[j][:, 0:kn, :], in_=stg[i][:, 0:kn, :]),
                     reads=[stg[i]], writes=[wbf[j]])
            else:
                S.op("dve", lambda: nc.vector.tensor_copy(out=wbf[j][:, 0:kn, :], in_=stg[i][:, 0:kn, :]),
                     reads=[stg[i]], writes=[wbf[j]])
            return wbf[j]

        def acc_aps(w, b, ntiles):
            res = []
            for (n0, nsz) in ntiles:
                if nsz == 512:
                    res.append((accb[w][b], accb[w][b][:, 0:512]))
                else:
                    off = (w * 2 + b) * 16
                    res.append((smallv[w][b], smallv[w][b][:, off:off + nsz]))
            return res

        def linear(x_t, KC, wsrcs, nmb, ntiles, consume, fills):
            for mb in range(nmb):
                b = state["blk"] % 2
                state["blk"] += 1
                accs = []
                for wi, wsrc in enumerate(wsrcs):
                    aps = acc_aps(wi, b, ntiles)
                    for fi, (k0, kn) in enumerate(fills):
                        wt = load_w(wsrc[mb, :, k0:k0 + kn, :], kn)
                        for kk in range(kn):
                            kc = k0 + kk
                            for ti, (n0, nsz) in enumerate(ntiles):
                                at, aap = aps[ti]
                                S.mm(at, aap, wt, wt[:, kk, :], x_t, x_t[:, kc, n0:n0 + nsz],
                                     start=(kc == 0), stop=(kc == KC - 1))
                    accs.append(aps)
                consume(mb, accs)

        def rstd_from(ss_list, ntiles):
            for (sst, ssap), (n0, nsz) in zip(ss_list, ntiles):
                S.op("act", lambda: nc.scalar.activation(out=lnt[:, n0:n0 + nsz], in_=ssap, func=AF.Ln,
                                                         bias=eps_t[:, 0:1], scale=1.0 / D),
                     reads=[sst, eps_t], writes=[lnt])
                S.op("act", lambda: nc.scalar.activation(out=rstd[:, n0:n0 + nsz], in_=lnt[:, n0:n0 + nsz],
                                                         func=AF.Exp, scale=-0.5),
                     reads=[lnt], writes=[rstd])

        eps_t = S.sb("eps_t", [128, 1], F32)
        S.op("dve", lambda: nc.vector.memset(eps_t[:], EPS), writes=[eps_t])

        def ss_aps(ntiles):
            res = []
            for (n0, nsz) in ntiles:
                if nsz == 512:
                    res.append((ssb, ssb[:, 0:512]))
                else:
                    res.append((sss, sss[:, 0:nsz]))
            return res

        def ss_accum(src_t, src_ap_fn, kc, ntiles, from_psum_aps=None):
            ssl = ss_aps(ntiles)
            for ti, (n0, nsz) in enumerate(ntiles):
                i = state["sq"] % 2
                state["sq"] += 1
                if from_psum_aps is not None:
                    st_, sap = from_psum_aps[ti]
                else:
                    st_, sap = src_t, src_ap_fn(n0, nsz)
                S.op("act", lambda: nc.scalar.activation(out=sq[i][:, 0:nsz], in_=sap, func=AF.Square),
                     reads=[st_], writes=[sq[i]])
                S.mm(ssl[ti][0], ssl[ti][1], ones, ones[:], sq[i], sq[i][:, 0:nsz], start=(kc == 0), stop=(kc == 15))

        def post_norm_residual(gidx, ntiles, hw):
            for kc in range(16):
                i = state["tmp"] % 2
                state["tmp"] += 1
                S.op("dve", lambda: nc.vector.scalar_tensor_tensor(
                    out=tmp[i][:, 0:hw], in0=y[:, kc, 0:hw], scalar=g_sb[:, gidx, kc:kc + 1], in1=rstd[:, 0:hw],
                    op0=ALU.mult, op1=ALU.mult), reads=[y, g_sb, rstd], writes=[tmp[i]])
                S.op("dve", lambda: nc.vector.tensor_tensor(out=hT[:, kc, 0:hw], in0=hT[:, kc, 0:hw],
                                                             in1=tmp[i][:, 0:hw], op=ALU.add),
                     reads=[tmp[i], hT], writes=[hT])

        def pre_norm(gidx, ntiles, hw):
            for kc in range(16):
                ss_accum(hT, lambda n0, nsz: hT[:, kc, n0:n0 + nsz], kc, ntiles)
            rstd_from(ss_aps(ntiles), ntiles)
            for kc in range(16):
                S.op("dve", lambda: nc.vector.scalar_tensor_tensor(
                    out=xb[:, kc, 0:hw], in0=hT[:, kc, 0:hw], scalar=g_sb[:, gidx, kc:kc + 1], in1=rstd[:, 0:hw],
                    op0=ALU.mult, op1=ALU.mult), reads=[hT, g_sb, rstd], writes=[xb])

        def consume_y(ntiles):
            def f(mb, accs):
                aps = accs[0]
                for ti, (n0, nsz) in enumerate(ntiles):
                    at, aap = aps[ti]
                    S.op("dve", lambda: nc.vector.tensor_copy(out=y[:, mb, n0:n0 + nsz], in_=aap),
                         reads=[at], writes=[y])
                if DBG['ss']:
                    ss_accum(y, lambda n0, nsz: y[:, mb, n0:n0 + nsz], mb, ntiles)
            return f

        for (h0, hw, ntiles) in halves:
            if inproj_mb:
                for k4 in range(0, 16, 2):
                    S.dma("sp", hT[:, k4:k4 + 2, 0:hw], hT_in[:, k4:k4 + 2, h0:h0 + hw], writes=[hT], sem=io_sem[0])
                pre_norm(0, ntiles, hw)

                def consume_ip(mb, accs):
                    i = state["ostg"] % 2
                    state["ostg"] += 1
                    for ti, (n0, nsz) in enumerate(ntiles):
                        at, aap = accs[0][ti]
                        S.op("dve", lambda: nc.vector.tensor_copy(out=ostg[i][:, n0:n0 + nsz], in_=aap),
                             reads=[at], writes=[ostg[i]])
                    S.dma("sp", proj_out[mb, :, h0:h0 + hw], ostg[i][:, 0:hw], reads=[ostg[i]], sem=ostg_sem[i])
                linear(xb, 16, [cd_t], CDMB, ntiles, consume_ip, [(0, 16)])
                continue
            for k4 in range(0, 16, 2):
                S.dma("sp", hT[:, k4:k4 + 2, 0:hw], hT_in[:, k4:k4 + 2, h0:h0 + hw], writes=[hT], sem=io_sem[0])
                S.dma("sp", y[:, k4:k4 + 2, 0:hw], mixT_in[:, k4:k4 + 2, h0:h0 + hw], writes=[y], sem=io_sem[1])
            for kc in range(16):
                eng = "dve"
                if eng == "dve":
                    S.op("dve", lambda: nc.vector.tensor_copy(out=xb[:, kc, 0:hw], in_=y[:, kc, 0:hw]),
                         reads=[y], writes=[xb])
                else:
                    S.op("pool", lambda: nc.gpsimd.tensor_copy(out=xb[:, kc, 0:hw], in_=y[:, kc, 0:hw]),
                         reads=[y], writes=[xb])
            if stop >= 1:
                linear(xb, 16, [w_out_t], 16, ntiles, consume_y(ntiles), [(0, 16)])
                if DBG['norm']:
                    rstd_from(ss_aps(ntiles), ntiles)
                    post_norm_residual(0, ntiles, hw)
                else:
                    for kc in range(16):
                        S.op("dve", lambda: nc.vector.tensor_copy(out=hT[:, kc, 0:hw], in_=y[:, kc, 0:hw]), reads=[y], writes=[hT])
            if stop == 0:
                for kc in range(16):
                    S.op("dve", lambda: nc.vector.tensor_copy(out=hT[:, kc, 0:hw], in_=y[:, kc, 0:hw]), reads=[y], writes=[hT])
            if stop <= 1:
                for k4 in range(0, 16, 2):
                    S.dma("sp", hT_out[:, k4:k4 + 2, h0:h0 + hw], hT[:, k4:k4 + 2, 0:hw], reads=[hT], sem=io_sem[3])
                continue
            pre_norm(1, ntiles, hw)

            def consume_ab(mb, accs):
                for ti, (n0, nsz) in enumerate(ntiles):
                    i = state["tmp"] % 2
                    state["tmp"] += 1
                    at, aap = accs[0][ti]
                    bt, bap = accs[1][ti]
                    S.op("act", lambda: nc.scalar.activation(out=tmp[i][:, 0:nsz], in_=aap, func=AF.Silu),
                         reads=[at], writes=[tmp[i]])
                    S.op("dve", lambda: nc.vector.tensor_tensor(out=m[:, mb, n0:n0 + nsz], in0=tmp[i][:, 0:nsz],
                                                                 in1=bap, op=ALU.mult),
                         reads=[tmp[i], bt], writes=[m])
            linear(xb, 16, [w1_t, w3_t], 44, ntiles, consume_ab, [(0, 16)])
            linear(m, 44, [w2_t], 16, ntiles, consume_y(ntiles), [(0, 22), (22, 22)])
            rstd_from(ss_aps(ntiles), ntiles)
            post_norm_residual(2, ntiles, hw)
            for k4 in range(0, 16, 2):
                S.dma("sp", hT_out[:, k4:k4 + 2, h0:h0 + hw], hT[:, k4:k4 + 2, 0:hw], reads=[hT], sem=io_sem[3])
            if with_cd:
                pre_norm(3, ntiles, hw)

                def consume_cd(mb, accs):
                    i = state["ostg"] % 2
                    state["ostg"] += 1
                    for ti, (n0, nsz) in enumerate(ntiles):
                        at, aap = accs[0][ti]
                        S.op("dve", lambda: nc.vector.tensor_copy(out=ostg[i][:, n0:n0 + nsz], in_=aap),
                             reads=[at], writes=[ostg[i]])
                    S.dma("sp", proj_out[mb, :, h0:h0 + hw], ostg[i][:, 0:hw], reads=[ostg[i]], sem=ostg_sem[i])
                linear(xb, 16, [cd_t], CDMB, ntiles, consume_cd, [(0, 16)])
        S.finish([hT, ostg[0], ostg[1]] if (with_cd or inproj_mb) else [hT])
        print("rowlocal layer", layer, "instructions", S.ninst)
    return nc


def tile_w(w, mw=128):
    K, M = w.shape
    Mp = -(-M // mw) * mw
    if Mp != M:
        w = np.concatenate([w, np.zeros((K, Mp - M), w.dtype)], axis=1)
    return np.ascontiguousarray(w.reshape(K // 128, 128, Mp // mw, mw).transpose(2, 1, 0, 3))


def fm(a):
    Tn, Fn = a.shape
    return np.ascontiguousarray(a.reshape(Tn, Fn // 128, 128).transpose(2, 1, 0))


def unfm(a):
    p, kc, Tn = a.shape
    return np.ascontiguousarray(a.transpose(2, 1, 0).reshape(Tn, kc * 128))


def gvec(g):
    return np.ascontiguousarray(g.reshape(16, 128).T)


_NC_CACHE = {}


def run_rowlocal(layer, h_tok, mix_tok, w_out, w1, w3, w2, gain_list, cd_w=None):
    if ("rl", layer) not in _NC_CACHE:
        _NC_CACHE[("rl", layer)] = build_rowlocal(layer)
    nc = _NC_CACHE[("rl", layer)]
    gains = np.ascontiguousarray(np.stack([gvec(g) for g in gain_list], axis=1)).astype(np.float32)
    common = {"w_out_t": tile_w(w_out), "w1_t": tile_w(w1), "w3_t": tile_w(w3), "w2_t": tile_w(w2), "gains": gains}
    if layer == 0:
        common["cd_t"] = tile_w(cd_w)
    in_maps = []
    idxs = []
    for c in range(NCORES):
        if layer == 0:
            r0 = NMETA + c * 1024
            idx = np.concatenate([np.arange(r0, r0 + 512), np.arange(0, NMETA), np.arange(r0 + 512, r0 + 1024)])
        else:
            idx = np.arange(c * 1024, (c + 1) * 1024)
        idxs.append(idx)
        d = dict(common)
        d["hT_in"] = fm(h_tok[idx])
        d["mixT_in"] = fm(mix_tok[idx])
        in_maps.append(d)
    res = run_bass_kernel_spmd(nc, in_maps, core_ids=list(range(NCORES)))
    h_out = np.zeros_like(h_tok)
    proj = np.zeros((h_tok.shape[0], CD_M), np.float32) if layer == 0 else None
    for c in range(NCORES):
        r = res.results[c]
        h_out[idxs[c]] = unfm(np.asarray(r["hT_out"]))
        if layer == 0:
            p = np.asarray(r["proj_out"])
            pt = p.transpose(2, 0, 1).reshape(p.shape[2], CD_MB * 128)[:, :CD_M]
            proj[idxs[c]] = pt
    return h_out, proj


TP0 = 8704
NCONST = 128 + 512 + 512


def mix0_consts():
    c = np.zeros((128, NCONST), np.float32)
    c[:, 0:128] = np.eye(128, dtype=np.float32)
    tri = (np.arange(64)[None, :] >= np.arange(64)[:, None]).astype(np.float32)
    c[0:64, 128:640] = np.tile(tri, (1, 8))
    r = np.ones(512, np.float32)
    r[0::64] = 0.0
    c[:, 640:1152] = r[None, :]
    return c


def build_mix0(ntile=17):
    TP = ntile * 512
    nc = bass.Bass("TRN2", target_bir_lowering=False)
    pj = nc.dram_tensor("pj", [7, 128, TP], F32, kind="ExternalInput").ap()
    par = nc.dram_tensor("par", [128, 8], F32, kind="ExternalInput").ap()
    cst = nc.dram_tensor("cst", [128, NCONST], F32, kind="ExternalInput").ap()
    mo = nc.dram_tensor("mo", [2, 128, TP], F32, kind="ExternalOutput").ap()
    with ExitStack() as st:
        S = Sched(nc, st)
        f32t = lambda n, w=512: S.sb(n, [128, w], F32)
        bft = lambda n, w=512: S.sb(n, [128, w], BF16)
        par_sb = f32t("par_sb", 8)
        cst_sb = f32t("cst_sb", NCONST)
        ident = bft("ident", 128)
        ones = bft("ones", 128)
        lb = f32t("lb", 1); oml = f32t("oml", 1); eps_t = f32t("eps_t", 1)
        inp = [[f32t(f"in{b}_{i}") for i in range(7)] for b in range(2)]
        in_sem = [[S.dsem(f"insem{b}_{i}") for i in range(7)] for b in range(2)]
        sig = f32t("sig"); f_ = f32t("f_"); logf = f32t("logf"); b_ = f32t("b_"); k_ = f32t("k_")
        eb = f32t("eb"); enb = f32t("enb"); e2 = f32t("e2")
        Qt = bft("Qt"); Kt = bft("Kt"); Kp = bft("Kp"); Vb = bft("Vb")
        Vtok = S.sb("Vtok", [64, 8, 128], BF16); Ktok = S.sb("Ktok", [64, 8, 128], BF16)
        attm = S.sb("attm", [64, 512], BF16)
        Sf = f32t("Sf", 128)
        NSB = 4
        Sb = [bft(f"Sb{i}", 128) for i in range(NSB)]
        o_sb = f32t("o_sb"); osq = bft("osq"); lnt = f32t("lnt"); rstd = f32t("rstd"); sg = f32t("sg")
        res = [f32t(f"res{i}") for i in range(2)]; res_sem = [S.dsem(f"ressem{i}") for i in range(2)]
        ub = [f32t(f"ub{i}", 514) for i in range(2)]
        t1 = f32t("t1")
        yb = [f32t(f"yb{i}") for i in range(2)]; yb_sem = [S.dsem(f"ybsem{i}") for i in range(2)]
        tpv = S.ps("tpv", (128, 1024), BF16); tpk = S.ps("tpk", (128, 1024), BF16)
        att = S.ps("att"); o_ps = S.ps("o_ps"); U = [S.ps("U0"), S.ps("U1")]; sso = S.ps("sso")
        csem = S.dsem("csem")
        csem2 = S.dsem("csem2")
        S.dma("sp", par_sb[:], par[:, :], writes=[par_sb], sem=csem)
        S.dma("sp", cst_sb[:], cst[:, :], writes=[cst_sb], sem=csem2)
        tri = View(cst_sb.ap, "tri"); rmask = View(cst_sb.ap, "rmask")
        S.op("dve", lambda: nc.vector.tensor_copy(out=ident[:], in_=cst_sb[:, 0:128]), reads=[cst_sb], writes=[ident])
        S.op("dve", lambda: nc.vector.memset(ones[:], 1.0), writes=[ones])
        S.op("dve", lambda: nc.vector.memset(eps_t[:], EPS), writes=[eps_t])
        S.op("dve", lambda: nc.vector.memset(Sf[:], 0.0), writes=[Sf])
        S.op("dve", lambda: nc.vector.memset(Sb[0][:], 0.0), writes=[Sb[0]])
        S.op("dve", lambda: nc.vector.memset(ub[0][:], 0.0), writes=[ub[0]])
        S.op("dve", lambda: nc.vector.tensor_tensor(out=oml[:], in0=par_sb[:, 0:1], in1=par_sb[:, 1:2], op=ALU.subtract),
             reads=[par_sb], writes=[oml])
        S.op("act", lambda: nc.scalar.activation(out=lb[:], in_=oml[:], func=AF.Sigmoid), reads=[oml], writes=[lb])
        S.op("dve", lambda: nc.vector.tensor_scalar(out=oml[:], in0=lb[:], scalar1=-1.0, scalar2=1.0, op0=ALU.mult, op1=ALU.add),
             reads=[lb], writes=[oml])
        sbi = 0
        for tt in range(ntile):
            c0 = tt * 512
            bi = tt % 2
            I = inp[bi]
            for i in range(7):
                S.dma("sp", I[i][:], pj[i, :, c0:c0 + 512], writes=[I[i]], sem=in_sem[bi][i])
            q, fl, v, g, sx, sbb, sc = I
            S.op("act", lambda: nc.scalar.activation(out=sig[:], in_=fl[:], func=AF.Sigmoid), reads=[fl], writes=[sig])
            S.op("dve", lambda: nc.vector.tensor_scalar(out=f_[:], in0=sig[:], scalar1=oml[:, 0:1], scalar2=lb[:, 0:1],
                                                        op0=ALU.mult, op1=ALU.add), reads=[sig, oml, lb], writes=[f_])
            S.op("act", lambda: nc.scalar.activation(out=logf[:], in_=f_[:], func=AF.Ln), reads=[f_], writes=[logf])
            S.op("dve", lambda: nc.vector.tensor_tensor_scan(out=b_[:], data0=cst_sb[:, 640:1152], data1=logf[:], initial=0.0,
                                                             op0=ALU.mult, op1=ALU.add), reads=[cst_sb, logf], writes=[b_])
            S.op("dve", lambda: nc.vector.tensor_scalar(out=k_[:], in0=f_[:], scalar1=-1.0, scalar2=1.0, op0=ALU.mult, op1=ALU.add),
                 reads=[f_], writes=[k_])
            S.op("act", lambda: nc.scalar.activation(out=eb[:], in_=b_[:], func=AF.Exp), reads=[b_], writes=[eb])
            S.op("act", lambda: nc.scalar.activation(out=enb[:], in_=b_[:], func=AF.Exp, scale=-1.0), reads=[b_], writes=[enb])
            for j in range(8):
                S.op("act", lambda: nc.scalar.activation(out=e2[:, j * 64:(j + 1) * 64], in_=b_[:, j * 64:(j + 1) * 64], func=AF.Exp,
                                                         scale=-1.0, bias=b_[:, j * 64 + 63:j * 64 + 64]), reads=[b_], writes=[e2])
            S.op("dve", lambda: nc.vector.tensor_tensor(out=Qt[:], in0=q[:], in1=eb[:], op=ALU.mult), reads=[q, eb], writes=[Qt])
            S.op("dve", lambda: nc.vector.tensor_tensor(out=Kt[:], in0=k_[:], in1=enb[:], op=ALU.mult), reads=[k_, enb], writes=[Kt])
            S.op("dve", lambda: nc.vector.tensor_tensor(out=Kp[:], in0=k_[:], in1=e2[:], op=ALU.mult), reads=[k_, e2], writes=[Kp])
            S.op("dve", lambda: nc.vector.tensor_copy(out=Vb[:], in_=v[:]), reads=[v], writes=[Vb])
            for j in range(8):
                S.op("pe", lambda: nc.tensor.transpose(out=tpv[0:64, j * 128:(j + 1) * 128], in_=Vb[:, j * 64:(j + 1) * 64], identity=ident[:]),
                     reads=[Vb, ident], writes=[tpv])
            for j in range(8):
                S.op("pe", lambda: nc.tensor.transpose(out=tpk[0:64, j * 128:(j + 1) * 128], in_=Kp[:, j * 64:(j + 1) * 64], identity=ident[:]),
                     reads=[Kp, ident], writes=[tpk])
            S.op("act", lambda: nc.scalar.copy(out=Vtok[:, :, :], in_=tpv[0:64, :]), reads=[tpv], writes=[Vtok])
            S.op("act", lambda: nc.scalar.copy(out=Ktok[:, :, :], in_=tpk[0:64, :]), reads=[tpk], writes=[Ktok])
            for j in range(8):
                S.mm(att, att[0:64, j * 64:(j + 1) * 64], Kt, Kt[:, j * 64:(j + 1) * 64], Qt, Qt[:, j * 64:(j + 1) * 64], start=True, stop=True)
            S.op("dve", lambda: nc.vector.tensor_tensor(out=attm[:], in0=att[0:64, :], in1=cst_sb[0:64, 128:640], op=ALU.mult),
                 reads=[att, cst_sb], writes=[attm])
            for j in range(8):
                Uj = U[j // 4]
                S.mm(Uj, Uj[:, (j % 4) * 128:(j % 4 + 1) * 128], Ktok, Ktok[:, j, :], Vtok, Vtok[:, j, :], start=True, stop=True)
            for j in range(8):
                cur = Sb[sbi % NSB]
                nxt = Sb[(sbi + 1) % NSB]
                sbi += 1
                S.mm(o_ps, o_ps[:, j * 64:(j + 1) * 64], Vtok, Vtok[:, j, :], attm, attm[:, j * 64:(j + 1) * 64], start=True, stop=False)
                S.mm(o_ps, o_ps[:, j * 64:(j + 1) * 64], cur, cur[:], Qt, Qt[:, j * 64:(j + 1) * 64], start=False, stop=True)
                Uj = U[j // 4]
                uap = Uj[:, (j % 4) * 128:(j % 4 + 1) * 128]
                dcol = eb[:, j * 64 + 63:j * 64 + 64]
                S.op("dve", lambda: nc.vector.scalar_tensor_tensor(out=nxt[:], in0=Sf[:], scalar=dcol, in1=uap, op0=ALU.mult, op1=ALU.add),
                     reads=[Sf, eb, Uj], writes=[nxt])
                S.op("dve", lambda: nc.vector.scalar_tensor_tensor(out=Sf[:], in0=Sf[:], scalar=dcol, in1=uap, op0=ALU.mult, op1=ALU.add),
                     reads=[Sf, eb, Uj], writes=[Sf])
            S.op("act", lambda: nc.scalar.copy(out=o_sb[:], in_=o_ps[:]), reads=[o_ps], writes=[o_sb])
            S.op("act", lambda: nc.scalar.activation(out=osq[:], in_=o_sb[:], func=AF.Square), reads=[o_sb], writes=[osq])
            S.mm(sso, sso[:], ones, ones[:], osq, osq[:], start=True, stop=True)
            S.op("act", lambda: nc.scalar.activation(out=lnt[:], in_=sso[:], func=AF.Ln, bias=eps_t[:, 0:1], scale=1.0 / 128),
                 reads=[sso, eps_t], writes=[lnt])
            S.op("act", lambda: nc.scalar.activation(out=rstd[:], in_=lnt[:], func=AF.Exp, scale=-0.5), reads=[lnt], writes=[rstd])
            S.op("act", lambda: nc.scalar.activation(out=sg[:], in_=g[:], func=AF.Silu), reads=[g], writes=[sg])
            r_ = res[bi]
            S.op("dve", lambda: nc.vector.scalar_tensor_tensor(out=r_[:], in0=o_sb[:], scalar=par_sb[:, 2:3], in1=rstd[:], op0=ALU.mult, op1=ALU.mult),
                 reads=[o_sb, par_sb, rstd], writes=[r_])
            S.op("dve", lambda: nc.vector.tensor_tensor(out=r_[:], in0=r_[:], in1=sg[:], op=ALU.mult), reads=[r_, sg], writes=[r_])
            S.dma("sp", mo[0, :, c0:c0 + 512], r_[:], reads=[r_], sem=res_sem[bi])
            u = ub[bi]; un = ub[1 - bi]
            S.op("dve", lambda: nc.vector.tensor_tensor(out=u[:, 2:514], in0=sc[:], in1=sx[:], op=ALU.mult), reads=[sc, sx], writes=[u])
            S.op("dve", lambda: nc.vector.tensor_scalar(out=t1[:], in0=u[:, 2:514], scalar1=par_sb[:, 5:6], scalar2=None, op0=ALU.mult),
                 reads=[u, par_sb], writes=[t1])
            S.op("dve", lambda: nc.vector.scalar_tensor_tensor(out=t1[:], in0=u[:, 1:513], scalar=par_sb[:, 4:5], in1=t1[:], op0=ALU.mult, op1=ALU.add),
                 reads=[u, par_sb, t1], writes=[t1])
            S.op("dve", lambda: nc.vector.scalar_tensor_tensor(out=t1[:], in0=u[:, 0:512], scalar=par_sb[:, 3:4], in1=t1[:], op0=ALU.mult, op1=ALU.add),
                 reads=[u, par_sb, t1], writes=[t1])
            y_ = yb[bi]
            S.op("dve", lambda: nc.vector.tensor_tensor(out=y_[:], in0=t1[:], in1=sbb[:], op=ALU.mult), reads=[t1, sbb], writes=[y_])
            S.op("dve", lambda: nc.vector.tensor_copy(out=un[:, 0:2], in_=u[:, 512:514]), reads=[u], writes=[un])
            S.dma("sp", mo[1, :, c0:c0 + 512], y_[:], reads=[y_], sem=yb_sem[bi])
        S.finish(res + yb)
        print("mix0 instructions", S.ninst)
    return nc


KSEL = 256
NBIS = 24


def build_rglru(rg_tiles=17):
    TPR = rg_tiles * 512
    nc = bass.Bass("TRN2", target_bir_lowering=False)
    rxy = nc.dram_tensor("rxy", [2, 128, TPR], F32, kind="ExternalInput").ap()
    rgp = nc.dram_tensor("rgp", [128, 8], F32, kind="ExternalInput").ap()
    rgw = nc.dram_tensor("rgw", [2, 128, 128], F32, kind="ExternalInput").ap()
    hc_out = nc.dram_tensor("hc_out", [128, TPR], F32, kind="ExternalOutput").ap()
    with ExitStack() as st:
        S = Sched(nc, st)
        f32t = lambda n, w=512, p=128: S.sb(n, [p, w], F32)
        bft = lambda n, w=512, p=128: S.sb(n, [p, w], BF16)
        one_t = f32t("one_t", 1)
        S.op("dve", lambda: nc.vector.memset(one_t[:], 1.0), writes=[one_t])
        STp = [S.ps("ST0"), S.ps("ST1")]
        rgp_sb = f32t("rgp_sb", 8); rgw_f = S.sb("rgw_f", [128, 2, 128], F32); rgw_b = S.sb("rgw_b", [128, 2, 128], BF16)
        sem_r = [S.dsem("semr0"), S.dsem("semr1")]
        S.dma("sp", rgp_sb[:], rgp[:, :], writes=[rgp_sb], sem=sem_r[0])
        for i in range(2):
            S.dma("sp", rgw_f[:, i, :], rgw[i, :, :], writes=[rgw_f], sem=sem_r[1])
        S.op("dve", lambda: nc.vector.tensor_copy(out=rgw_b[:], in_=rgw_f[:]), reads=[rgw_f], writes=[rgw_b])
        c8 = f32t("c8", 1); c16 = f32t("c16", 1); tsm = f32t("tsm", 1)
        S.op("act", lambda: nc.scalar.activation(out=tsm[:], in_=rgp_sb[:, 7:8], func=AF.Exp, scale=-1.0), reads=[rgp_sb], writes=[tsm])
        S.op("act", lambda: nc.scalar.activation(out=tsm[:], in_=tsm[:], func=AF.Ln, bias=one_t[:, 0:1], scale=1.0), reads=[tsm, one_t], writes=[tsm])
        S.op("dve", lambda: nc.vector.tensor_scalar(out=c8[:], in0=tsm[:], scalar1=-8.0, scalar2=None, op0=ALU.mult), reads=[tsm], writes=[c8])
        S.op("dve", lambda: nc.vector.tensor_scalar(out=c16[:], in0=tsm[:], scalar1=-16.0, scalar2=None, op0=ALU.mult), reads=[tsm], writes=[c16])
        rxb = [f32t(f"rxb{i}", 515) for i in range(2)]
        ryb = [f32t(f"ryb{i}") for i in range(2)]
        sem_x = [S.dsem("semx0"), S.dsem("semx1")]; sem_y = [S.dsem("semy0"), S.dsem("semy1")]
        u_ = f32t("u_"); ub16 = bft("ub16"); r_ = f32t("r_"); ig = f32t("ig"); a_ = f32t("a_"); a2 = f32t("a2"); gx = f32t("gx")
        xin = f32t("xin"); hst = [f32t(f"hst{i}") for i in range(2)]; gl = f32t("gl"); gl2 = f32t("gl2")
        hco = [f32t(f"hco{i}") for i in range(2)]; sem_h = [S.dsem("semh0"), S.dsem("semh1")]
        S.op("dve", lambda: nc.vector.memset(rxb[0][:], 0.0), writes=[rxb[0]])
        S.op("dve", lambda: nc.vector.memset(hst[1][:], 0.0), writes=[hst[1]])
        for tt in range(rg_tiles):
            c0 = tt * 512; bi = tt % 2
            xb_ = rxb[bi]; xn = rxb[1 - bi]; yb_ = ryb[bi]
            S.dma("sp", xb_[:, 3:515], rxy[0, :, c0:c0 + 512], writes=[xb_], sem=sem_x[bi])
            S.dma("sp", yb_[:], rxy[1, :, c0:c0 + 512], writes=[yb_], sem=sem_y[bi])
            S.op("dve", lambda: nc.vector.tensor_scalar(out=u_[:], in0=xb_[:, 3:515], scalar1=rgp_sb[:, 3:4], scalar2=rgp_sb[:, 4:5],
                                                        op0=ALU.mult, op1=ALU.add), reads=[xb_, rgp_sb], writes=[u_])
            for jtap in range(3):
                S.op("dve", lambda: nc.vector.scalar_tensor_tensor(out=u_[:], in0=xb_[:, jtap:jtap + 512], scalar=rgp_sb[:, jtap:jtap + 1],
                                                                   in1=u_[:], op0=ALU.mult, op1=ALU.add), reads=[xb_, rgp_sb, u_], writes=[u_])
            S.op("dve", lambda: nc.vector.tensor_copy(out=xn[:, 0:3], in_=xb_[:, 512:515]), reads=[xb_], writes=[xn])
            S.op("dve", lambda: nc.vector.tensor_copy(out=ub16[:], in_=u_[:]), reads=[u_], writes=[ub16])
            S.mm(STp[0], STp[0][:], rgw_b, rgw_b[:, 0, :], ub16, ub16[:], start=True, stop=True)
            S.mm(STp[1], STp[1][:], rgw_b, rgw_b[:, 1, :], ub16, ub16[:], start=True, stop=True)
            S.op("act", lambda: nc.scalar.activation(out=r_[:], in_=STp[0][:], func=AF.Sigmoid, bias=rgp_sb[:, 5:6], scale=1.0),
                 reads=[STp[0], rgp_sb], writes=[r_])
            S.op("act", lambda: nc.scalar.activation(out=ig[:], in_=STp[1][:], func=AF.Sigmoid, bias=rgp_sb[:, 6:7], scale=1.0),
                 reads=[STp[1], rgp_sb], writes=[ig])
            S.op("act", lambda: nc.scalar.activation(out=a_[:], in_=r_[:], func=AF.Exp, scale=c8[:, 0:1]), reads=[r_, c8], writes=[a_])
            S.op("act", lambda: nc.scalar.activation(out=a2[:], in_=r_[:], func=AF.Exp, scale=c16[:, 0:1]), reads=[r_, c16], writes=[a2])
            S.op("dve", lambda: nc.vector.tensor_scalar(out=a2[:], in0=a2[:], scalar1=-1.0, scalar2=1.0, op0=ALU.mult, op1=ALU.add),
                 reads=[a2], writes=[a2])
            S.op("act", lambda: nc.scalar.activation(out=gx[:], in_=a2[:], func=AF.Sqrt), reads=[a2], writes=[gx])
            S.op("dve", lambda: nc.vector.tensor_tensor(out=xin[:], in0=ig[:], in1=u_[:], op=ALU.mult), reads=[ig, u_], writes=[xin])
            S.op("dve", lambda: nc.vector.tensor_tensor(out=xin[:], in0=xin[:], in1=gx[:], op=ALU.mult), reads=[xin, gx], writes=[xin])
            hprev = hst[1 - bi]; hcur = hst[bi]
            S.op("dve", lambda: nc.vector.tensor_tensor_scan(out=hcur[:], data0=a_[:], data1=xin[:], initial=hprev[:, 511:512],
                                                             op0=ALU.mult, op1=ALU.add), reads=[a_, xin, hprev], writes=[hcur])
            S.op("dve", lambda: nc.vector.tensor_tensor(out=gl[:], in0=yb_[:], in1=yb_[:], op=ALU.mult), reads=[yb_], writes=[gl])
            S.op("dve", lambda: nc.vector.tensor_scalar(out=gl[:], in0=gl[:], scalar1=0.044715, scalar2=1.0, op0=ALU.mult, op1=ALU.add),
                 reads=[gl], writes=[gl])
            S.op("dve", lambda: nc.vector.tensor_tensor(out=gl[:], in0=gl[:], in1=yb_[:], op=ALU.mult), reads=[gl, yb_], writes=[gl])
            S.op("act", lambda: nc.scalar.activation(out=gl2[:], in_=gl[:], func=AF.Sigmoid, scale=1.5957691216), reads=[gl], writes=[gl2])
            S.op("dve", lambda: nc.vector.tensor_tensor(out=gl2[:], in0=gl2[:], in1=yb_[:], op=ALU.mult), reads=[gl2, yb_], writes=[gl2])
            ho = hco[bi]
            S.op("dve", lambda: nc.vector.tensor_tensor(out=ho[:], in0=hcur[:], in1=gl2[:], op=ALU.mult), reads=[hcur, gl2], writes=[ho])
            S.dma("sp", hc_out[:, c0:c0 + 512], ho[:], reads=[ho], sem=sem_h[bi])

        S.finish(hco)
        print("rglru instructions", S.ninst)
    return nc


def build_attn(nblk=8):
    NKEY = 1024 * nblk
    NSB = NKEY // 128
    nc = bass.Bass("TRN2", target_bir_lowering=False)
    ckv_tok = nc.dram_tensor("ckv_tok", [NSB + 1, 128, 256], F32, kind="ExternalInput").ap()
    ikT = nc.dram_tensor("ikT", [64, NKEY], F32, kind="ExternalInput").ap()
    gkv_bc = nc.dram_tensor("gkv_bc", [128, 256], F32, kind="ExternalInput").ap()
    wukT = nc.dram_tensor("wukT", [128, 8, 256], F32, kind="ExternalInput").ap()
    wuv = nc.dram_tensor("wuv", [2, 128, 8, 128], F32, kind="ExternalInput").ap()
    qT = nc.dram_tensor("qT", [nblk, 128, 8, 128], F32, kind="ExternalInput").ap()
    iqT = nc.dram_tensor("iqT", [nblk, 64, 16, 128], F32, kind="ExternalInput").ap()
    iw_bc = nc.dram_tensor("iw_bc", [nblk, 64, 16, 128], F32, kind="ExternalInput").ap()
    iw_tok = nc.dram_tensor("iw_tok", [nblk, 128, 16], F32, kind="ExternalInput").ap()
    trel = nc.dram_tensor("trel", [128, nblk], F32, kind="ExternalInput").ap()
    cst = nc.dram_tensor("cst", [128, 128 + 1024], F32, kind="ExternalInput").ap()
    att_out = nc.dram_tensor("att_out", [nblk, 128, 8, 128], F32, kind="ExternalOutput").ap()

    with ExitStack() as st:
        S = Sched(nc, st)
        f32t = lambda n, w=512, p=128: S.sb(n, [p, w], F32)
        bft = lambda n, w=512, p=128: S.sb(n, [p, w], BF16)
        cst_sb = f32t("cst_sb", 1152)
        ident = bft("ident", 128); ones = bft("ones", 128)
        eps_t = f32t("eps_t", 1); one_t = f32t("one_t", 1)
        sem_c = S.dsem("semc")
        S.dma("sp", cst_sb[:], cst[:, :], writes=[cst_sb], sem=sem_c)
        S.op("dve", lambda: nc.vector.tensor_copy(out=ident[:], in_=cst_sb[:, 0:128]), reads=[cst_sb], writes=[ident])
        S.op("dve", lambda: nc.vector.memset(ones[:], 1.0), writes=[ones])
        S.op("dve", lambda: nc.vector.memset(eps_t[:], EPS), writes=[eps_t])
        S.op("dve", lambda: nc.vector.memset(one_t[:], 1.0), writes=[one_t])
        oacc = [S.ps(f"oacc{i}") for i in range(4)]
        den = S.ps("den")
        STp = [S.ps("ST0"), S.ps("ST1")]
        tpb = S.ps("tpb", (128, 1024), BF16)

        gkv = f32t("gkv", 256); sem_g = S.dsem("semg")
        S.dma("sp", gkv[:], gkv_bc[:, :], writes=[gkv], sem=sem_g)
        cT = [S.sb(f"cT{rc}", [128, NKEY + 128], BF16) for rc in range(2)]
        ctok = S.sb("ctok", [128, NSB + 1, 256], BF16)
        ikb = S.sb("ikb", [64, NKEY], BF16)
        sc = f32t("sc", NKEY)
        kst = [f32t(f"kst{i}", 256) for i in range(2)]; sem_k = [S.dsem("semk0"), S.dsem("semk1")]
        ksq = f32t("ksq", 256); kss = f32t("kss", 1); krs = f32t("krs", 1)
        S.op("dve", lambda: nc.vector.memset(kst[0][:], 0.0), writes=[kst[0]])
        for sb_ in range(NSB + 1):
            ks = kst[sb_ % 2]
            rows = 16 if sb_ == 0 else 128
            S.dma("sp", ks[0:rows, :], ckv_tok[sb_, 0:rows, :], writes=[ks], sem=sem_k[sb_ % 2])
            S.op("act", lambda: nc.scalar.activation(out=ksq[:], in_=ks[:], func=AF.Square, accum_out=kss[:, 0:1]), reads=[ks], writes=[ksq, kss])
            S.op("act", lambda: nc.scalar.activation(out=krs[:], in_=kss[:], func=AF.Ln, bias=eps_t[:, 0:1], scale=1.0 / 256), reads=[kss, eps_t], writes=[krs])
            S.op("act", lambda: nc.scalar.activation(out=krs[:], in_=krs[:], func=AF.Exp, scale=-0.5), reads=[krs], writes=[krs])
            S.op("dve", lambda: nc.vector.scalar_tensor_tensor(out=ctok[:, sb_, :], in0=ks[:], scalar=krs[:, 0:1], in1=gkv[:], op0=ALU.mult, op1=ALU.mult),
                 reads=[ks, krs, gkv], writes=[ctok])
            if sb_ == 0:
                S.op("dve", lambda: nc.vector.memset(kst[0][:], 0.0), reads=[ctok], writes=[kst[0]])
            for rc in range(2):
                S.op("pe", lambda: nc.tensor.transpose(out=tpb[:, rc * 128:(rc + 1) * 128], in_=ctok[:, sb_, rc * 128:(rc + 1) * 128], identity=ident[:]),
                     reads=[ctok, ident], writes=[tpb])
            for rc in range(2):
                S.op("act", lambda: nc.scalar.copy(out=cT[rc][:, sb_ * 128:(sb_ + 1) * 128], in_=tpb[:, rc * 128:(rc + 1) * 128]), reads=[tpb], writes=[cT[rc]])
        sem_i = S.dsem("semi0")
        for kc_ in range(NKEY // 1024):
            S.dma("sp", sc[0:64, 0:1024], ikT[:, kc_ * 1024:(kc_ + 1) * 1024], writes=[sc], sem=sem_i)
            S.op("dve", lambda: nc.vector.tensor_copy(out=ikb[:, kc_ * 1024:(kc_ + 1) * 1024], in_=sc[0:64, 0:1024]), reads=[sc], writes=[ikb])
        wukb = S.sb("wukb", [128, 8, 256], BF16); wuvb = S.sb("wuvb", [128, 2, 8, 128], BF16)
        sem_w = [S.dsem("semw0"), S.dsem("semw1")]
        S.dma("sp", sc[:, 0:2048], wukT.rearrange("p h r -> p (h r)"), writes=[sc], sem=sem_w[0])
        S.op("dve", lambda: nc.vector.tensor_copy(out=wukb[:].rearrange("p h r -> p (h r)"), in_=sc[:, 0:2048]), reads=[sc], writes=[wukb])
        for rc in range(2):
            S.dma("sp", sc[:, 0:1024], wuv[rc, :, :, :].rearrange("p h d -> p (h d)"), writes=[sc], sem=sem_w[1])
            S.op("dve", lambda: nc.vector.tensor_copy(out=wuvb[:, rc, :, :].rearrange("p h d -> p (h d)"), in_=sc[:, 0:1024]), reads=[sc], writes=[wuvb])
        cmax = f32t("cmax", 1)
        S.op("dve", lambda: nc.vector.tensor_reduce(out=cmax[:], in_=gkv[:], axis=AX.X, op=ALU.max, apply_absolute_value=True), reads=[gkv], writes=[cmax])
        S.op("dve", lambda: nc.vector.tensor_scalar(out=cmax[:], in0=cmax[:], scalar1=-16.0, scalar2=None, op0=ALU.mult), reads=[cmax], writes=[cmax])
        trel_sb = f32t("trel_sb", nblk); sem_t = S.dsem("semt")
        S.dma("sp", trel_sb[:], trel[:, :], writes=[trel_sb], sem=sem_t)

        qst = S.sb("qst", [128, 8, 128], F32); qb16 = S.sb("qb16", [128, 8, 128], BF16); sem_q = S.dsem("semq")
        iqst = S.sb("iqst", [64, 16, 128], F32); iwst = S.sb("iwst", [64, 16, 128], F32); sem_iq = S.dsem("semiq"); sem_iw = S.dsem("semiw")
        iqs = S.sb("iqs", [64, 16, 128], BF16)
        iwt = f32t("iwt", 16); sgn = f32t("sgn", 16); sem_it = S.dsem("semit")
        qlat = [S.sb(f"qlat{rc}", [128, 1024], BF16) for rc in range(2)]
        qsq = bft("qsq", 1024); negm = S.sb("negm", [1, 1024], BF16); nrm = S.sb("nrm", [1, 1024], F32)
        Rr = [f32t(f"Rr{i}") for i in range(2)]
        junk = bft("junk", 2048)
        lo = f32t("lo", 1); hi = f32t("hi", 1); mid = f32t("mid", 1); cnt = f32t("cnt", 1); prd = f32t("prd", 1); dd = f32t("dd", 1)
        pen = f32t("pen", 1024)
        maskb = bft("maskb"); maskT = S.sb("maskT", [128, 4, 128], BF16)
        PT = [bft(f"PT{i}") for i in range(2)]; PTm = [bft(f"PTm{i}") for i in range(2)]
        den_sb = f32t("den_sb", 8); rec = f32t("rec", 8)
        o_sb = S.sb("o_sb", [128, 8, 256], BF16); oT = [S.sb(f"oT{rc}", [128, 8, 128], BF16) for rc in range(2)]
        ao = [S.sb(f"ao{i}", [128, 4, 128], F32) for i in range(2)]; sem_ao = [S.dsem("semao0"), S.dsem("semao1")]
        onesrow = S.sb("onesrow", [1, 128], BF16)
        S.op("dve", lambda: nc.vector.memset(onesrow[:], 1.0), writes=[onesrow])
        IDX_SCALE = (64 ** -0.5) * (16 ** -0.5)
        rri = 0; pti = 0
        for j in range(nblk):
            nkt = 2 * (j + 1)
            nsb = 8 * (j + 1)
            S.dma("sp", qst[:], qT[j, :, :, :], writes=[qst], sem=sem_q)
            S.dma("sp", iqst[:], iqT[j, :, :, :], writes=[iqst], sem=sem_iq)
            S.dma("sp", iwst[:], iw_bc[j, :, :, :], writes=[iwst], sem=sem_iw)
            S.dma("sp", iwt[:], iw_tok[j, :, :], writes=[iwt], sem=sem_it)
            S.op("dve", lambda: nc.vector.tensor_copy(out=qb16[:], in_=qst[:]), reads=[qst], writes=[qb16])
            S.op("act", lambda: nc.scalar.activation(out=iwst[:], in_=iwst[:], func=AF.Abs), reads=[iwst], writes=[iwst])
            S.op("dve", lambda: nc.vector.scalar_tensor_tensor(out=iqs[:], in0=iqst[:], scalar=IDX_SCALE, in1=iwst[:], op0=ALU.mult, op1=ALU.mult),
                 reads=[iqst, iwst], writes=[iqs])
            S.op("act", lambda: nc.scalar.activation(out=sgn[:], in_=iwt[:], func=AF.Sign), reads=[iwt], writes=[sgn])
            for rc in range(2):
                for hg in range(2):
                    for hh in range(4):
                        h = hg * 4 + hh
                        S.mm(STp[hg], STp[hg][:, hh * 128:(hh + 1) * 128], wukb, wukb[:, h, rc * 128:(rc + 1) * 128], qb16, qb16[:, h, :], start=True, stop=True)
                    S.op("act", lambda: nc.scalar.activation(out=qlat[rc][:, hg * 512:(hg + 1) * 512], in_=STp[hg][:], func=AF.Identity, scale=128 ** -0.5),
                         reads=[STp[hg]], writes=[qlat[rc]])
            for hg in range(2):
                for rc in range(2):
                    S.op("dve", lambda: nc.vector.tensor_tensor(out=qsq[:, 0:512], in0=qlat[rc][:, hg * 512:(hg + 1) * 512], in1=qlat[rc][:, hg * 512:(hg + 1) * 512], op=ALU.mult),
                         reads=[qlat[rc]], writes=[qsq])
                    S.mm(STp[hg], STp[hg][:], ones, ones[:], qsq, qsq[:, 0:512], start=(rc == 0), stop=(rc == 1))
                S.op("act", lambda: nc.scalar.activation(out=nrm[:, hg * 512:(hg + 1) * 512], in_=STp[hg][0:1, :], func=AF.Sqrt), reads=[STp[hg]], writes=[nrm])
            S.op("dve", lambda: nc.vector.tensor_scalar(out=negm[:], in0=nrm[:], scalar1=cmax[0:1, 0:1], scalar2=None, op0=ALU.mult), reads=[nrm, cmax], writes=[negm])
            for kt in range(nkt):
                for h in range(16):
                    pb = STp[h % 2]
                    S.mm(pb, pb[:], iqs, iqs[:, h, :], ikb, ikb[:, kt * 512:(kt + 1) * 512], start=True, stop=True)
                    R = Rr[rri % 2]; rri += 1
                    S.op("act", lambda: nc.scalar.activation(out=R[:], in_=pb[:], func=AF.Relu), reads=[pb], writes=[R])
                    if h == 0:
                        S.op("dve", lambda: nc.vector.tensor_scalar(out=sc[:, kt * 512:(kt + 1) * 512], in0=R[:], scalar1=sgn[:, 0:1], scalar2=None, op0=ALU.mult),
                             reads=[R, sgn], writes=[sc])
                    else:
                        S.op("dve", lambda: nc.vector.scalar_tensor_tensor(out=sc[:, kt * 512:(kt + 1) * 512], in0=R[:], scalar=sgn[:, h:h + 1],
                                                                           in1=sc[:, kt * 512:(kt + 1) * 512], op0=ALU.mult, op1=ALU.add),
                             reads=[R, sgn, sc], writes=[sc])
            nk = 1024 * (j + 1)
            S.op("dve", lambda: nc.vector.tensor_reduce(out=hi[:], in_=sc[:, 0:nk], axis=AX.X, op=ALU.max, apply_absolute_value=True), reads=[sc], writes=[hi])
            S.op("dve", lambda: nc.vector.tensor_scalar(out=lo[:], in0=hi[:], scalar1=-1.0, scalar2=-1.0, op0=ALU.mult, op1=ALU.add), reads=[hi], writes=[lo])
            S.op("dve", lambda: nc.vector.tensor_scalar(out=hi[:], in0=hi[:], scalar1=1.0, scalar2=None, op0=ALU.add), reads=[hi], writes=[hi])
            S.op("dve", lambda: nc.vector.tensor_scalar(out=pen[:], in0=cst_sb[:, 128:1152], scalar1=trel_sb[:, j:j + 1], scalar2=-1e30, op0=ALU.is_gt, op1=ALU.mult),
                 reads=[cst_sb, trel_sb], writes=[pen])
            S.op("dve", lambda: nc.vector.tensor_tensor(out=sc[:, nk - 1024:nk], in0=sc[:, nk - 1024:nk], in1=pen[:], op=ALU.add), reads=[sc, pen], writes=[sc])
            for itn in range(NBIS):
                S.op("dve", lambda: nc.vector.tensor_tensor(out=mid[:], in0=lo[:], in1=hi[:], op=ALU.add), reads=[lo, hi], writes=[mid])
                S.op("dve", lambda: nc.vector.tensor_scalar(out=mid[:], in0=mid[:], scalar1=0.5, scalar2=None, op0=ALU.mult), reads=[mid], writes=[mid])
                first = True
                for c0 in range(0, nk, 2048):
                    w = min(2048, nk - c0)
                    if first:
                        S.op("dve", lambda: nc.vector.tensor_scalar(out=junk[:, 0:w], in0=sc[:, c0:c0 + w], scalar1=mid[:, 0:1], scalar2=None, op0=ALU.is_ge,
                                                                    op1=ALU.add, accum_out=cnt[:, 0:1]), reads=[sc, mid], writes=[junk, cnt])
                    else:
                        S.op("dve", lambda: nc.vector.tensor_scalar(out=junk[:, 0:w], in0=sc[:, c0:c0 + w], scalar1=mid[:, 0:1], scalar2=cnt[:, 0:1], op0=ALU.is_ge,
                                                                    op1=ALU.add, accum_out=cnt[:, 0:1]), reads=[sc, mid, cnt], writes=[junk, cnt])
                    first = False
                S.op("dve", lambda: nc.vector.tensor_scalar(out=prd[:], in0=cnt[:], scalar1=KSEL - 0.5, scalar2=None, op0=ALU.is_ge), reads=[cnt], writes=[prd])
                S.op("dve", lambda: nc.vector.tensor_tensor(out=dd[:], in0=mid[:], in1=lo[:], op=ALU.subtract), reads=[mid, lo], writes=[dd])
                S.op("dve", lambda: nc.vector.scalar_tensor_tensor(out=lo[:], in0=dd[:], scalar=prd[:, 0:1], in1=lo[:], op0=ALU.mult, op1=ALU.add),
                     reads=[dd, prd, lo], writes=[lo])
                S.op("dve", lambda: nc.vector.tensor_tensor(out=dd[:], in0=hi[:], in1=mid[:], op=ALU.subtract), reads=[hi, mid], writes=[dd])
                S.op("dve", lambda: nc.vector.scalar_tensor_tensor(out=hi[:], in0=dd[:], scalar=prd[:, 0:1], in1=mid[:], op0=ALU.mult, op1=ALU.add),
                     reads=[dd, prd, mid], writes=[hi])
            started = [False] * 4
            den_started = False
            for sbk in range(nsb + 1):
                rows = 16 if sbk == 0 else 128
                kb = sbk - 1
                if sbk >= 1 and kb % 4 == 0:
                    ktile = kb // 4
                    S.op("dve", lambda: nc.vector.tensor_scalar(out=maskb[:], in0=sc[:, ktile * 512:(ktile + 1) * 512], scalar1=lo[:, 0:1], scalar2=None, op0=ALU.is_ge),
                         reads=[sc, lo], writes=[maskb])
                    for q4 in range(4):
                        S.op("pe", lambda: nc.tensor.transpose(out=tpb[:, q4 * 128:(q4 + 1) * 128], in_=maskb[:, q4 * 128:(q4 + 1) * 128], identity=ident[:]),
                             reads=[maskb, ident], writes=[tpb])
                    S.op("act", lambda: nc.scalar.copy(out=maskT[:, :, :], in_=tpb[:, 0:512]), reads=[tpb], writes=[maskT])
                for hg in range(2):
                    stp = STp[hg]
                    for rc in range(2):
                        S.mm(stp, stp[0:rows, :], cT[rc], cT[rc][:, sbk * 128:sbk * 128 + rows], qlat[rc], qlat[rc][:, hg * 512:(hg + 1) * 512],
                             start=(rc == 0), stop=False)
                    S.mm(stp, stp[0:rows, :], onesrow, onesrow[0:1, 0:rows], negm, negm[0:1, hg * 512:(hg + 1) * 512], start=False, stop=True)
                    P = PT[pti % 2]; Pm = PTm[pti % 2]; pti += 1
                    S.op("act", lambda: nc.scalar.activation(out=P[0:rows, :], in_=stp[0:rows, :], func=AF.Exp), reads=[stp], writes=[P])
                    if sbk == 0:
                        Pm = P
                    else:
                        q4 = kb % 4
                        S.op("dve", lambda: nc.vector.tensor_tensor(out=Pm[:].rearrange("p (h t) -> p h t", h=4), in0=P[:].rearrange("p (h t) -> p h t", h=4),
                                                                     in1=maskT[:, q4:q4 + 1, :].to_broadcast([128, 4, 128]), op=ALU.mult),
                             reads=[P, maskT], writes=[Pm])
                    for hh in range(4):
                        h = hg * 4 + hh
                        bank = oacc[h // 2]
                        S.mm(bank, bank[:, (h % 2) * 256:(h % 2 + 1) * 256], Pm, Pm[0:rows, hh * 128:(hh + 1) * 128], ctok, ctok[0:rows, sbk, :],
                             start=(not started[h // 2]), stop=(sbk == nsb), skip_group_check=True)
                        started[h // 2] = True
                        S.mm(den, den[:, h:h + 1], Pm, Pm[0:rows, hh * 128:(hh + 1) * 128], ones, ones[0:rows, 0:1],
                             start=(not den_started), stop=(sbk == nsb), skip_group_check=True)
                        den_started = True
            S.op("dve", lambda: nc.vector.tensor_copy(out=den_sb[:], in_=den[:, 0:8]), reads=[den], writes=[den_sb])
            S.op("dve", lambda: nc.vector.reciprocal(out=rec[:], in_=den_sb[:]), reads=[den_sb], writes=[rec])
            for h in range(8):
                bank = oacc[h // 2]
                S.op("dve", lambda: nc.vector.tensor_scalar(out=o_sb[:, h, :], in0=bank[:, (h % 2) * 256:(h % 2 + 1) * 256], scalar1=rec[:, h:h + 1], scalar2=None, op0=ALU.mult),
                     reads=[bank, rec], writes=[o_sb])
            for rc in range(2):
                for h in range(8):
                    S.op("pe", lambda: nc.tensor.transpose(out=tpb[:, h * 128:(h + 1) * 128], in_=o_sb[:, h, rc * 128:(rc + 1) * 128], identity=ident[:]),
                         reads=[o_sb, ident], writes=[tpb])
                S.op("act", lambda: nc.scalar.copy(out=oT[rc][:, :, :], in_=tpb[:, :]), reads=[tpb], writes=[oT[rc]])
            for hg in range(2):
                stp = STp[hg]
                for hh in range(4):
                    h = hg * 4 + hh
                    for rc in range(2):
                        S.mm(stp, stp[:, hh * 128:(hh + 1) * 128], wuvb, wuvb[:, rc, h, :], oT[rc], oT[rc][:, h, :], start=(rc == 0), stop=(rc == 1))
                a = ao[hg]
                S.op("act", lambda: nc.scalar.copy(out=a[:, :, :], in_=stp[:]), reads=[stp], writes=[a])
                S.dma("sp", att_out[j, :, hg * 4:(hg + 1) * 4, :], a[:, :, :], reads=[a], sem=sem_ao[hg])
        S.finish(ao)
        print("attn instructions", S.ninst)
    return nc


def attn_consts():
    c = np.zeros((128, 1152), np.float32)
    c[:, 0:128] = np.eye(128, dtype=np.float32)
    c[:, 128:1152] = np.arange(1024, dtype=np.float32)[None, :]
    return c


def attn_inputs(core, nblk, q, ckv, iq, ik, iw, kvg, w_uk, w_uv):
    NKEY = 1024 * nblk
    NSB = NKEY // 128
    ckv_tok = np.zeros((NSB + 1, 128, 256), np.float32)
    ckv_tok[0, :NMETA] = ckv[:NMETA]
    ckv_tok[1:] = ckv[NMETA:NMETA + NKEY].reshape(NSB, 128, 256)
    d = {"ckv_tok": ckv_tok,
         "ikT": np.ascontiguousarray(ik[NMETA:NMETA + NKEY].T),
         "gkv_bc": np.ascontiguousarray(np.broadcast_to(kvg[None, :], (128, 256))).astype(np.float32),
         "wukT": np.ascontiguousarray(w_uk.transpose(2, 1, 0)),
         "wuv": np.ascontiguousarray(w_uv.reshape(2, 128, 8, 128)),
         "cst": attn_consts()}
    qT = np.zeros((nblk, 128, 8, 128), np.float32)
    iqT = np.zeros((nblk, 64, 16, 128), np.float32)
    iwb = np.zeros((nblk, 64, 16, 128), np.float32)
    iwt = np.zeros((nblk, 128, 16), np.float32)
    trel = np.zeros((128, nblk), np.float32)
    toks = []
    for j in range(nblk):
        qb = 8 * j + core
        tok = NMETA + 128 * qb + np.arange(128)
        toks.append(tok)
        qT[j] = q[tok].reshape(128, 8, 128).transpose(2, 1, 0)
        iqT[j] = iq[tok].reshape(128, 16, 64).transpose(2, 1, 0)
        iwb[j] = np.broadcast_to(iw[tok].T[None, :, :], (64, 16, 128))
        iwt[j] = iw[tok]
        trel[:, j] = (128 * qb + np.arange(128)) - 1024 * j
    d.update({"qT": qT, "iqT": iqT, "iw_bc": iwb, "iw_tok": iwt, "trel": trel})
    return d, toks


def attn_unpack(att_out, toks, att_full):
    for j, tok in enumerate(toks):
        att_full[tok] = att_out[j].transpose(2, 1, 0).reshape(128, 1024)


def rglru_inputs(core, ntiles, rx, ry, conv_w, conv_b, w_a, b_a, w_i, b_i, lam):
    TPR = ntiles * 512
    T = rx.shape[0]
    sl = slice(core * 128, (core + 1) * 128)
    rxy = np.zeros((2, 128, TPR), np.float32)
    n = min(T, TPR)
    rxy[0, :, :n] = rx[:n, sl].T
    rxy[1, :, :n] = ry[:n, sl].T
    rgp = np.zeros((128, 8), np.float32)
    for jt in range(4):
        rgp[:, jt] = conv_w[jt, sl]
    rgp[:, 4] = conv_b[sl]; rgp[:, 5] = b_a[sl]; rgp[:, 6] = b_i[sl]; rgp[:, 7] = lam[sl]
    rgw = np.ascontiguousarray(np.stack([w_a[core], w_i[core]], 0))
    return {"rxy": rxy, "rgp": rgp, "rgw": rgw}


def tok_idx_l0(c):
    r0 = NMETA + c * 1024
    return np.concatenate([np.arange(r0, r0 + 512), np.arange(0, NMETA), np.arange(r0 + 512, r0 + 1024)])


def run_inproj(h_tok, w_in, gain):
    M = w_in.shape[1]
    nmb = M // 128
    key = ("ip", nmb)
    if key not in _NC_CACHE:
        _NC_CACHE[key] = build_rowlocal(0, inproj_mb=nmb)
    nc = _NC_CACHE[key]
    gains = np.ascontiguousarray(np.stack([gvec(gain)] * 4, axis=1)).astype(np.float32)
    wt = tile_w(w_in)
    in_maps = []
    for c in range(NCORES):
        in_maps.append({"hT_in": fm(h_tok[tok_idx_l0(c)]), "gains": gains, "cd_t": wt})
    res = run_bass_kernel_spmd(nc, in_maps, core_ids=list(range(NCORES)))
    proj = np.zeros((h_tok.shape[0], M), np.float32)
    for c in range(NCORES):
        p = np.asarray(res.results[c]["proj_out"])
        proj[tok_idx_l0(c)] = p.transpose(2, 0, 1).reshape(p.shape[2], nmb * 128)
    return proj


def run_mix0(proj0, lb_logits, out_norm_g, sconv_w):
    if "m0" not in _NC_CACHE:
        _NC_CACHE["m0"] = build_mix0(17)
    nc = _NC_CACHE["m0"]
    T = proj0.shape[0]
    P = np.zeros((TP0, 7168), np.float32)
    P[48:48 + T] = proj0
    cst = mix0_consts()
    in_maps = []
    for c in range(NCORES):
        pj = np.ascontiguousarray(np.stack([P[:, i * 1024 + c * 128: i * 1024 + (c + 1) * 128].T for i in range(7)], 0))
        par = np.zeros((128, 8), np.float32)
        par[:, 0] = lb_logits[0, c * 128:(c + 1) * 128]
        par[:, 1] = lb_logits[1, c * 128:(c + 1) * 128]
        par[:, 2] = out_norm_g
        for jt in range(3):
            par[:, 3 + jt] = sconv_w[jt, c * 128:(c + 1) * 128]
        in_maps.append({"pj": pj, "par": par, "cst": cst})
    res = run_bass_kernel_spmd(nc, in_maps, core_ids=list(range(NCORES)))
    mix = np.zeros((T, 2048), np.float32)
    for c in range(NCORES):
        mo = np.asarray(res.results[c]["mo"])
        mix[:, c * 128:(c + 1) * 128] = mo[0].T[48:48 + T]
        mix[:, 1024 + c * 128:1024 + (c + 1) * 128] = mo[1].T[48:48 + T]
    return mix


def run_mix1(proj1, inp):
    T = proj1.shape[0]
    sizes = [1024, 1024, 1024, 256, 1024, 64, 16]
    rx, ry, q, ckv, iq, ik, iw = np.split(proj1, np.cumsum(sizes)[:-1], axis=-1)
    if "rg" not in _NC_CACHE:
        _NC_CACHE["rg"] = build_rglru(17)
    nc = _NC_CACHE["rg"]
    in_maps = [rglru_inputs(c, 17, rx, ry, inp["rg_conv_w"][0], inp["rg_conv_b"][0], inp["rg_w_a"][0], inp["rg_b_a"][0],
                            inp["rg_w_i"][0], inp["rg_b_i"][0], inp["rg_lambda"][0]) for c in range(NCORES)]
    res = run_bass_kernel_spmd(nc, in_maps, core_ids=list(range(NCORES)))
    mix = np.zeros((T, 2048), np.float32)
    for c in range(NCORES):
        mix[:, c * 128:(c + 1) * 128] = np.asarray(res.results[c]["hc_out"]).T[:T]
    if "at" not in _NC_CACHE:
        _NC_CACHE["at"] = build_attn(8)
    nc = _NC_CACHE["at"]
    in_maps = []
    toks_all = []
    for c in range(NCORES):
        d, toks = attn_inputs(c, 8, q, ckv, iq, ik, iw, inp["mla_kv_norm"][0], inp["mla_w_uk"][0], inp["mla_w_uv"][0])
        in_maps.append(d)
        toks_all.append(toks)
    res = run_bass_kernel_spmd(nc, in_maps, core_ids=list(range(NCORES)))
    att = np.zeros((T, 1024), np.float32)
    for c in range(NCORES):
        attn_unpack(np.asarray(res.results[c]["att_out"]), toks_all[c], att)
    mix[:, 1024:] = att
    return mix


def kernel(**inp):
    inp = {k: np.asarray(v) for k, v in inp.items()}
    x = inp["x"]
    h0 = np.concatenate([inp["meta_tokens"].astype(np.float32), x[0]], axis=0)
    proj0 = run_inproj(h0, inp["ab_w_in"][0], inp["ln_mix_pre"][0])
    mix0 = run_mix0(proj0, inp["hgrn_lb_logits"], inp["hgrn_out_norm"][0], inp["sconv_w"][0])
    h1, proj1 = run_rowlocal(0, h0, mix0, inp["ab_w_out"][0], inp["ffn_w1"][0], inp["ffn_w3"][0], inp["ffn_w2"][0],
                             [inp["ln_mix_post"][0], inp["ln_ffn_pre"][0], inp["ln_ffn_post"][0], inp["ln_mix_pre"][1]],
                             inp["cd_w_in"][0])
    mix1 = run_mix1(proj1, inp)
    h2, _ = run_rowlocal(1, h1[NMETA:], mix1[NMETA:], inp["cd_w_out"][0], inp["ffn_w1"][1], inp["ffn_w3"][1], inp["ffn_w2"][1],
                         [inp["ln_mix_post"][1], inp["ln_ffn_pre"][1], inp["ln_ffn_post"][1], inp["ln_mix_pre"][1]])
    return h2[None].astype(np.float32)
```

```python
import numpy as np
from contextlib import ExitStack
import concourse.bass as bass
import concourse.mybir as mybir
from concourse.bass_utils import run_bass_kernel_spmd

F32 = mybir.dt.float32
BF16 = mybir.dt.bfloat16
AF = mybir.ActivationFunctionType
ALU = mybir.AluOpType
AX = mybir.AxisListType

D = 2048
DFF = 5632
NMETA = 16
SEQ = 8192
EPS = 1e-6
NCORES = 8


class T:
    def __init__(self, ap, name=""):
        self.ap = ap
        self.name = name
        self.w = None
        self.r = []

    def __getitem__(self, k):
        return self.ap[k]


class View(T):
    pass


class Sched:
    def __init__(self, nc, stack):
        self.nc = nc
        self.stack = stack
        self.eng = {"pe": nc.tensor, "act": nc.scalar, "dve": nc.vector, "pool": nc.gpsimd, "sp": nc.sync}
        self.sem = {k: stack.enter_context(nc.semaphore("s_" + k)) for k in self.eng}
        self.cnt = {k: 0 for k in self.eng}
        self.seen = {k: {} for k in self.eng}
        self.nsem = 0
        self.ninst = 0

    def sb(self, name, shape, dt):
        t = self.stack.enter_context(self.nc.sbuf_tensor(name, shape, dt))
        return T(t, name)

    def ps(self, name, shape=(128, 512), dt=F32):
        t = self.stack.enter_context(self.nc.psum_tensor(name, list(shape), dt))
        return T(t, name)

    def dsem(self, name):
        self.nsem += 1
        return [self.stack.enter_context(self.nc.semaphore(name)), 0]

    def _wait(self, e, tok):
        if tok is None:
            return
        sem, val, owner = tok
        if owner == e and e == "pe":
            return
        seen = self.seen[e]
        key = id(sem)
        if seen.get(key, 0) >= val:
            return
        self.eng[e].wait_ge(sem, val)
        seen[key] = val

    def deps(self, e, reads, writes):
        toks = []
        for t in reads:
            toks.append(t.w)
        for t in writes:
            toks.append(t.w)
            toks.extend(t.r)
        best = {}
        for tok in toks:
            if tok is None:
                continue
            k = id(tok[0])
            if k not in best or best[k][1] < tok[1]:
                best[k] = tok
        for tok in best.values():
            self._wait(e, tok)

    def done(self, tok, reads, writes):
        for t in reads:
            t.r.append(tok)
            if len(t.r) > 64:
                t.r = t.r[-48:]
        for t in writes:
            t.w = tok
            t.r = []

    def op(self, e, fn, reads=(), writes=()):
        self.deps(e, reads, writes)
        ins = fn()
        self.cnt[e] += 1
        self.ninst += 1
        ins.then_inc(self.sem[e], 1)
        tok = (self.sem[e], self.cnt[e], e)
        self.done(tok, reads, writes)

    def dma(self, q, out, in_, reads=(), writes=(), sem=None, **kw):
        self.deps(q, reads, writes)
        ins = self.eng[q].dma_start(out=out, in_=in_, **kw)
        sem[1] += 16
        self.ninst += 1
        ins.then_inc(sem[0], 16)
        tok = (sem[0], sem[1], "dma")
        self.done(tok, reads, writes)

    def finish(self, tiles):
        self.deps("sp", [], tiles)

    def mm(self, out_t, out_ap, lhsT_t, lhsT_ap, rhs_t, rhs_ap, start, stop, **kw):
        nc = self.nc
        self.op("pe", lambda: nc.tensor.matmul(out_ap, lhsT=lhsT_ap, rhs=rhs_ap, start=start, stop=stop, **kw),
                reads=[lhsT_t, rhs_t], writes=[out_t])


def trim_reads(t):
    pass


CD_M = 4432
CD_MB = 35


DBG = {'ss': True, 'norm': True, 'castpool': True}


def build_rowlocal(layer, stop=99, inproj_mb=None):
    with_cd = (layer == 0)
    CDMB = inproj_mb if inproj_mb else CD_MB
    if layer == 0:
        NT = 1040
        halves = [(0, 528, [(0, 512), (512, 16)]), (528, 512, [(0, 512)])]
    else:
        NT = 1024
        halves = [(0, 512, [(0, 512)]), (512, 512, [(0, 512)])]
    HN = 528
    nc = bass.Bass("TRN2", target_bir_lowering=False)
    hT_in = nc.dram_tensor("hT_in", [128, 16, NT], F32, kind="ExternalInput").ap()
    if not inproj_mb:
        mixT_in = nc.dram_tensor("mixT_in", [128, 16, NT], F32, kind="ExternalInput").ap()
        w_out_t = nc.dram_tensor("w_out_t", [16, 128, 16, 128], F32, kind="ExternalInput").ap()
        w1_t = nc.dram_tensor("w1_t", [44, 128, 16, 128], F32, kind="ExternalInput").ap()
        w3_t = nc.dram_tensor("w3_t", [44, 128, 16, 128], F32, kind="ExternalInput").ap()
        w2_t = nc.dram_tensor("w2_t", [16, 128, 44, 128], F32, kind="ExternalInput").ap()
        hT_out = nc.dram_tensor("hT_out", [128, 16, NT], F32, kind="ExternalOutput").ap()
    NG = 4
    gains = nc.dram_tensor("gains", [128, NG, 16], F32, kind="ExternalInput").ap()
    if with_cd:
        cd_t = nc.dram_tensor("cd_t", [CDMB, 128, 16, 128], F32, kind="ExternalInput").ap()
        proj_out = nc.dram_tensor("proj_out", [CDMB, 128, NT], F32, kind="ExternalOutput").ap()

    with ExitStack() as st:
        S = Sched(nc, st)
        hT = S.sb("hT", [128, 16, HN], F32)
        xb = S.sb("xb", [128, 16, HN], BF16)
        y = S.sb("y", [128, 16, HN], F32)
        m = S.sb("m", [128, 44, HN], BF16)
        g_sb = S.sb("g_sb", [128, NG, 16], F32)
        ones = S.sb("ones", [128, 128], BF16)
        rstd = S.sb("rstd", [128, HN], F32)
        lnt = S.sb("lnt", [128, HN], F32)
        NSTG = 3
        stg = [S.sb(f"stg{i}", [128, 16, 128], F32) for i in range(NSTG)]
        NWB = 4
        wbf = [S.sb(f"wbf{i}", [128, 16, 128], BF16) for i in range(NWB)]
        stg_sem = [S.dsem(f"stgsem{i}") for i in range(NSTG)]
        sq = [S.sb(f"sq{i}", [128, HN], BF16) for i in range(2)]
        tmp = [S.sb(f"tmp{i}", [128, HN], F32) for i in range(2)]
        ostg = [S.sb(f"ostg{i}", [128, HN], F32) for i in range(2)]
        ostg_sem = [S.dsem(f"ostgsem{i}") for i in range(2)]
        accb = [[S.ps(f"acc{w}{b}") for b in range(2)] for w in range(2)]
        small = [S.ps("small0"), S.ps("small1")]
        smallv = [[small[w] for b in range(2)] for w in range(2)]
        ssb = S.ps("ssb")
        sss = S.ps("sss")
        io_sem = [S.dsem("io0"), S.dsem("io1"), S.dsem("io2"), S.dsem("io3")]

        S.dma("sp", g_sb[:], gains[:, :, :], writes=[g_sb], sem=io_sem[2])
        S.op("dve", lambda: nc.vector.memset(ones[:], 1.0), writes=[ones])

        state = {"fill": 0, "wb": 0, "blk": 0, "sq": 0, "tmp": 0, "ostg": 0, "cast": 0}

        def load_w(src_ap, kn):
            i = state["fill"] % NSTG
            state["fill"] += 1
            S.dma("sp", stg[i][:, 0:kn, :], src_ap, writes=[stg[i]], sem=stg_sem[i])
            j = state["wb"] % NWB
            state["wb"] += 1
            c = state["cast"] % 3
            state["cast"] += 1
            if c != 2:
                S.op("act", lambda: nc.scalar.copy(out=wbf[j][:, 0:kn, :], in_=stg[i][:, 0:kn, :]),
                     reads=[stg[i]], writes=[wbf[j]])
            else:
                S.op("dve", lambda: nc.vector.tensor_copy(out=wbf[j][:, 0:kn, :], in_=stg[i][:, 0:kn, :]),
                     reads=[stg[i]], writes=[wbf[j]])
            return wbf[j]

        def acc_aps(w, b, ntiles):
            res = []
            for (n0, nsz) in ntiles:
                if nsz == 512:
                    res.append((accb[w][b], accb[w][b][:, 0:512]))
                else:
                    off = (w * 2 + b) * 16
                    res.append((smallv[w][b], smallv[w][b][:, off:off + nsz]))
            return res

        def linear(x_t, KC, wsrcs, nmb, ntiles, consume, fills):
            seq = [(mb, wi, fi) for mb in range(nmb) for wi in range(len(wsrcs)) for fi in range(len(fills))]
            loaded = {}
            nxt = [0]
            LOOK = 2

            def ensure(upto):
                while nxt[0] <= min(upto, len(seq) - 1):
                    mb_, wi_, fi_ = seq[nxt[0]]
                    k0_, kn_ = fills[fi_]
                    loaded[nxt[0]] = load_w(wsrcs[wi_][mb_, :, k0_:k0_ + kn_, :], kn_)
                    nxt[0] += 1
            b = 0
            accs = []
            aps = None
            for i, (mb, wi, fi) in enumerate(seq):
                ensure(i + LOOK)
                if wi == 0 and fi == 0:
                    b = state["blk"] % 2
                    state["blk"] += 1
                    accs = []
                if fi == 0:
                    aps = acc_aps(wi, b, ntiles)
                wt = loaded.pop(i)
                k0, kn = fills[fi]
                for kk in range(kn):
                    kc = k0 + kk
                    for ti, (n0, nsz) in enumerate(ntiles):
                        at, aap = aps[ti]
                        S.mm(at, aap, wt, wt[:, kk, :], x_t, x_t[:, kc, n0:n0 + nsz],
                             start=(kc == 0), stop=(kc == KC - 1))
                if fi == len(fills) - 1:
                    accs.append(aps)
                    if wi == len(wsrcs) - 1:
                        consume(mb, accs)

        def rstd_from(ss_list, ntiles):
            for (sst, ssap), (n0, nsz) in zip(ss_list, ntiles):
                S.op("act", lambda: nc.scalar.activation(out=lnt[:, n0:n0 + nsz], in_=ssap, func=AF.Ln,
                                                         bias=eps_t[:, 0:1], scale=1.0 / D),
                     reads=[sst, eps_t], writes=[lnt])
                S.op("act", lambda: nc.scalar.activation(out=rstd[:, n0:n0 + nsz], in_=lnt[:, n0:n0 + nsz],
                                                         func=AF.Exp, scale=-0.5),
                     reads=[lnt], writes=[rstd])

        eps_t = S.sb("eps_t", [128, 1], F32)
        S.op("dve", lambda: nc.vector.memset(eps_t[:], EPS), writes=[eps_t])

        def ss_aps(ntiles):
            res = []
            for (n0, nsz) in ntiles:
                if nsz == 512:
                    res.append((ssb, ssb[:, 0:512]))
                else:
                    res.append((sss, sss[:, 0:nsz]))
            return res

        def ss_accum(src_t, src_ap_fn, kc, ntiles, from_psum_aps=None):
            ssl = ss_aps(ntiles)
            for ti, (n0, nsz) in enumerate(ntiles):
                i = state["sq"] % 2
                state["sq"] += 1
                if from_psum_aps is not None:
                    st_, sap = from_psum_aps[ti]
                else:
                    st_, sap = src_t, src_ap_fn(n0, nsz)
                S.op("act", lambda: nc.scalar.activation(out=sq[i][:, 0:nsz], in_=sap, func=AF.Square),
                     reads=[st_], writes=[sq[i]])
                S.mm(ssl[ti][0], ssl[ti][1], ones, ones[:], sq[i], sq[i][:, 0:nsz], start=(kc == 0), stop=(kc == 15))

        def post_norm_residual(gidx, ntiles, hw):
            for kc in range(16):
                i = state["tmp"] % 2
                state["tmp"] += 1
                S.op("dve", lambda: nc.vector.scalar_tensor_tensor(
                    out=tmp[i][:, 0:hw], in0=y[:, kc, 0:hw], scalar=g_sb[:, gidx, kc:kc + 1], in1=rstd[:, 0:hw],
                    op0=ALU.mult, op1=ALU.mult), reads=[y, g_sb, rstd], writes=[tmp[i]])
                S.op("dve", lambda: nc.vector.tensor_tensor(out=hT[:, kc, 0:hw], in0=hT[:, kc, 0:hw],
                                                             in1=tmp[i][:, 0:hw], op=ALU.add),
                     reads=[tmp[i], hT], writes=[hT])

        def pre_norm(gidx, ntiles, hw):
            for kc in range(16):
                ss_accum(hT, lambda n0, nsz: hT[:, kc, n0:n0 + nsz], kc, ntiles)
            rstd_from(ss_aps(ntiles), ntiles)
            for kc in range(16):
                S.op("dve", lambda: nc.vector.scalar_tensor_tensor(
                    out=xb[:, kc, 0:hw], in0=hT[:, kc, 0:hw], scalar=g_sb[:, gidx, kc:kc + 1], in1=rstd[:, 0:hw],
                    op0=ALU.mult, op1=ALU.mult), reads=[hT, g_sb, rstd], writes=[xb])

        def consume_y(ntiles):
            def f(mb, accs):
                aps = accs[0]
                for ti, (n0, nsz) in enumerate(ntiles):
                    at, aap = aps[ti]
                    S.op("dve", lambda: nc.vector.tensor_copy(out=y[:, mb, n0:n0 + nsz], in_=aap),
                         reads=[at], writes=[y])
                if DBG['ss']:
                    ss_accum(y, lambda n0, nsz: y[:, mb, n0:n0 + nsz], mb, ntiles)
            return f

        for (h0, hw, ntiles) in halves:
            if inproj_mb:
                for k4 in range(0, 16, 2):
                    S.dma("sp", hT[:, k4:k4 + 2, 0:hw], hT_in[:, k4:k4 + 2, h0:h0 + hw], writes=[hT], sem=io_sem[0])
                pre_norm(0, ntiles, hw)

                def consume_ip(mb, accs):
                    i = state["ostg"] % 2
                    state["ostg"] += 1
                    for ti, (n0, nsz) in enumerate(ntiles):
                        at, aap = accs[0][ti]
                        S.op("dve", lambda: nc.vector.tensor_copy(out=ostg[i][:, n0:n0 + nsz], in_=aap),
                             reads=[at], writes=[ostg[i]])
                    S.dma("sp", proj_out[mb, :, h0:h0 + hw], ostg[i][:, 0:hw], reads=[ostg[i]], sem=ostg_sem[i])
                linear(xb, 16, [cd_t], CDMB, ntiles, consume_ip, [(0, 16)])
                continue
            for k4 in range(0, 16, 2):
                S.dma("sp", hT[:, k4:k4 + 2, 0:hw], hT_in[:, k4:k4 + 2, h0:h0 + hw], writes=[hT], sem=io_sem[0])
                S.dma("sp", y[:, k4:k4 + 2, 0:hw], mixT_in[:, k4:k4 + 2, h0:h0 + hw], writes=[y], sem=io_sem[1])
            for kc in range(16):
                eng = "dve"
                if eng == "dve":
                    S.op("dve", lambda: nc.vector.tensor_copy(out=xb[:, kc, 0:hw], in_=y[:, kc, 0:hw]),
                         reads=[y], writes=[xb])
                else:
                    S.op("pool", lambda: nc.gpsimd.tensor_copy(out=xb[:, kc, 0:hw], in_=y[:, kc, 0:hw]),
                         reads=[y], writes=[xb])
            if stop >= 1:
                linear(xb, 16, [w_out_t], 16, ntiles, consume_y(ntiles), [(0, 16)])
                if DBG['norm']:
                    rstd_from(ss_aps(ntiles), ntiles)
                    post_norm_residual(0, ntiles, hw)
                else:
                    for kc in range(16):
                        S.op("dve", lambda: nc.vector.tensor_copy(out=hT[:, kc, 0:hw], in_=y[:, kc, 0:hw]), reads=[y], writes=[hT])
            if stop == 0:
                for kc in range(16):
                    S.op("dve", lambda: nc.vector.tensor_copy(out=hT[:, kc, 0:hw], in_=y[:, kc, 0:hw]), reads=[y], writes=[hT])
            if stop <= 1:
                for k4 in range(0, 16, 2):
                    S.dma("sp", hT_out[:, k4:k4 + 2, h0:h0 + hw], hT[:, k4:k4 + 2, 0:hw], reads=[hT], sem=io_sem[3])
                continue
            pre_norm(1, ntiles, hw)

            def consume_ab(mb, accs):
                for ti, (n0, nsz) in enumerate(ntiles):
                    i = state["tmp"] % 2
                    state["tmp"] += 1
                    at, aap = accs[0][ti]
                    bt, bap = accs[1][ti]
                    S.op("act", lambda: nc.scalar.activation(out=tmp[i][:, 0:nsz], in_=aap, func=AF.Silu),
                         reads=[at], writes=[tmp[i]])
                    S.op("dve", lambda: nc.vector.tensor_tensor(out=m[:, mb, n0:n0 + nsz], in0=tmp[i][:, 0:nsz],
                                                                 in1=bap, op=ALU.mult),
                         reads=[tmp[i], bt], writes=[m])
            linear(xb, 16, [w1_t, w3_t], 44, ntiles, consume_ab, [(0, 16)])
            linear(m, 44, [w2_t], 16, ntiles, consume_y(ntiles), [(0, 16), (16, 16), (32, 12)])
            rstd_from(ss_aps(ntiles), ntiles)
            post_norm_residual(2, ntiles, hw)
            for k4 in range(0, 16, 2):
                S.dma("sp", hT_out[:, k4:k4 + 2, h0:h0 + hw], hT[:, k4:k4 + 2, 0:hw], reads=[hT], sem=io_sem[3])
            if with_cd:
                pre_norm(3, ntiles, hw)

                def consume_cd(mb, accs):
                    i = state["ostg"] % 2
                    state["ostg"] += 1
                    for ti, (n0, nsz) in enumerate(ntiles):
                        at, aap = accs[0][ti]
                        S.op("dve", lambda: nc.vector.tensor_copy(out=ostg[i][:, n0:n0 + nsz], in_=aap),
                             reads=[at], writes=[ostg[i]])
                    S.dma("sp", proj_out[mb, :, h0:h0 + hw], ostg[i][:, 0:hw], reads=[ostg[i]], sem=ostg_sem[i])
                linear(xb, 16, [cd_t], CDMB, ntiles, consume_cd, [(0, 16)])
        S.finish([hT, ostg[0], ostg[1]] if (with_cd or inproj_mb) else [hT])
        print("rowlocal layer", layer, "instructions", S.ninst)
    return nc


def tile_w(w, mw=128):
    K, M = w.shape
    Mp = -(-M // mw) * mw
    if Mp != M:
        w = np.concatenate([w, np.zeros((K, Mp - M), w.dtype)], axis=1)
    return np.ascontiguousarray(w.reshape(K // 128, 128, Mp // mw, mw).transpose(2, 1, 0, 3))


def fm(a):
    Tn, Fn = a.shape
    return np.ascontiguousarray(a.reshape(Tn, Fn // 128, 128).transpose(2, 1, 0))


def unfm(a):
    p, kc, Tn = a.shape
    return np.ascontiguousarray(a.transpose(2, 1, 0).reshape(Tn, kc * 128))


def gvec(g):
    return np.ascontiguousarray(g.reshape(16, 128).T)


_NC_CACHE = {}


def run_rowlocal(layer, h_tok, mix_tok, w_out, w1, w3, w2, gain_list, cd_w=None):
    if ("rl", layer) not in _NC_CACHE:
        _NC_CACHE[("rl", layer)] = build_rowlocal(layer)
    nc = _NC_CACHE[("rl", layer)]
    gains = np.ascontiguousarray(np.stack([gvec(g) for g in gain_list], axis=1)).astype(np.float32)
    common = {"w_out_t": tile_w(w_out), "w1_t": tile_w(w1), "w3_t": tile_w(w3), "w2_t": tile_w(w2), "gains": gains}
    if layer == 0:
        common["cd_t"] = tile_w(cd_w)
    in_maps = []
    idxs = []
    for c in range(NCORES):
        if layer == 0:
            r0 = NMETA + c * 1024
            idx = np.concatenate([np.arange(r0, r0 + 512), np.arange(0, NMETA), np.arange(r0 + 512, r0 + 1024)])
        else:
            idx = np.arange(c * 1024, (c + 1) * 1024)
        idxs.append(idx)
        d = dict(common)
        d["hT_in"] = fm(h_tok[idx])
        d["mixT_in"] = fm(mix_tok[idx])
        in_maps.append(d)
    res = run_bass_kernel_spmd(nc, in_maps, core_ids=list(range(NCORES)))
    h_out = np.zeros_like(h_tok)
    proj = np.zeros((h_tok.shape[0], CD_M), np.float32) if layer == 0 else None
    for c in range(NCORES):
        r = res.results[c]
        h_out[idxs[c]] = unfm(np.asarray(r["hT_out"]))
        if layer == 0:
            p = np.asarray(r["proj_out"])
            pt = p.transpose(2, 0, 1).reshape(p.shape[2], CD_MB * 128)[:, :CD_M]
            proj[idxs[c]] = pt
    return h_out, proj


TP0 = 8704
NCONST = 128 + 512 + 512


def mix0_consts():
    c = np.zeros((128, NCONST), np.float32)
    c[:, 0:128] = np.eye(128, dtype=np.float32)
    tri = (np.arange(64)[None, :] >= np.arange(64)[:, None]).astype(np.float32)
    c[0:64, 128:640] = np.tile(tri, (1, 8))
    r = np.ones(512, np.float32)
    r[0::64] = 0.0
    c[:, 640:1152] = r[None, :]
    return c


def build_mix0(ntile=17):
    TP = ntile * 512
    nc = bass.Bass("TRN2", target_bir_lowering=False)
    pj = nc.dram_tensor("pj", [7, 128, TP], F32, kind="ExternalInput").ap()
    par = nc.dram_tensor("par", [128, 8], F32, kind="ExternalInput").ap()
    cst = nc.dram_tensor("cst", [128, NCONST], F32, kind="ExternalInput").ap()
    mo = nc.dram_tensor("mo", [2, 128, TP], F32, kind="ExternalOutput").ap()
    with ExitStack() as st:
        S = Sched(nc, st)
        f32t = lambda n, w=512: S.sb(n, [128, w], F32)
        bft = lambda n, w=512: S.sb(n, [128, w], BF16)
        par_sb = f32t("par_sb", 8)
        cst_sb = f32t("cst_sb", NCONST)
        ident = bft("ident", 128)
        ones = bft("ones", 128)
        lb = f32t("lb", 1); oml = f32t("oml", 1); eps_t = f32t("eps_t", 1)
        inp = [[f32t(f"in{b}_{i}") for i in range(7)] for b in range(2)]
        in_sem = [[S.dsem(f"insem{b}_{i}") for i in range(7)] for b in range(2)]
        sig = f32t("sig"); f_ = f32t("f_"); logf = f32t("logf"); b_ = f32t("b_"); k_ = f32t("k_")
        eb = f32t("eb"); enb = f32t("enb"); e2 = f32t("e2")
        Qt = bft("Qt"); Kt = bft("Kt"); Kp = bft("Kp"); Vb = bft("Vb")
        Vtok = S.sb("Vtok", [64, 8, 128], BF16); Ktok = S.sb("Ktok", [64, 8, 128], BF16)
        attm = S.sb("attm", [64, 512], BF16)
        Sf = f32t("Sf", 128)
        NSB = 4
        Sb = [bft(f"Sb{i}", 128) for i in range(NSB)]
        o_sb = f32t("o_sb"); osq = bft("osq"); lnt = f32t("lnt"); rstd = f32t("rstd"); sg = f32t("sg")
        res = [f32t(f"res{i}") for i in range(2)]; res_sem = [S.dsem(f"ressem{i}") for i in range(2)]
        ub = [f32t(f"ub{i}", 514) for i in range(2)]
        t1 = f32t("t1")
        yb = [f32t(f"yb{i}") for i in range(2)]; yb_sem = [S.dsem(f"ybsem{i}") for i in range(2)]
        tpv = S.ps("tpv", (128, 1024), BF16); tpk = S.ps("tpk", (128, 1024), BF16)
        att = S.ps("att"); o_ps = S.ps("o_ps"); U = [S.ps("U0"), S.ps("U1")]; sso = S.ps("sso")
        csem = S.dsem("csem")
        csem2 = S.dsem("csem2")
        S.dma("sp", par_sb[:], par[:, :], writes=[par_sb], sem=csem)
        S.dma("sp", cst_sb[:], cst[:, :], writes=[cst_sb], sem=csem2)
        tri = View(cst_sb.ap, "tri"); rmask = View(cst_sb.ap, "rmask")
        S.op("dve", lambda: nc.vector.tensor_copy(out=ident[:], in_=cst_sb[:, 0:128]), reads=[cst_sb], writes=[ident])
        S.op("dve", lambda: nc.vector.memset(ones[:], 1.0), writes=[ones])
        S.op("dve", lambda: nc.vector.memset(eps_t[:], EPS), writes=[eps_t])
        S.op("dve", lambda: nc.vector.memset(Sf[:], 0.0), writes=[Sf])
        S.op("dve", lambda: nc.vector.memset(Sb[0][:], 0.0), writes=[Sb[0]])
        S.op("dve", lambda: nc.vector.memset(ub[0][:], 0.0), writes=[ub[0]])
        S.op("dve", lambda: nc.vector.tensor_tensor(out=oml[:], in0=par_sb[:, 0:1], in1=par_sb[:, 1:2], op=ALU.subtract),
             reads=[par_sb], writes=[oml])
        S.op("act", lambda: nc.scalar.activation(out=lb[:], in_=oml[:], func=AF.Sigmoid), reads=[oml], writes=[lb])
        S.op("dve", lambda: nc.vector.tensor_scalar(out=oml[:], in0=lb[:], scalar1=-1.0, scalar2=1.0, op0=ALU.mult, op1=ALU.add),
             reads=[lb], writes=[oml])
        sbi = 0
        for tt in range(ntile):
            c0 = tt * 512
            bi = tt % 2
            I = inp[bi]
            for i in range(7):
                S.dma("sp", I[i][:], pj[i, :, c0:c0 + 512], writes=[I[i]], sem=in_sem[bi][i])
            q, fl, v, g, sx, sbb, sc = I
            S.op("act", lambda: nc.scalar.activation(out=sig[:], in_=fl[:], func=AF.Sigmoid), reads=[fl], writes=[sig])
            S.op("dve", lambda: nc.vector.tensor_scalar(out=f_[:], in0=sig[:], scalar1=oml[:, 0:1], scalar2=lb[:, 0:1],
                                                        op0=ALU.mult, op1=ALU.add), reads=[sig, oml, lb], writes=[f_])
            S.op("act", lambda: nc.scalar.activation(out=logf[:], in_=f_[:], func=AF.Ln), reads=[f_], writes=[logf])
            S.op("dve", lambda: nc.vector.tensor_tensor_scan(out=b_[:], data0=cst_sb[:, 640:1152], data1=logf[:], initial=0.0,
                                                             op0=ALU.mult, op1=ALU.add), reads=[cst_sb, logf], writes=[b_])
            S.op("dve", lambda: nc.vector.tensor_scalar(out=k_[:], in0=f_[:], scalar1=-1.0, scalar2=1.0, op0=ALU.mult, op1=ALU.add),
                 reads=[f_], writes=[k_])
            S.op("act", lambda: nc.scalar.activation(out=eb[:], in_=b_[:], func=AF.Exp), reads=[b_], writes=[eb])
            S.op("act", lambda: nc.scalar.activation(out=enb[:], in_=b_[:], func=AF.Exp, scale=-1.0), reads=[b_], writes=[enb])
            for j in range(8):
                S.op("act", lambda: nc.scalar.activation(out=e2[:, j * 64:(j + 1) * 64], in_=b_[:, j * 64:(j + 1) * 64], func=AF.Exp,
                                                         scale=-1.0, bias=b_[:, j * 64 + 63:j * 64 + 64]), reads=[b_], writes=[e2])
            S.op("dve", lambda: nc.vector.tensor_tensor(out=Qt[:], in0=q[:], in1=eb[:], op=ALU.mult), reads=[q, eb], writes=[Qt])
            S.op("dve", lambda: nc.vector.tensor_tensor(out=Kt[:], in0=k_[:], in1=enb[:], op=ALU.mult), reads=[k_, enb], writes=[Kt])
            S.op("dve", lambda: nc.vector.tensor_tensor(out=Kp[:], in0=k_[:], in1=e2[:], op=ALU.mult), reads=[k_, e2], writes=[Kp])
            S.op("dve", lambda: nc.vector.tensor_copy(out=Vb[:], in_=v[:]), reads=[v], writes=[Vb])
            for j in range(8):
                S.op("pe", lambda: nc.tensor.transpose(out=tpv[0:64, j * 128:(j + 1) * 128], in_=Vb[:, j * 64:(j + 1) * 64], identity=ident[:]),
                     reads=[Vb, ident], writes=[tpv])
            for j in range(8):
                S.op("pe", lambda: nc.tensor.transpose(out=tpk[0:64, j * 128:(j + 1) * 128], in_=Kp[:, j * 64:(j + 1) * 64], identity=ident[:]),
                     reads=[Kp, ident], writes=[tpk])
            S.op("act", lambda: nc.scalar.copy(out=Vtok[:, :, :], in_=tpv[0:64, :]), reads=[tpv], writes=[Vtok])
            S.op("act", lambda: nc.scalar.copy(out=Ktok[:, :, :], in_=tpk[0:64, :]), reads=[tpk], writes=[Ktok])
            for j in range(8):
                S.mm(att, att[0:64, j * 64:(j + 1) * 64], Kt, Kt[:, j * 64:(j + 1) * 64], Qt, Qt[:, j * 64:(j + 1) * 64], start=True, stop=True)
            S.op("dve", lambda: nc.vector.tensor_tensor(out=attm[:], in0=att[0:64, :], in1=cst_sb[0:64, 128:640], op=ALU.mult),
                 reads=[att, cst_sb], writes=[attm])
            for j in range(8):
                Uj = U[j // 4]
                S.mm(Uj, Uj[:, (j % 4) * 128:(j % 4 + 1) * 128], Ktok, Ktok[:, j, :], Vtok, Vtok[:, j, :], start=True, stop=True)
            for j in range(8):
                cur = Sb[sbi % NSB]
                nxt = Sb[(sbi + 1) % NSB]
                sbi += 1
                S.mm(o_ps, o_ps[:, j * 64:(j + 1) * 64], Vtok, Vtok[:, j, :], attm, attm[:, j * 64:(j + 1) * 64], start=True, stop=False)
                S.mm(o_ps, o_ps[:, j * 64:(j + 1) * 64], cur, cur[:], Qt, Qt[:, j * 64:(j + 1) * 64], start=False, stop=True)
                Uj = U[j // 4]
                uap = Uj[:, (j % 4) * 128:(j % 4 + 1) * 128]
                dcol = eb[:, j * 64 + 63:j * 64 + 64]
                S.op("dve", lambda: nc.vector.scalar_tensor_tensor(out=nxt[:], in0=Sf[:], scalar=dcol, in1=uap, op0=ALU.mult, op1=ALU.add),
                     reads=[Sf, eb, Uj], writes=[nxt])
                S.op("dve", lambda: nc.vector.scalar_tensor_tensor(out=Sf[:], in0=Sf[:], scalar=dcol, in1=uap, op0=ALU.mult, op1=ALU.add),
                     reads=[Sf, eb, Uj], writes=[Sf])
            S.op("act", lambda: nc.scalar.copy(out=o_sb[:], in_=o_ps[:]), reads=[o_ps], writes=[o_sb])
            S.op("act", lambda: nc.scalar.activation(out=osq[:], in_=o_sb[:], func=AF.Square), reads=[o_sb], writes=[osq])
            S.mm(sso, sso[:], ones, ones[:], osq, osq[:], start=True, stop=True)
            S.op("act", lambda: nc.scalar.activation(out=lnt[:], in_=sso[:], func=AF.Ln, bias=eps_t[:, 0:1], scale=1.0 / 128),
                 reads=[sso, eps_t], writes=[lnt])
            S.op("act", lambda: nc.scalar.activation(out=rstd[:], in_=lnt[:], func=AF.Exp, scale=-0.5), reads=[lnt], writes=[rstd])
            S.op("act", lambda: nc.scalar.activation(out=sg[:], in_=g[:], func=AF.Silu), reads=[g], writes=[sg])
            r_ = res[bi]
            S.op("dve", lambda: nc.vector.scalar_tensor_tensor(out=r_[:], in0=o_sb[:], scalar=par_sb[:, 2:3], in1=rstd[:], op0=ALU.mult, op1=ALU.mult),
                 reads=[o_sb, par_sb, rstd], writes=[r_])
            S.op("dve", lambda: nc.vector.tensor_tensor(out=r_[:], in0=r_[:], in1=sg[:], op=ALU.mult), reads=[r_, sg], writes=[r_])
            S.dma("sp", mo[0, :, c0:c0 + 512], r_[:], reads=[r_], sem=res_sem[bi])
            u = ub[bi]; un = ub[1 - bi]
            S.op("dve", lambda: nc.vector.tensor_tensor(out=u[:, 2:514], in0=sc[:], in1=sx[:], op=ALU.mult), reads=[sc, sx], writes=[u])
            S.op("dve", lambda: nc.vector.tensor_scalar(out=t1[:], in0=u[:, 2:514], scalar1=par_sb[:, 5:6], scalar2=None, op0=ALU.mult),
                 reads=[u, par_sb], writes=[t1])
            S.op("dve", lambda: nc.vector.scalar_tensor_tensor(out=t1[:], in0=u[:, 1:513], scalar=par_sb[:, 4:5], in1=t1[:], op0=ALU.mult, op1=ALU.add),
                 reads=[u, par_sb, t1], writes=[t1])
            S.op("dve", lambda: nc.vector.scalar_tensor_tensor(out=t1[:], in0=u[:, 0:512], scalar=par_sb[:, 3:4], in1=t1[:], op0=ALU.mult, op1=ALU.add),
                 reads=[u, par_sb, t1], writes=[t1])
            y_ = yb[bi]
            S.op("dve", lambda: nc.vector.tensor_tensor(out=y_[:], in0=t1[:], in1=sbb[:], op=ALU.mult), reads=[t1, sbb], writes=[y_])
            S.op("dve", lambda: nc.vector.tensor_copy(out=un[:, 0:2], in_=u[:, 512:514]), reads=[u], writes=[un])
            S.dma("sp", mo[1, :, c0:c0 + 512], y_[:], reads=[y_], sem=yb_sem[bi])
        S.finish(res + yb)
        print("mix0 instructions", S.ninst)
    return nc


KSEL = 256
NBIS = 16


def build_rglru(rg_tiles=17):
    TPR = rg_tiles * 512
    nc = bass.Bass("TRN2", target_bir_lowering=False)
    rxy = nc.dram_tensor("rxy", [2, 128, TPR], F32, kind="ExternalInput").ap()
    rgp = nc.dram_tensor("rgp", [128, 8], F32, kind="ExternalInput").ap()
    rgw = nc.dram_tensor("rgw", [2, 128, 128], F32, kind="ExternalInput").ap()
    hc_out = nc.dram_tensor("hc_out", [128, TPR], F32, kind="ExternalOutput").ap()
    with ExitStack() as st:
        S = Sched(nc, st)
        f32t = lambda n, w=512, p=128: S.sb(n, [p, w], F32)
        bft = lambda n, w=512, p=128: S.sb(n, [p, w], BF16)
        one_t = f32t("one_t", 1)
        S.op("dve", lambda: nc.vector.memset(one_t[:], 1.0), writes=[one_t])
        STp = [S.ps("ST0"), S.ps("ST1")]
        rgp_sb = f32t("rgp_sb", 8); rgw_f = S.sb("rgw_f", [128, 2, 128], F32); rgw_b = S.sb("rgw_b", [128, 2, 128], BF16)
        sem_r = [S.dsem("semr0"), S.dsem("semr1")]
        S.dma("sp", rgp_sb[:], rgp[:, :], writes=[rgp_sb], sem=sem_r[0])
        for i in range(2):
            S.dma("sp", rgw_f[:, i, :], rgw[i, :, :], writes=[rgw_f], sem=sem_r[1])
        S.op("dve", lambda: nc.vector.tensor_copy(out=rgw_b[:], in_=rgw_f[:]), reads=[rgw_f], writes=[rgw_b])
        c8 = f32t("c8", 1); c16 = f32t("c16", 1); tsm = f32t("tsm", 1)
        S.op("act", lambda: nc.scalar.activation(out=tsm[:], in_=rgp_sb[:, 7:8], func=AF.Exp, scale=-1.0), reads=[rgp_sb], writes=[tsm])
        S.op("act", lambda: nc.scalar.activation(out=tsm[:], in_=tsm[:], func=AF.Ln, bias=one_t[:, 0:1], scale=1.0), reads=[tsm, one_t], writes=[tsm])
        S.op("dve", lambda: nc.vector.tensor_scalar(out=c8[:], in0=tsm[:], scalar1=-8.0, scalar2=None, op0=ALU.mult), reads=[tsm], writes=[c8])
        S.op("dve", lambda: nc.vector.tensor_scalar(out=c16[:], in0=tsm[:], scalar1=-16.0, scalar2=None, op0=ALU.mult), reads=[tsm], writes=[c16])
        rxb = [f32t(f"rxb{i}", 515) for i in range(2)]
        ryb = [f32t(f"ryb{i}") for i in range(2)]
        sem_x = [S.dsem("semx0"), S.dsem("semx1")]; sem_y = [S.dsem("semy0"), S.dsem("semy1")]
        u_ = f32t("u_"); ub16 = bft("ub16"); r_ = f32t("r_"); ig = f32t("ig"); a_ = f32t("a_"); a2 = f32t("a2"); gx = f32t("gx")
        xin = f32t("xin"); hst = [f32t(f"hst{i}") for i in range(2)]; gl = f32t("gl"); gl2 = f32t("gl2")
        hco = [f32t(f"hco{i}") for i in range(2)]; sem_h = [S.dsem("semh0"), S.dsem("semh1")]
        S.op("dve", lambda: nc.vector.memset(rxb[0][:], 0.0), writes=[rxb[0]])
        S.op("dve", lambda: nc.vector.memset(hst[1][:], 0.0), writes=[hst[1]])
        for tt in range(rg_tiles):
            c0 = tt * 512; bi = tt % 2
            xb_ = rxb[bi]; xn = rxb[1 - bi]; yb_ = ryb[bi]
            S.dma("sp", xb_[:, 3:515], rxy[0, :, c0:c0 + 512], writes=[xb_], sem=sem_x[bi])
            S.dma("sp", yb_[:], rxy[1, :, c0:c0 + 512], writes=[yb_], sem=sem_y[bi])
            S.op("dve", lambda: nc.vector.tensor_scalar(out=u_[:], in0=xb_[:, 3:515], scalar1=rgp_sb[:, 3:4], scalar2=rgp_sb[:, 4:5],
                                                        op0=ALU.mult, op1=ALU.add), reads=[xb_, rgp_sb], writes=[u_])
            for jtap in range(3):
                S.op("dve", lambda: nc.vector.scalar_tensor_tensor(out=u_[:], in0=xb_[:, jtap:jtap + 512], scalar=rgp_sb[:, jtap:jtap + 1],
                                                                   in1=u_[:], op0=ALU.mult, op1=ALU.add), reads=[xb_, rgp_sb, u_], writes=[u_])
            S.op("dve", lambda: nc.vector.tensor_copy(out=xn[:, 0:3], in_=xb_[:, 512:515]), reads=[xb_], writes=[xn])
            S.op("dve", lambda: nc.vector.tensor_copy(out=ub16[:], in_=u_[:]), reads=[u_], writes=[ub16])
            S.mm(STp[0], STp[0][:], rgw_b, rgw_b[:, 0, :], ub16, ub16[:], start=True, stop=True)
            S.mm(STp[1], STp[1][:], rgw_b, rgw_b[:, 1, :], ub16, ub16[:], start=True, stop=True)
            S.op("act", lambda: nc.scalar.activation(out=r_[:], in_=STp[0][:], func=AF.Sigmoid, bias=rgp_sb[:, 5:6], scale=1.0),
                 reads=[STp[0], rgp_sb], writes=[r_])
            S.op("act", lambda: nc.scalar.activation(out=ig[:], in_=STp[1][:], func=AF.Sigmoid, bias=rgp_sb[:, 6:7], scale=1.0),
                 reads=[STp[1], rgp_sb], writes=[ig])
            S.op("act", lambda: nc.scalar.activation(out=a_[:], in_=r_[:], func=AF.Exp, scale=c8[:, 0:1]), reads=[r_, c8], writes=[a_])
            S.op("act", lambda: nc.scalar.activation(out=a2[:], in_=r_[:], func=AF.Exp, scale=c16[:, 0:1]), reads=[r_, c16], writes=[a2])
            S.op("dve", lambda: nc.vector.tensor_scalar(out=a2[:], in0=a2[:], scalar1=-1.0, scalar2=1.0, op0=ALU.mult, op1=ALU.add),
                 reads=[a2], writes=[a2])
            S.op("act", lambda: nc.scalar.activation(out=gx[:], in_=a2[:], func=AF.Sqrt), reads=[a2], writes=[gx])
            S.op("dve", lambda: nc.vector.tensor_tensor(out=xin[:], in0=ig[:], in1=u_[:], op=ALU.mult), reads=[ig, u_], writes=[xin])
            S.op("dve", lambda: nc.vector.tensor_tensor(out=xin[:], in0=xin[:], in1=gx[:], op=ALU.mult), reads=[xin, gx], writes=[xin])
            hprev = hst[1 - bi]; hcur = hst[bi]
            S.op("dve", lambda: nc.vector.tensor_tensor_scan(out=hcur[:], data0=a_[:], data1=xin[:], initial=hprev[:, 511:512],
                                                             op0=ALU.mult, op1=ALU.add), reads=[a_, xin, hprev], writes=[hcur])
            S.op("dve", lambda: nc.vector.tensor_tensor(out=gl[:], in0=yb_[:], in1=yb_[:], op=ALU.mult), reads=[yb_], writes=[gl])
            S.op("dve", lambda: nc.vector.tensor_scalar(out=gl[:], in0=gl[:], scalar1=0.044715, scalar2=1.0, op0=ALU.mult, op1=ALU.add),
                 reads=[gl], writes=[gl])
            S.op("dve", lambda: nc.vector.tensor_tensor(out=gl[:], in0=gl[:], in1=yb_[:], op=ALU.mult), reads=[gl, yb_], writes=[gl])
            S.op("act", lambda: nc.scalar.activation(out=gl2[:], in_=gl[:], func=AF.Sigmoid, scale=1.5957691216), reads=[gl], writes=[gl2])
            S.op("dve", lambda: nc.vector.tensor_tensor(out=gl2[:], in0=gl2[:], in1=yb_[:], op=ALU.mult), reads=[gl2, yb_], writes=[gl2])
            ho = hco[bi]
            S.op("dve", lambda: nc.vector.tensor_tensor(out=ho[:], in0=hcur[:], in1=gl2[:], op=ALU.mult), reads=[hcur, gl2], writes=[ho])
            S.dma("sp", hc_out[:, c0:c0 + 512], ho[:], reads=[ho], sem=sem_h[bi])

        S.finish(hco)
        print("rglru instructions", S.ninst)
    return nc


def build_attn(nblk=8):
    NKEY = 1024 * nblk
    NSB = NKEY // 128
    nc = bass.Bass("TRN2", target_bir_lowering=False)
    ckv_tok = nc.dram_tensor("ckv_tok", [NSB + 1, 128, 256], F32, kind="ExternalInput").ap()
    ikT = nc.dram_tensor("ikT", [64, NKEY], F32, kind="ExternalInput").ap()
    gkv_bc = nc.dram_tensor("gkv_bc", [128, 256], F32, kind="ExternalInput").ap()
    wukT = nc.dram_tensor("wukT", [128, 8, 256], F32, kind="ExternalInput").ap()
    wuv = nc.dram_tensor("wuv", [2, 128, 8, 128], F32, kind="ExternalInput").ap()
    qT = nc.dram_tensor("qT", [nblk, 128, 8, 128], F32, kind="ExternalInput").ap()
    iqT = nc.dram_tensor("iqT", [nblk, 64, 16, 128], F32, kind="ExternalInput").ap()
    iw_bc = nc.dram_tensor("iw_bc", [nblk, 64, 16, 128], F32, kind="ExternalInput").ap()
    iw_tok = nc.dram_tensor("iw_tok", [nblk, 128, 16], F32, kind="ExternalInput").ap()
    trel = nc.dram_tensor("trel", [128, nblk], F32, kind="ExternalInput").ap()
    cst = nc.dram_tensor("cst", [128, 128 + 1024], F32, kind="ExternalInput").ap()
    att_out = nc.dram_tensor("att_out", [nblk, 128, 8, 128], F32, kind="ExternalOutput").ap()

    with ExitStack() as st:
        S = Sched(nc, st)
        f32t = lambda n, w=512, p=128: S.sb(n, [p, w], F32)
        bft = lambda n, w=512, p=128: S.sb(n, [p, w], BF16)
        cst_sb = f32t("cst_sb", 1152)
        ident = bft("ident", 128); ones = bft("ones", 128)
        eps_t = f32t("eps_t", 1); one_t = f32t("one_t", 1)
        sem_c = S.dsem("semc")
        S.dma("sp", cst_sb[:], cst[:, :], writes=[cst_sb], sem=sem_c)
        S.op("dve", lambda: nc.vector.tensor_copy(out=ident[:], in_=cst_sb[:, 0:128]), reads=[cst_sb], writes=[ident])
        S.op("dve", lambda: nc.vector.memset(ones[:], 1.0), writes=[ones])
        S.op("dve", lambda: nc.vector.memset(eps_t[:], EPS), writes=[eps_t])
        S.op("dve", lambda: nc.vector.memset(one_t[:], 1.0), writes=[one_t])
        oacc = [S.ps(f"oacc{i}") for i in range(4)]
        den = S.ps("den")
        STp = [S.ps("ST0"), S.ps("ST1")]
        tpb = S.ps("tpb", (128, 1024), BF16)

        gkv = f32t("gkv", 256); sem_g = S.dsem("semg")
        S.dma("sp", gkv[:], gkv_bc[:, :], writes=[gkv], sem=sem_g)
        cT = [S.sb(f"cT{rc}", [128, NKEY + 128], BF16) for rc in range(2)]
        ctok = S.sb("ctok", [128, NSB + 1, 256], BF16)
        ikb = S.sb("ikb", [64, NKEY], BF16)
        sc = f32t("sc", NKEY)
        kst = [f32t(f"kst{i}", 256) for i in range(2)]; sem_k = [S.dsem("semk0"), S.dsem("semk1")]
        ksq = f32t("ksq", 256); kss = f32t("kss", 1); krs = f32t("krs", 1)
        S.op("dve", lambda: nc.vector.memset(kst[0][:], 0.0), writes=[kst[0]])
        for sb_ in range(NSB + 1):
            ks = kst[sb_ % 2]
            rows = 16 if sb_ == 0 else 128
            S.dma("sp", ks[0:rows, :], ckv_tok[sb_, 0:rows, :], writes=[ks], sem=sem_k[sb_ % 2])
            S.op("act", lambda: nc.scalar.activation(out=ksq[:], in_=ks[:], func=AF.Square, accum_out=kss[:, 0:1]), reads=[ks], writes=[ksq, kss])
            S.op("act", lambda: nc.scalar.activation(out=krs[:], in_=kss[:], func=AF.Ln, bias=eps_t[:, 0:1], scale=1.0 / 256), reads=[kss, eps_t], writes=[krs])
            S.op("act", lambda: nc.scalar.activation(out=krs[:], in_=krs[:], func=AF.Exp, scale=-0.5), reads=[krs], writes=[krs])
            S.op("dve", lambda: nc.vector.scalar_tensor_tensor(out=ctok[:, sb_, :], in0=ks[:], scalar=krs[:, 0:1], in1=gkv[:], op0=ALU.mult, op1=ALU.mult),
                 reads=[ks, krs, gkv], writes=[ctok])
            if sb_ == 0:
                S.op("dve", lambda: nc.vector.memset(kst[0][:], 0.0), reads=[ctok], writes=[kst[0]])
            for rc in range(2):
                S.op("pe", lambda: nc.tensor.transpose(out=tpb[:, rc * 128:(rc + 1) * 128], in_=ctok[:, sb_, rc * 128:(rc + 1) * 128], identity=ident[:]),
                     reads=[ctok, ident], writes=[tpb])
            for rc in range(2):
                S.op("act", lambda: nc.scalar.copy(out=cT[rc][:, sb_ * 128:(sb_ + 1) * 128], in_=tpb[:, rc * 128:(rc + 1) * 128]), reads=[tpb], writes=[cT[rc]])
        sem_i = S.dsem("semi0")
        for kc_ in range(NKEY // 1024):
            S.dma("sp", sc[0:64, 0:1024], ikT[:, kc_ * 1024:(kc_ + 1) * 1024], writes=[sc], sem=sem_i)
            S.op("dve", lambda: nc.vector.tensor_copy(out=ikb[:, kc_ * 1024:(kc_ + 1) * 1024], in_=sc[0:64, 0:1024]), reads=[sc], writes=[ikb])
        wukb = S.sb("wukb", [128, 8, 256], BF16); wuvb = S.sb("wuvb", [128, 2, 8, 128], BF16)
        sem_w = [S.dsem("semw0"), S.dsem("semw1")]
        S.dma("sp", sc[:, 0:2048], wukT.rearrange("p h r -> p (h r)"), writes=[sc], sem=sem_w[0])
        S.op("dve", lambda: nc.vector.tensor_copy(out=wukb[:].rearrange("p h r -> p (h r)"), in_=sc[:, 0:2048]), reads=[sc], writes=[wukb])
        for rc in range(2):
            S.dma("sp", sc[:, 0:1024], wuv[rc, :, :, :].rearrange("p h d -> p (h d)"), writes=[sc], sem=sem_w[1])
            S.op("dve", lambda: nc.vector.tensor_copy(out=wuvb[:, rc, :, :].rearrange("p h d -> p (h d)"), in_=sc[:, 0:1024]), reads=[sc], writes=[wuvb])
        cmax = f32t("cmax", 1)
        S.op("dve", lambda: nc.vector.tensor_reduce(out=cmax[:], in_=gkv[:], axis=AX.X, op=ALU.max, apply_absolute_value=True), reads=[gkv], writes=[cmax])
        S.op("dve", lambda: nc.vector.tensor_scalar(out=cmax[:], in0=cmax[:], scalar1=-16.0, scalar2=None, op0=ALU.mult), reads=[cmax], writes=[cmax])
        trel_sb = f32t("trel_sb", nblk); sem_t = S.dsem("semt")
        S.dma("sp", trel_sb[:], trel[:, :], writes=[trel_sb], sem=sem_t)

        qst = S.sb("qst", [128, 8, 128], F32); qb16 = S.sb("qb16", [128, 8, 128], BF16); sem_q = S.dsem("semq")
        iqst = S.sb("iqst", [64, 16, 128], F32); iwst = S.sb("iwst", [64, 16, 128], F32); sem_iq = S.dsem("semiq"); sem_iw = S.dsem("semiw")
        iqs = S.sb("iqs", [64, 16, 128], BF16)
        iwt = f32t("iwt", 16); sgn = f32t("sgn", 16); sem_it = S.dsem("semit")
        qlat = [S.sb(f"qlat{rc}", [128, 1024], BF16) for rc in range(2)]
        qsq = bft("qsq", 1024); negm = S.sb("negm", [1, 1024], BF16); nrm = S.sb("nrm", [1, 1024], F32)
        Rb = [bft(f"Rb{i}") for i in range(3)]
        dg = S.sb("dg", [128, 16, 128], BF16)
        junk = bft("junk", 2048)
        lo = f32t("lo", 1); hi = f32t("hi", 1); mid = f32t("mid", 1); cnt = f32t("cnt", 1); prd = f32t("prd", 1); dd = f32t("dd", 1)
        pen = f32t("pen", 1024)
        maskb = bft("maskb"); maskT = S.sb("maskT", [128, 4, 128], BF16)
        PT = [bft(f"PT{i}") for i in range(2)]; PTm = [bft(f"PTm{i}") for i in range(2)]
        den_sb = f32t("den_sb", 8); rec = f32t("rec", 8)
        o_sb = S.sb("o_sb", [128, 8, 256], BF16); oT = [S.sb(f"oT{rc}", [128, 8, 128], BF16) for rc in range(2)]
        ao = [S.sb(f"ao{i}", [128, 4, 128], F32) for i in range(2)]; sem_ao = [S.dsem("semao0"), S.dsem("semao1")]
        onesrow = S.sb("onesrow", [1, 128], BF16)
        S.op("dve", lambda: nc.vector.memset(onesrow[:], 1.0), writes=[onesrow])
        IDX_SCALE = (64 ** -0.5) * (16 ** -0.5)
        rri = 0; pti = 0
        for j in range(nblk):
            nkt = 2 * (j + 1)
            nsb = 8 * (j + 1)
            S.dma("sp", qst[:], qT[j, :, :, :], writes=[qst], sem=sem_q)
            S.dma("sp", iqst[:], iqT[j, :, :, :], writes=[iqst], sem=sem_iq)
            S.dma("sp", iwst[:], iw_bc[j, :, :, :], writes=[iwst], sem=sem_iw)
            S.dma("sp", iwt[:], iw_tok[j, :, :], writes=[iwt], sem=sem_it)
            S.op("dve", lambda: nc.vector.tensor_copy(out=qb16[:], in_=qst[:]), reads=[qst], writes=[qb16])
            S.op("act", lambda: nc.scalar.activation(out=iwst[:], in_=iwst[:], func=AF.Abs), reads=[iwst], writes=[iwst])
            S.op("dve", lambda: nc.vector.scalar_tensor_tensor(out=iqs[:], in0=iqst[:], scalar=IDX_SCALE, in1=iwst[:], op0=ALU.mult, op1=ALU.mult),
                 reads=[iqst, iwst], writes=[iqs])
            S.op("act", lambda: nc.scalar.activation(out=sgn[:], in_=iwt[:], func=AF.Sign), reads=[iwt], writes=[sgn])
            for rc in range(2):
                for hg in range(2):
                    for hh in range(4):
                        h = hg * 4 + hh
                        S.mm(STp[hg], STp[hg][:, hh * 128:(hh + 1) * 128], wukb, wukb[:, h, rc * 128:(rc + 1) * 128], qb16, qb16[:, h, :], start=True, stop=True)
                    S.op("act", lambda: nc.scalar.activation(out=qlat[rc][:, hg * 512:(hg + 1) * 512], in_=STp[hg][:], func=AF.Identity, scale=128 ** -0.5),
                         reads=[STp[hg]], writes=[qlat[rc]])
            for hg in range(2):
                for rc in range(2):
                    S.op("dve", lambda: nc.vector.tensor_tensor(out=qsq[:, 0:512], in0=qlat[rc][:, hg * 512:(hg + 1) * 512], in1=qlat[rc][:, hg * 512:(hg + 1) * 512], op=ALU.mult),
                         reads=[qlat[rc]], writes=[qsq])
                    S.mm(STp[hg], STp[hg][:], ones, ones[:], qsq, qsq[:, 0:512], start=(rc == 0), stop=(rc == 1))
                S.op("act", lambda: nc.scalar.activation(out=nrm[:, hg * 512:(hg + 1) * 512], in_=STp[hg][0:1, :], func=AF.Sqrt), reads=[STp[hg]], writes=[nrm])
            S.op("dve", lambda: nc.vector.tensor_scalar(out=negm[:], in0=nrm[:], scalar1=cmax[0:1, 0:1], scalar2=None, op0=ALU.mult), reads=[nrm, cmax], writes=[negm])
            for h in range(16):
                S.op("dve", lambda: nc.vector.tensor_scalar(out=dg[:, h, :], in0=ident[:], scalar1=sgn[:, h:h + 1], scalar2=None, op0=ALU.mult),
                     reads=[ident, sgn], writes=[dg])
            scp = oacc[0]
            for kt in range(nkt):
                S.mm(STp[0], STp[0][:], iqs, iqs[:, 0, :], ikb, ikb[:, kt * 512:(kt + 1) * 512], start=True, stop=True)
                for h in range(16):
                    pb = STp[h % 2]
                    if h + 1 < 16:
                        pn = STp[(h + 1) % 2]
                        S.mm(pn, pn[:], iqs, iqs[:, h + 1, :], ikb, ikb[:, kt * 512:(kt + 1) * 512], start=True, stop=True)
                    R = Rb[rri % 3]; rri += 1
                    S.op("act", lambda: nc.scalar.activation(out=R[:], in_=pb[:], func=AF.Relu), reads=[pb], writes=[R])
                    S.mm(scp, scp[:], dg, dg[:, h, :], R, R[:], start=(h == 0), stop=(h == 15))
                S.op("dve", lambda: nc.vector.tensor_copy(out=sc[:, kt * 512:(kt + 1) * 512], in_=scp[:]), reads=[scp], writes=[sc])
            nk = 1024 * (j + 1)
            S.op("dve", lambda: nc.vector.tensor_reduce(out=hi[:], in_=sc[:, 0:nk], axis=AX.X, op=ALU.max, apply_absolute_value=True), reads=[sc], writes=[hi])
            S.op("dve", lambda: nc.vector.tensor_scalar(out=lo[:], in0=hi[:], scalar1=-1.0, scalar2=-1.0, op0=ALU.mult, op1=ALU.add), reads=[hi], writes=[lo])
            S.op("dve", lambda: nc.vector.tensor_scalar(out=hi[:], in0=hi[:], scalar1=1.0, scalar2=None, op0=ALU.add), reads=[hi], writes=[hi])
            S.op("dve", lambda: nc.vector.tensor_scalar(out=pen[:], in0=cst_sb[:, 128:1152], scalar1=trel_sb[:, j:j + 1], scalar2=-1e30, op0=ALU.is_gt, op1=ALU.mult),
                 reads=[cst_sb, trel_sb], writes=[pen])
            S.op("dve", lambda: nc.vector.tensor_tensor(out=sc[:, nk - 1024:nk], in0=sc[:, nk - 1024:nk], in1=pen[:], op=ALU.add), reads=[sc, pen], writes=[sc])
            for itn in range(NBIS):
                S.op("dve", lambda: nc.vector.tensor_tensor(out=mid[:], in0=lo[:], in1=hi[:], op=ALU.add), reads=[lo, hi], writes=[mid])
                S.op("dve", lambda: nc.vector.tensor_scalar(out=mid[:], in0=mid[:], scalar1=0.5, scalar2=None, op0=ALU.mult), reads=[mid], writes=[mid])
                first = True
                for c0 in range(0, nk, 2048):
                    w = min(2048, nk - c0)
                    if first:
                        S.op("dve", lambda: nc.vector.tensor_scalar(out=junk[:, 0:w], in0=sc[:, c0:c0 + w], scalar1=mid[:, 0:1], scalar2=None, op0=ALU.is_ge,
                                                                    op1=ALU.add, accum_out=cnt[:, 0:1]), reads=[sc, mid], writes=[junk, cnt])
                    else:
                        S.op("dve", lambda: nc.vector.tensor_scalar(out=junk[:, 0:w], in0=sc[:, c0:c0 + w], scalar1=mid[:, 0:1], scalar2=cnt[:, 0:1], op0=ALU.is_ge,
                                                                    op1=ALU.add, accum_out=cnt[:, 0:1]), reads=[sc, mid, cnt], writes=[junk, cnt])
                    first = False
                S.op("dve", lambda: nc.vector.tensor_scalar(out=prd[:], in0=cnt[:], scalar1=KSEL - 0.5, scalar2=None, op0=ALU.is_ge), reads=[cnt], writes=[prd])
                S.op("dve", lambda: nc.vector.tensor_tensor(out=dd[:], in0=mid[:], in1=lo[:], op=ALU.subtract), reads=[mid, lo], writes=[dd])
                S.op("dve", lambda: nc.vector.scalar_tensor_tensor(out=lo[:], in0=dd[:], scalar=prd[:, 0:1], in1=lo[:], op0=ALU.mult, op1=ALU.add),
                     reads=[dd, prd, lo], writes=[lo])
                S.op("dve", lambda: nc.vector.tensor_tensor(out=dd[:], in0=hi[:], in1=mid[:], op=ALU.subtract), reads=[hi, mid], writes=[dd])
                S.op("dve", lambda: nc.vector.scalar_tensor_tensor(out=hi[:], in0=dd[:], scalar=prd[:, 0:1], in1=mid[:], op0=ALU.mult, op1=ALU.add),
                     reads=[dd, prd, mid], writes=[hi])
            started = [False] * 4
            den_started = False
            for sbk in range(nsb + 1):
                rows = 16 if sbk == 0 else 128
                kb = sbk - 1
                if sbk >= 1 and kb % 4 == 0:
                    ktile = kb // 4
                    S.op("dve", lambda: nc.vector.tensor_scalar(out=maskb[:], in0=sc[:, ktile * 512:(ktile + 1) * 512], scalar1=lo[:, 0:1], scalar2=None, op0=ALU.is_ge),
                         reads=[sc, lo], writes=[maskb])
                    for q4 in range(4):
                        S.op("pe", lambda: nc.tensor.transpose(out=tpb[:, q4 * 128:(q4 + 1) * 128], in_=maskb[:, q4 * 128:(q4 + 1) * 128], identity=ident[:]),
                             reads=[maskb, ident], writes=[tpb])
                    S.op("act", lambda: nc.scalar.copy(out=maskT[:, :, :], in_=tpb[:, 0:512]), reads=[tpb], writes=[maskT])
                for hg in range(2):
                    stp = STp[hg]
                    for rc in range(2):
                        S.mm(stp, stp[0:rows, :], cT[rc], cT[rc][:, sbk * 128:sbk * 128 + rows], qlat[rc], qlat[rc][:, hg * 512:(hg + 1) * 512],
                             start=(rc == 0), stop=False)
                    S.mm(stp, stp[0:rows, :], onesrow, onesrow[0:1, 0:rows], negm, negm[0:1, hg * 512:(hg + 1) * 512], start=False, stop=True)
                    P = PT[pti % 2]; Pm = PTm[pti % 2]; pti += 1
                    S.op("act", lambda: nc.scalar.activation(out=P[0:rows, :], in_=stp[0:rows, :], func=AF.Exp), reads=[stp], writes=[P])
                    if sbk == 0:
                        Pm = P
                    else:
                        q4 = kb % 4
                        S.op("dve", lambda: nc.vector.tensor_tensor(out=Pm[:].rearrange("p (h t) -> p h t", h=4), in0=P[:].rearrange("p (h t) -> p h t", h=4),
                                                                     in1=maskT[:, q4:q4 + 1, :].to_broadcast([128, 4, 128]), op=ALU.mult),
                             reads=[P, maskT], writes=[Pm])
                    for hh in range(4):
                        h = hg * 4 + hh
                        bank = oacc[h // 2]
                        S.mm(bank, bank[:, (h % 2) * 256:(h % 2 + 1) * 256], Pm, Pm[0:rows, hh * 128:(hh + 1) * 128], ctok, ctok[0:rows, sbk, :],
                             start=(not started[h // 2]), stop=(sbk == nsb), skip_group_check=True)
                        started[h // 2] = True
                        S.mm(den, den[:, h:h + 1], Pm, Pm[0:rows, hh * 128:(hh + 1) * 128], ones, ones[0:rows, 0:1],
                             start=(not den_started), stop=(sbk == nsb), skip_group_check=True)
                        den_started = True
            S.op("dve", lambda: nc.vector.tensor_copy(out=den_sb[:], in_=den[:, 0:8]), reads=[den], writes=[den_sb])
            S.op("dve", lambda: nc.vector.reciprocal(out=rec[:], in_=den_sb[:]), reads=[den_sb], writes=[rec])
            for h in range(8):
                bank = oacc[h // 2]
                S.op("dve", lambda: nc.vector.tensor_scalar(out=o_sb[:, h, :], in0=bank[:, (h % 2) * 256:(h % 2 + 1) * 256], scalar1=rec[:, h:h + 1], scalar2=None, op0=ALU.mult),
                     reads=[bank, rec], writes=[o_sb])
            for rc in range(2):
                for h in range(8):
                    S.op("pe", lambda: nc.tensor.transpose(out=tpb[:, h * 128:(h + 1) * 128], in_=o_sb[:, h, rc * 128:(rc + 1) * 128], identity=ident[:]),
                         reads=[o_sb, ident], writes=[tpb])
                S.op("act", lambda: nc.scalar.copy(out=oT[rc][:, :, :], in_=tpb[:, :]), reads=[tpb], writes=[oT[rc]])
            for hg in range(2):
                stp = STp[hg]
                for hh in range(4):
                    h = hg * 4 + hh
                    for rc in range(2):
                        S.mm(stp, stp[:, hh * 128:(hh + 1) * 128], wuvb, wuvb[:, rc, h, :], oT[rc], oT[rc][:, h, :], start=(rc == 0), stop=(rc == 1))
                a = ao[hg]
                S.op("act", lambda: nc.scalar.copy(out=a[:, :, :], in_=stp[:]), reads=[stp], writes=[a])
                S.dma("sp", att_out[j, :, hg * 4:(hg + 1) * 4, :], a[:, :, :], reads=[a], sem=sem_ao[hg])
        S.finish(ao)
        print("attn instructions", S.ninst)
    return nc


def attn_consts():
    c = np.zeros((128, 1152), np.float32)
    c[:, 0:128] = np.eye(128, dtype=np.float32)
    c[:, 128:1152] = np.arange(1024, dtype=np.float32)[None, :]
    return c


def attn_inputs(core, nblk, q, ckv, iq, ik, iw, kvg, w_uk, w_uv):
    NKEY = 1024 * nblk
    NSB = NKEY // 128
    ckv_tok = np.zeros((NSB + 1, 128, 256), np.float32)
    ckv_tok[0, :NMETA] = ckv[:NMETA]
    ckv_tok[1:] = ckv[NMETA:NMETA + NKEY].reshape(NSB, 128, 256)
    d = {"ckv_tok": ckv_tok,
         "ikT": np.ascontiguousarray(ik[NMETA:NMETA + NKEY].T),
         "gkv_bc": np.ascontiguousarray(np.broadcast_to(kvg[None, :], (128, 256))).astype(np.float32),
         "wukT": np.ascontiguousarray(w_uk.transpose(2, 1, 0)),
         "wuv": np.ascontiguousarray(w_uv.reshape(2, 128, 8, 128)),
         "cst": attn_consts()}
    qT = np.zeros((nblk, 128, 8, 128), np.float32)
    iqT = np.zeros((nblk, 64, 16, 128), np.float32)
    iwb = np.zeros((nblk, 64, 16, 128), np.float32)
    iwt = np.zeros((nblk, 128, 16), np.float32)
    trel = np.zeros((128, nblk), np.float32)
    toks = []
    for j in range(nblk):
        qb = 8 * j + core
        tok = NMETA + 128 * qb + np.arange(128)
        toks.append(tok)
        qT[j] = q[tok].reshape(128, 8, 128).transpose(2, 1, 0)
        iqT[j] = iq[tok].reshape(128, 16, 64).transpose(2, 1, 0)
        iwb[j] = np.broadcast_to(iw[tok].T[None, :, :], (64, 16, 128))
        iwt[j] = iw[tok]
        trel[:, j] = (128 * qb + np.arange(128)) - 1024 * j
    d.update({"qT": qT, "iqT": iqT, "iw_bc": iwb, "iw_tok": iwt, "trel": trel})
    return d, toks


def attn_unpack(att_out, toks, att_full):
    for j, tok in enumerate(toks):
        att_full[tok] = att_out[j].transpose(2, 1, 0).reshape(128, 1024)


def rglru_inputs(core, ntiles, rx, ry, conv_w, conv_b, w_a, b_a, w_i, b_i, lam):
    TPR = ntiles * 512
    T = rx.shape[0]
    sl = slice(core * 128, (core + 1) * 128)
    rxy = np.zeros((2, 128, TPR), np.float32)
    n = min(T, TPR)
    rxy[0, :, :n] = rx[:n, sl].T
    rxy[1, :, :n] = ry[:n, sl].T
    rgp = np.zeros((128, 8), np.float32)
    for jt in range(4):
        rgp[:, jt] = conv_w[jt, sl]
    rgp[:, 4] = conv_b[sl]; rgp[:, 5] = b_a[sl]; rgp[:, 6] = b_i[sl]; rgp[:, 7] = lam[sl]
    rgw = np.ascontiguousarray(np.stack([w_a[core], w_i[core]], 0))
    return {"rxy": rxy, "rgp": rgp, "rgw": rgw}


def tok_idx_l0(c):
    r0 = NMETA + c * 1024
    return np.concatenate([np.arange(r0, r0 + 512), np.arange(0, NMETA), np.arange(r0 + 512, r0 + 1024)])


def run_inproj(h_tok, w_in, gain):
    M = w_in.shape[1]
    nmb = M // 128
    key = ("ip", nmb)
    if key not in _NC_CACHE:
        _NC_CACHE[key] = build_rowlocal(0, inproj_mb=nmb)
    nc = _NC_CACHE[key]
    gains = np.ascontiguousarray(np.stack([gvec(gain)] * 4, axis=1)).astype(np.float32)
    wt = tile_w(w_in)
    in_maps = []
    for c in range(NCORES):
        in_maps.append({"hT_in": fm(h_tok[tok_idx_l0(c)]), "gains": gains, "cd_t": wt})
    res = run_bass_kernel_spmd(nc, in_maps, core_ids=list(range(NCORES)))
    proj = np.zeros((h_tok.shape[0], M), np.float32)
    for c in range(NCORES):
        p = np.asarray(res.results[c]["proj_out"])
        proj[tok_idx_l0(c)] = p.transpose(2, 0, 1).reshape(p.shape[2], nmb * 128)
    return proj


def run_mix0(proj0, lb_logits, out_norm_g, sconv_w):
    if "m0" not in _NC_CACHE:
        _NC_CACHE["m0"] = build_mix0(17)
    nc = _NC_CACHE["m0"]
    T = proj0.shape[0]
    P = np.zeros((TP0, 7168), np.float32)
    P[48:48 + T] = proj0
    cst = mix0_consts()
    in_maps = []
    for c in range(NCORES):
        pj = np.ascontiguousarray(np.stack([P[:, i * 1024 + c * 128: i * 1024 + (c + 1) * 128].T for i in range(7)], 0))
        par = np.zeros((128, 8), np.float32)
        par[:, 0] = lb_logits[0, c * 128:(c + 1) * 128]
        par[:, 1] = lb_logits[1, c * 128:(c + 1) * 128]
        par[:, 2] = out_norm_g
        for jt in range(3):
            par[:, 3 + jt] = sconv_w[jt, c * 128:(c + 1) * 128]
        in_maps.append({"pj": pj, "par": par, "cst": cst})
    res = run_bass_kernel_spmd(nc, in_maps, core_ids=list(range(NCORES)))
    mix = np.zeros((T, 2048), np.float32)
    for c in range(NCORES):
        mo = np.asarray(res.results[c]["mo"])
        mix[:, c * 128:(c + 1) * 128] = mo[0].T[48:48 + T]
        mix[:, 1024 + c * 128:1024 + (c + 1) * 128] = mo[1].T[48:48 + T]
    return mix


def run_mix1(proj1, inp):
    T = proj1.shape[0]
    sizes = [1024, 1024, 1024, 256, 1024, 64, 16]
    rx, ry, q, ckv, iq, ik, iw = np.split(proj1, np.cumsum(sizes)[:-1], axis=-1)
    if "rg" not in _NC_CACHE:
        _NC_CACHE["rg"] = build_rglru(17)
    nc = _NC_CACHE["rg"]
    in_maps = [rglru_inputs(c, 17, rx, ry, inp["rg_conv_w"][0], inp["rg_conv_b"][0], inp["rg_w_a"][0], inp["rg_b_a"][0],
                            inp["rg_w_i"][0], inp["rg_b_i"][0], inp["rg_lambda"][0]) for c in range(NCORES)]
    res = run_bass_kernel_spmd(nc, in_maps, core_ids=list(range(NCORES)))
    mix = np.zeros((T, 2048), np.float32)
    for c in range(NCORES):
        mix[:, c * 128:(c + 1) * 128] = np.asarray(res.results[c]["hc_out"]).T[:T]
    if "at" not in _NC_CACHE:
        _NC_CACHE["at"] = build_attn(8)
    nc = _NC_CACHE["at"]
    in_maps = []
    toks_all = []
    for c in range(NCORES):
        d, toks = attn_inputs(c, 8, q, ckv, iq, ik, iw, inp["mla_kv_norm"][0], inp["mla_w_uk"][0], inp["mla_w_uv"][0])
        in_maps.append(d)
        toks_all.append(toks)
    res = run_bass_kernel_spmd(nc, in_maps, core_ids=list(range(NCORES)))
    att = np.zeros((T, 1024), np.float32)
    for c in range(NCORES):
        attn_unpack(np.asarray(res.results[c]["att_out"]), toks_all[c], att)
    mix[:, 1024:] = att
    return mix


def kernel(**inp):
    inp = {k: np.asarray(v) for k, v in inp.items()}
    x = inp["x"]
    h0 = np.concatenate([inp["meta_tokens"].astype(np.float32), x[0]], axis=0)
    proj0 = run_inproj(h0, inp["ab_w_in"][0], inp["ln_mix_pre"][0])
    mix0 = run_mix0(proj0, inp["hgrn_lb_logits"], inp["hgrn_out_norm"][0], inp["sconv_w"][0])
    h1, proj1 = run_rowlocal(0, h0, mix0, inp["ab_w_out"][0], inp["ffn_w1"][0], inp["ffn_w3"][0], inp["ffn_w2"][0],
                             [inp["ln_mix_post"][0], inp["ln_ffn_pre"][0], inp["ln_ffn_post"][0], inp["ln_mix_pre"][1]],
                             inp["cd_w_in"][0])
    mix1 = run_mix1(proj1, inp)
    h2, _ = run_rowlocal(1, h1[NMETA:], mix1[NMETA:], inp["cd_w_out"][0], inp["ffn_w1"][1], inp["ffn_w3"][1], inp["ffn_w2"][1],
                         [inp["ln_mix_post"][1], inp["ln_ffn_pre"][1], inp["ln_ffn_post"][1], inp["ln_mix_pre"][1]])
    return h2[None].astype(np.float32)
```

```python
import numpy as np
from contextlib import ExitStack
import concourse.bass as bass
import concourse.mybir as mybir
from concourse.bass_utils import run_bass_kernel_spmd

F32 = mybir.dt.float32
BF16 = mybir.dt.bfloat16
AF = mybir.ActivationFunctionType
ALU = mybir.AluOpType
AX = mybir.AxisListType

D = 2048
DFF = 5632
NMETA = 16
SEQ = 8192
EPS = 1e-6
NCORES = 8


class T:
    def __init__(self, ap, name=""):
        self.ap = ap
        self.name = name
        self.w = None
        self.r = []

    def __getitem__(self, k):
        return self.ap[k]


class View(T):
    pass


class Sched:
    def __init__(self, nc, stack):
        self.nc = nc
        self.stack = stack
        self.eng = {"pe": nc.tensor, "act": nc.scalar, "dve": nc.vector, "pool": nc.gpsimd, "sp": nc.sync}
        self.sem = {k: stack.enter_context(nc.semaphore("s_" + k)) for k in self.eng}
        self.cnt = {k: 0 for k in self.eng}
        self.seen = {k: {} for k in self.eng}
        self.nsem = 0
        self.ninst = 0

    def sb(self, name, shape, dt):
        t = self.stack.enter_context(self.nc.sbuf_tensor(name, shape, dt))
        return T(t, name)

    def ps(self, name, shape=(128, 512), dt=F32):
        t = self.stack.enter_context(self.nc.psum_tensor(name, list(shape), dt))
        return T(t, name)

    def dsem(self, name):
        self.nsem += 1
        return [self.stack.enter_context(self.nc.semaphore(name)), 0]

    def _wait(self, e, tok):
        if tok is None:
            return
        sem, val, owner = tok
        if owner == e and e == "pe":
            return
        seen = self.seen[e]
        key = id(sem)
        if seen.get(key, 0) >= val:
            return
        self.eng[e].wait_ge(sem, val)
        seen[key] = val

    def deps(self, e, reads, writes):
        toks = []
        for t in reads:
            toks.append(t.w)
        for t in writes:
            toks.append(t.w)
            toks.extend(t.r)
        best = {}
        for tok in toks:
            if tok is None:
                continue
            k = id(tok[0])
            if k not in best or best[k][1] < tok[1]:
                best[k] = tok
        for tok in best.values():
            self._wait(e, tok)

    def done(self, tok, reads, writes):
        for t in reads:
            t.r.append(tok)
            if len(t.r) > 64:
                t.r = t.r[-48:]
        for t in writes:
            t.w = tok
            t.r = []

    def op(self, e, fn, reads=(), writes=()):
        self.deps(e, reads, writes)
        ins = fn()
        self.cnt[e] += 1
        self.ninst += 1
        ins.then_inc(self.sem[e], 1)
        tok = (self.sem[e], self.cnt[e], e)
        self.done(tok, reads, writes)

    def dma(self, q, out, in_, reads=(), writes=(), sem=None, **kw):
        self.deps(q, reads, writes)
        ins = self.eng[q].dma_start(out=out, in_=in_, **kw)
        sem[1] += 16
        self.ninst += 1
        ins.then_inc(sem[0], 16)
        tok = (sem[0], sem[1], "dma")
        self.done(tok, reads, writes)

    def finish(self, tiles):
        self.deps("sp", [], tiles)

    def mm(self, out_t, out_ap, lhsT_t, lhsT_ap, rhs_t, rhs_ap, start, stop, **kw):
        nc = self.nc
        self.op("pe", lambda: nc.tensor.matmul(out_ap, lhsT=lhsT_ap, rhs=rhs_ap, start=start, stop=stop, **kw),
                reads=[lhsT_t, rhs_t], writes=[out_t])


def trim_reads(t):
    pass


CD_M = 4432
CD_MB = 35


DBG = {'ss': True, 'norm': True, 'castpool': True}


def build_rowlocal(layer, stop=99, inproj_mb=None):
    with_cd = (layer == 0)
    CDMB = inproj_mb if inproj_mb else CD_MB
    if layer == 0:
        NT = 1040
        halves = [(0, 528, [(0, 512), (512, 16)]), (528, 512, [(0, 512)])]
    else:
        NT = 1024
        halves = [(0, 512, [(0, 512)]), (512, 512, [(0, 512)])]
    HN = 528
    nc = bass.Bass("TRN2", target_bir_lowering=False)
    hT_in = nc.dram_tensor("hT_in", [128, 16, NT], F32, kind="ExternalInput").ap()
    if not inproj_mb:
        mixT_in = nc.dram_tensor("mixT_in", [128, 16, NT], F32, kind="ExternalInput").ap()
        w_out_t = nc.dram_tensor("w_out_t", [16, 128, 16, 128], BF16, kind="ExternalInput").ap()
        w1_t = nc.dram_tensor("w1_t", [44, 128, 16, 128], BF16, kind="ExternalInput").ap()
        w3_t = nc.dram_tensor("w3_t", [44, 128, 16, 128], BF16, kind="ExternalInput").ap()
        w2_t = nc.dram_tensor("w2_t", [16, 128, 44, 128], BF16, kind="ExternalInput").ap()
        hT_out = nc.dram_tensor("hT_out", [128, 16, NT], F32, kind="ExternalOutput").ap()
    NG = 4
    gains = nc.dram_tensor("gains", [128, NG, 16], F32, kind="ExternalInput").ap()
    if with_cd:
        cd_t = nc.dram_tensor("cd_t", [CDMB, 128, 16, 128], BF16, kind="ExternalInput").ap()
        proj_out = nc.dram_tensor("proj_out", [CDMB, 128, NT], F32, kind="ExternalOutput").ap()

    with ExitStack() as st:
        S = Sched(nc, st)
        hT = S.sb("hT", [128, 16, HN], F32)
        xb = S.sb("xb", [128, 16, HN], BF16)
        y = S.sb("y", [128, 16, HN], F32)
        m = S.sb("m", [128, 44, HN], BF16)
        g_sb = S.sb("g_sb", [128, NG, 16], F32)
        ones = S.sb("ones", [128, 128], BF16)
        rstd = S.sb("rstd", [128, HN], F32)
        lnt = S.sb("lnt", [128, HN], F32)
        NWB = 6
        wbf = [S.sb(f"wbf{i}", [128, 16, 128], BF16) for i in range(NWB)]
        wbf_sem = [S.dsem(f"wbfsem{i}") for i in range(NWB)]
        sq = [S.sb(f"sq{i}", [128, HN], BF16) for i in range(2)]
        tmp = [S.sb(f"tmp{i}", [128, HN], F32) for i in range(2)]
        ostg = [S.sb(f"ostg{i}", [128, HN], F32) for i in range(2)]
        ostg_sem = [S.dsem(f"ostgsem{i}") for i in range(2)]
        accb = [[S.ps(f"acc{w}{b}") for b in range(2)] for w in range(2)]
        small = [S.ps("small0"), S.ps("small1")]
        smallv = [[small[w] for b in range(2)] for w in range(2)]
        ssb = S.ps("ssb")
        sss = S.ps("sss")
        io_sem = [S.dsem("io0"), S.dsem("io1"), S.dsem("io2"), S.dsem("io3")]

        S.dma("sp", g_sb[:], gains[:, :, :], writes=[g_sb], sem=io_sem[2])
        S.op("dve", lambda: nc.vector.memset(ones[:], 1.0), writes=[ones])

        state = {"fill": 0, "wb": 0, "blk": 0, "sq": 0, "tmp": 0, "ostg": 0, "cast": 0}

        def load_w(src_ap, kn):
            j = state["wb"] % NWB
            state["wb"] += 1
            S.dma("sp", wbf[j][:, 0:kn, :], src_ap, writes=[wbf[j]], sem=wbf_sem[j])
            return wbf[j]

        def acc_aps(w, b, ntiles):
            res = []
            for (n0, nsz) in ntiles:
                if nsz == 512:
                    res.append((accb[w][b], accb[w][b][:, 0:512]))
                else:
                    off = (w * 2 + b) * 16
                    res.append((smallv[w][b], smallv[w][b][:, off:off + nsz]))
            return res

        def linear(x_t, KC, wsrcs, nmb, ntiles, consume, fills):
            seq = [(mb, wi, fi) for mb in range(nmb) for wi in range(len(wsrcs)) for fi in range(len(fills))]
            loaded = {}
            nxt = [0]
            LOOK = 4

            def ensure(upto):
                while nxt[0] <= min(upto, len(seq) - 1):
                    mb_, wi_, fi_ = seq[nxt[0]]
                    k0_, kn_ = fills[fi_]
                    loaded[nxt[0]] = load_w(wsrcs[wi_][mb_, :, k0_:k0_ + kn_, :], kn_)
                    nxt[0] += 1
            b = 0
            accs = []
            aps = None
            for i, (mb, wi, fi) in enumerate(seq):
                ensure(i + LOOK)
                if wi == 0 and fi == 0:
                    b = state["blk"] % 2
                    state["blk"] += 1
                    accs = []
                if fi == 0:
                    aps = acc_aps(wi, b, ntiles)
                wt = loaded.pop(i)
                k0, kn = fills[fi]
                for kk in range(kn):
                    kc = k0 + kk
                    for ti, (n0, nsz) in enumerate(ntiles):
                        at, aap = aps[ti]
                        S.mm(at, aap, wt, wt[:, kk, :], x_t, x_t[:, kc, n0:n0 + nsz],
                             start=(kc == 0), stop=(kc == KC - 1))
                if fi == len(fills) - 1:
                    accs.append(aps)
                    if wi == len(wsrcs) - 1:
                        consume(mb, accs)

        def rstd_from(ss_list, ntiles):
            for (sst, ssap), (n0, nsz) in zip(ss_list, ntiles):
                S.op("act", lambda: nc.scalar.activation(out=lnt[:, n0:n0 + nsz], in_=ssap, func=AF.Ln,
                                                         bias=eps_t[:, 0:1], scale=1.0 / D),
                     reads=[sst, eps_t], writes=[lnt])
                S.op("act", lambda: nc.scalar.activation(out=rstd[:, n0:n0 + nsz], in_=lnt[:, n0:n0 + nsz],
                                                         func=AF.Exp, scale=-0.5),
                     reads=[lnt], writes=[rstd])

        eps_t = S.sb("eps_t", [128, 1], F32)
        S.op("dve", lambda: nc.vector.memset(eps_t[:], EPS), writes=[eps_t])

        def ss_aps(ntiles):
            res = []
            for (n0, nsz) in ntiles:
                if nsz == 512:
                    res.append((ssb, ssb[:, 0:512]))
                else:
                    res.append((sss, sss[:, 0:nsz]))
            return res

        def ss_accum(src_t, src_ap_fn, kc, ntiles, from_psum_aps=None):
            ssl = ss_aps(ntiles)
            for ti, (n0, nsz) in enumerate(ntiles):
                i = state["sq"] % 2
                state["sq"] += 1
                if from_psum_aps is not None:
                    st_, sap = from_psum_aps[ti]
                else:
                    st_, sap = src_t, src_ap_fn(n0, nsz)
                S.op("act", lambda: nc.scalar.activation(out=sq[i][:, 0:nsz], in_=sap, func=AF.Square),
                     reads=[st_], writes=[sq[i]])
                S.mm(ssl[ti][0], ssl[ti][1], ones, ones[:], sq[i], sq[i][:, 0:nsz], start=(kc == 0), stop=(kc == 15))

        def post_norm_residual(gidx, ntiles, hw):
            for kc in range(16):
                i = state["tmp"] % 2
                state["tmp"] += 1
                S.op("dve", lambda: nc.vector.scalar_tensor_tensor(
                    out=tmp[i][:, 0:hw], in0=y[:, kc, 0:hw], scalar=g_sb[:, gidx, kc:kc + 1], in1=rstd[:, 0:hw],
                    op0=ALU.mult, op1=ALU.mult), reads=[y, g_sb, rstd], writes=[tmp[i]])
                S.op("dve", lambda: nc.vector.tensor_tensor(out=hT[:, kc, 0:hw], in0=hT[:, kc, 0:hw],
                                                             in1=tmp[i][:, 0:hw], op=ALU.add),
                     reads=[tmp[i], hT], writes=[hT])

        def pre_norm(gidx, ntiles, hw):
            for kc in range(16):
                ss_accum(hT, lambda n0, nsz: hT[:, kc, n0:n0 + nsz], kc, ntiles)
            rstd_from(ss_aps(ntiles), ntiles)
            for kc in range(16):
                S.op("dve", lambda: nc.vector.scalar_tensor_tensor(
                    out=xb[:, kc, 0:hw], in0=hT[:, kc, 0:hw], scalar=g_sb[:, gidx, kc:kc + 1], in1=rstd[:, 0:hw],
                    op0=ALU.mult, op1=ALU.mult), reads=[hT, g_sb, rstd], writes=[xb])

        def consume_y(ntiles):
            def f(mb, accs):
                aps = accs[0]
                for ti, (n0, nsz) in enumerate(ntiles):
                    at, aap = aps[ti]
                    S.op("dve", lambda: nc.vector.tensor_copy(out=y[:, mb, n0:n0 + nsz], in_=aap),
                         reads=[at], writes=[y])
                if DBG['ss']:
                    ss_accum(y, lambda n0, nsz: y[:, mb, n0:n0 + nsz], mb, ntiles)
            return f

        for (h0, hw, ntiles) in halves:
            if inproj_mb:
                for k4 in range(0, 16, 2):
                    S.dma("sp", hT[:, k4:k4 + 2, 0:hw], hT_in[:, k4:k4 + 2, h0:h0 + hw], writes=[hT], sem=io_sem[0])
                pre_norm(0, ntiles, hw)

                def consume_ip(mb, accs):
                    i = state["ostg"] % 2
                    state["ostg"] += 1
                    for ti, (n0, nsz) in enumerate(ntiles):
                        at, aap = accs[0][ti]
                        S.op("dve", lambda: nc.vector.tensor_copy(out=ostg[i][:, n0:n0 + nsz], in_=aap),
                             reads=[at], writes=[ostg[i]])
                    S.dma("sp", proj_out[mb, :, h0:h0 + hw], ostg[i][:, 0:hw], reads=[ostg[i]], sem=ostg_sem[i])
                linear(xb, 16, [cd_t], CDMB, ntiles, consume_ip, [(0, 16)])
                continue
            for k4 in range(0, 16, 2):
                S.dma("sp", hT[:, k4:k4 + 2, 0:hw], hT_in[:, k4:k4 + 2, h0:h0 + hw], writes=[hT], sem=io_sem[0])
                S.dma("sp", y[:, k4:k4 + 2, 0:hw], mixT_in[:, k4:k4 + 2, h0:h0 + hw], writes=[y], sem=io_sem[1])
            for kc in range(16):
                eng = "dve"
                if eng == "dve":
                    S.op("dve", lambda: nc.vector.tensor_copy(out=xb[:, kc, 0:hw], in_=y[:, kc, 0:hw]),
                         reads=[y], writes=[xb])
                else:
                    S.op("pool", lambda: nc.gpsimd.tensor_copy(out=xb[:, kc, 0:hw], in_=y[:, kc, 0:hw]),
                         reads=[y], writes=[xb])
            if stop >= 1:
                linear(xb, 16, [w_out_t], 16, ntiles, consume_y(ntiles), [(0, 16)])
                if DBG['norm']:
                    rstd_from(ss_aps(ntiles), ntiles)
                    post_norm_residual(0, ntiles, hw)
                else:
                    for kc in range(16):
                        S.op("dve", lambda: nc.vector.tensor_copy(out=hT[:, kc, 0:hw], in_=y[:, kc, 0:hw]), reads=[y], writes=[hT])
            if stop == 0:
                for kc in range(16):
                    S.op("dve", lambda: nc.vector.tensor_copy(out=hT[:, kc, 0:hw], in_=y[:, kc, 0:hw]), reads=[y], writes=[hT])
            if stop <= 1:
                for k4 in range(0, 16, 2):
                    S.dma("sp", hT_out[:, k4:k4 + 2, h0:h0 + hw], hT[:, k4:k4 + 2, 0:hw], reads=[hT], sem=io_sem[3])
                continue
            pre_norm(1, ntiles, hw)

            def consume_ab(mb, accs):
                for ti, (n0, nsz) in enumerate(ntiles):
                    i = state["tmp"] % 2
                    state["tmp"] += 1
                    at, aap = accs[0][ti]
                    bt, bap = accs[1][ti]
                    S.op("act", lambda: nc.scalar.activation(out=tmp[i][:, 0:nsz], in_=aap, func=AF.Silu),
                         reads=[at], writes=[tmp[i]])
                    S.op("dve", lambda: nc.vector.tensor_tensor(out=m[:, mb, n0:n0 + nsz], in0=tmp[i][:, 0:nsz],
                                                                 in1=bap, op=ALU.mult),
                         reads=[tmp[i], bt], writes=[m])
            linear(xb, 16, [w1_t, w3_t], 44, ntiles, consume_ab, [(0, 16)])
            linear(m, 44, [w2_t], 16, ntiles, consume_y(ntiles), [(0, 16), (16, 16), (32, 12)])
            rstd_from(ss_aps(ntiles), ntiles)
            post_norm_residual(2, ntiles, hw)
            for k4 in range(0, 16, 2):
                S.dma("sp", hT_out[:, k4:k4 + 2, h0:h0 + hw], hT[:, k4:k4 + 2, 0:hw], reads=[hT], sem=io_sem[3])
            if with_cd:
                pre_norm(3, ntiles, hw)

                def consume_cd(mb, accs):
                    i = state["ostg"] % 2
                    state["ostg"] += 1
                    for ti, (n0, nsz) in enumerate(ntiles):
                        at, aap = accs[0][ti]
                        S.op("dve", lambda: nc.vector.tensor_copy(out=ostg[i][:, n0:n0 + nsz], in_=aap),
                             reads=[at], writes=[ostg[i]])
                    S.dma("sp", proj_out[mb, :, h0:h0 + hw], ostg[i][:, 0:hw], reads=[ostg[i]], sem=ostg_sem[i])
                linear(xb, 16, [cd_t], CDMB, ntiles, consume_cd, [(0, 16)])
        S.finish([hT, ostg[0], ostg[1]] if (with_cd or inproj_mb) else [hT])
        print("rowlocal layer", layer, "instructions", S.ninst)
    return nc


def tile_w(w, mw=128):
    K, M = w.shape
    Mp = -(-M // mw) * mw
    if Mp != M:
        w = np.concatenate([w, np.zeros((K, Mp - M), w.dtype)], axis=1)
    return np.ascontiguousarray(w.reshape(K // 128, 128, Mp // mw, mw).transpose(2, 1, 0, 3))


def fm(a):
    Tn, Fn = a.shape
    return np.ascontiguousarray(a.reshape(Tn, Fn // 128, 128).transpose(2, 1, 0))


def unfm(a):
    p, kc, Tn = a.shape
    return np.ascontiguousarray(a.transpose(2, 1, 0).reshape(Tn, kc * 128))


def gvec(g):
    return np.ascontiguousarray(g.reshape(16, 128).T)


_NC_CACHE = {}


def run_rowlocal(layer, h_tok, mix_tok, w_out, w1, w3, w2, gain_list, cd_w=None):
    if ("rl", layer) not in _NC_CACHE:
        _NC_CACHE[("rl", layer)] = build_rowlocal(layer)
    nc = _NC_CACHE[("rl", layer)]
    gains = np.ascontiguousarray(np.stack([gvec(g) for g in gain_list], axis=1)).astype(np.float32)
    common = {"w_out_t": w_out, "w1_t": w1, "w3_t": w3, "w2_t": w2, "gains": gains}
    if layer == 0:
        common["cd_t"] = cd_w
    in_maps = []
    idxs = []
    for c in range(NCORES):
        if layer == 0:
            r0 = NMETA + c * 1024
            idx = np.concatenate([np.arange(r0, r0 + 512), np.arange(0, NMETA), np.arange(r0 + 512, r0 + 1024)])
        else:
            idx = np.arange(c * 1024, (c + 1) * 1024)
        idxs.append(idx)
        d = dict(common)
        d["hT_in"] = fm(h_tok[idx])
        d["mixT_in"] = fm(mix_tok[idx])
        in_maps.append(d)
    res = run_bass_kernel_spmd(nc, in_maps, core_ids=list(range(NCORES)))
    h_out = np.zeros_like(h_tok)
    proj = np.zeros((h_tok.shape[0], CD_M), np.float32) if layer == 0 else None
    for c in range(NCORES):
        r = res.results[c]
        h_out[idxs[c]] = unfm(np.asarray(r["hT_out"]))
        if layer == 0:
            p = np.asarray(r["proj_out"])
            pt = p.transpose(2, 0, 1).reshape(p.shape[2], CD_MB * 128)[:, :CD_M]
            proj[idxs[c]] = pt
    return h_out, proj


TP0 = 8704
NCONST = 128 + 512 + 512


def mix0_consts():
    c = np.zeros((128, NCONST), np.float32)
    c[:, 0:128] = np.eye(128, dtype=np.float32)
    tri = (np.arange(64)[None, :] >= np.arange(64)[:, None]).astype(np.float32)
    c[0:64, 128:640] = np.tile(tri, (1, 8))
    r = np.ones(512, np.float32)
    r[0::64] = 0.0
    c[:, 640:1152] = r[None, :]
    return c


def build_mix0(ntile=17):
    TP = ntile * 512
    nc = bass.Bass("TRN2", target_bir_lowering=False)
    pj = nc.dram_tensor("pj", [7, 128, TP], F32, kind="ExternalInput").ap()
    par = nc.dram_tensor("par", [128, 8], F32, kind="ExternalInput").ap()
    cst = nc.dram_tensor("cst", [128, NCONST], F32, kind="ExternalInput").ap()
    mo = nc.dram_tensor("mo", [2, 128, TP], F32, kind="ExternalOutput").ap()
    with ExitStack() as st:
        S = Sched(nc, st)
        f32t = lambda n, w=512: S.sb(n, [128, w], F32)
        bft = lambda n, w=512: S.sb(n, [128, w], BF16)
        par_sb = f32t("par_sb", 8)
        cst_sb = f32t("cst_sb", NCONST)
        ident = bft("ident", 128)
        ones = bft("ones", 128)
        lb = f32t("lb", 1); oml = f32t("oml", 1); eps_t = f32t("eps_t", 1)
        inp = [[f32t(f"in{b}_{i}") for i in range(7)] for b in range(2)]
        in_sem = [[S.dsem(f"insem{b}_{i}") for i in range(7)] for b in range(2)]
        sig = f32t("sig"); f_ = f32t("f_"); logf = f32t("logf"); b_ = f32t("b_"); k_ = f32t("k_")
        eb = f32t("eb"); enb = f32t("enb"); e2 = f32t("e2")
        Qt = bft("Qt"); Kt = bft("Kt"); Kp = bft("Kp"); Vb = bft("Vb")
        Vtok = S.sb("Vtok", [64, 8, 128], BF16); Ktok = S.sb("Ktok", [64, 8, 128], BF16)
        attm = S.sb("attm", [64, 512], BF16)
        Sf = f32t("Sf", 128)
        NSB = 4
        Sb = [bft(f"Sb{i}", 128) for i in range(NSB)]
        o_sb = f32t("o_sb"); osq = bft("osq"); lnt = f32t("lnt"); rstd = f32t("rstd"); sg = f32t("sg")
        res = [f32t(f"res{i}") for i in range(2)]; res_sem = [S.dsem(f"ressem{i}") for i in range(2)]
        ub = [f32t(f"ub{i}", 514) for i in range(2)]
        t1 = f32t("t1")
        yb = [f32t(f"yb{i}") for i in range(2)]; yb_sem = [S.dsem(f"ybsem{i}") for i in range(2)]
        tpv = S.ps("tpv", (128, 1024), BF16); tpk = S.ps("tpk", (128, 1024), BF16)
        att = S.ps("att"); o_ps = S.ps("o_ps"); U = [S.ps("U0"), S.ps("U1")]; sso = S.ps("sso")
        csem = S.dsem("csem")
        csem2 = S.dsem("csem2")
        S.dma("sp", par_sb[:], par[:, :], writes=[par_sb], sem=csem)
        S.dma("sp", cst_sb[:], cst[:, :], writes=[cst_sb], sem=csem2)
        tri = View(cst_sb.ap, "tri"); rmask = View(cst_sb.ap, "rmask")
        S.op("dve", lambda: nc.vector.tensor_copy(out=ident[:], in_=cst_sb[:, 0:128]), reads=[cst_sb], writes=[ident])
        S.op("dve", lambda: nc.vector.memset(ones[:], 1.0), writes=[ones])
        S.op("dve", lambda: nc.vector.memset(eps_t[:], EPS), writes=[eps_t])
        S.op("dve", lambda: nc.vector.memset(Sf[:], 0.0), writes=[Sf])
        S.op("dve", lambda: nc.vector.memset(Sb[0][:], 0.0), writes=[Sb[0]])
        S.op("dve", lambda: nc.vector.memset(ub[0][:], 0.0), writes=[ub[0]])
        S.op("dve", lambda: nc.vector.tensor_tensor(out=oml[:], in0=par_sb[:, 0:1], in1=par_sb[:, 1:2], op=ALU.subtract),
             reads=[par_sb], writes=[oml])
        S.op("act", lambda: nc.scalar.activation(out=lb[:], in_=oml[:], func=AF.Sigmoid), reads=[oml], writes=[lb])
        S.op("dve", lambda: nc.vector.tensor_scalar(out=oml[:], in0=lb[:], scalar1=-1.0, scalar2=1.0, op0=ALU.mult, op1=ALU.add),
             reads=[lb], writes=[oml])
        sbi = 0
        for tt in range(ntile):
            c0 = tt * 512
            bi = tt % 2
            I = inp[bi]
            for i in range(7):
                S.dma("sp", I[i][:], pj[i, :, c0:c0 + 512], writes=[I[i]], sem=in_sem[bi][i])
            q, fl, v, g, sx, sbb, sc = I
            S.op("act", lambda: nc.scalar.activation(out=sig[:], in_=fl[:], func=AF.Sigmoid), reads=[fl], writes=[sig])
            S.op("dve", lambda: nc.vector.tensor_scalar(out=f_[:], in0=sig[:], scalar1=oml[:, 0:1], scalar2=lb[:, 0:1],
                                                        op0=ALU.mult, op1=ALU.add), reads=[sig, oml, lb], writes=[f_])
            S.op("act", lambda: nc.scalar.activation(out=logf[:], in_=f_[:], func=AF.Ln), reads=[f_], writes=[logf])
            S.op("dve", lambda: nc.vector.tensor_tensor_scan(out=b_[:], data0=cst_sb[:, 640:1152], data1=logf[:], initial=0.0,
                                                             op0=ALU.mult, op1=ALU.add), reads=[cst_sb, logf], writes=[b_])
            S.op("dve", lambda: nc.vector.tensor_scalar(out=k_[:], in0=f_[:], scalar1=-1.0, scalar2=1.0, op0=ALU.mult, op1=ALU.add),
                 reads=[f_], writes=[k_])
            S.op("act", lambda: nc.scalar.activation(out=eb[:], in_=b_[:], func=AF.Exp), reads=[b_], writes=[eb])
            S.op("act", lambda: nc.scalar.activation(out=enb[:], in_=b_[:], func=AF.Exp, scale=-1.0), reads=[b_], writes=[enb])
            for j in range(8):
                S.op("act", lambda: nc.scalar.activation(out=e2[:, j * 64:(j + 1) * 64], in_=b_[:, j * 64:(j + 1) * 64], func=AF.Exp,
                                                         scale=-1.0, bias=b_[:, j * 64 + 63:j * 64 + 64]), reads=[b_], writes=[e2])
            S.op("dve", lambda: nc.vector.tensor_tensor(out=Qt[:], in0=q[:], in1=eb[:], op=ALU.mult), reads=[q, eb], writes=[Qt])
            S.op("dve", lambda: nc.vector.tensor_tensor(out=Kt[:], in0=k_[:], in1=enb[:], op=ALU.mult), reads=[k_, enb], writes=[Kt])
            S.op("dve", lambda: nc.vector.tensor_tensor(out=Kp[:], in0=k_[:], in1=e2[:], op=ALU.mult), reads=[k_, e2], writes=[Kp])
            S.op("dve", lambda: nc.vector.tensor_copy(out=Vb[:], in_=v[:]), reads=[v], writes=[Vb])
            for j in range(8):
                S.op("pe", lambda: nc.tensor.transpose(out=tpv[0:64, j * 128:(j + 1) * 128], in_=Vb[:, j * 64:(j + 1) * 64], identity=ident[:]),
                     reads=[Vb, ident], writes=[tpv])
            for j in range(8):
                S.op("pe", lambda: nc.tensor.transpose(out=tpk[0:64, j * 128:(j + 1) * 128], in_=Kp[:, j * 64:(j + 1) * 64], identity=ident[:]),
                     reads=[Kp, ident], writes=[tpk])
            S.op("act", lambda: nc.scalar.copy(out=Vtok[:, :, :], in_=tpv[0:64, :]), reads=[tpv], writes=[Vtok])
            S.op("act", lambda: nc.scalar.copy(out=Ktok[:, :, :], in_=tpk[0:64, :]), reads=[tpk], writes=[Ktok])
            for j in range(8):
                S.mm(att, att[0:64, j * 64:(j + 1) * 64], Kt, Kt[:, j * 64:(j + 1) * 64], Qt, Qt[:, j * 64:(j + 1) * 64], start=True, stop=True)
            S.op("dve", lambda: nc.vector.tensor_tensor(out=attm[:], in0=att[0:64, :], in1=cst_sb[0:64, 128:640], op=ALU.mult),
                 reads=[att, cst_sb], writes=[attm])
            for j in range(8):
                Uj = U[j // 4]
                S.mm(Uj, Uj[:, (j % 4) * 128:(j % 4 + 1) * 128], Ktok, Ktok[:, j, :], Vtok, Vtok[:, j, :], start=True, stop=True)
            for j in range(8):
                cur = Sb[sbi % NSB]
                nxt = Sb[(sbi + 1) % NSB]
                sbi += 1
                S.mm(o_ps, o_ps[:, j * 64:(j + 1) * 64], Vtok, Vtok[:, j, :], attm, attm[:, j * 64:(j + 1) * 64], start=True, stop=False)
                S.mm(o_ps, o_ps[:, j * 64:(j + 1) * 64], cur, cur[:], Qt, Qt[:, j * 64:(j + 1) * 64], start=False, stop=True)
                Uj = U[j // 4]
                uap = Uj[:, (j % 4) * 128:(j % 4 + 1) * 128]
                dcol = eb[:, j * 64 + 63:j * 64 + 64]
                S.op("dve", lambda: nc.vector.scalar_tensor_tensor(out=nxt[:], in0=Sf[:], scalar=dcol, in1=uap, op0=ALU.mult, op1=ALU.add),
                     reads=[Sf, eb, Uj], writes=[nxt])
                S.op("dve", lambda: nc.vector.scalar_tensor_tensor(out=Sf[:], in0=Sf[:], scalar=dcol, in1=uap, op0=ALU.mult, op1=ALU.add),
                     reads=[Sf, eb, Uj], writes=[Sf])
            S.op("act", lambda: nc.scalar.copy(out=o_sb[:], in_=o_ps[:]), reads=[o_ps], writes=[o_sb])
            S.op("act", lambda: nc.scalar.activation(out=osq[:], in_=o_sb[:], func=AF.Square), reads=[o_sb], writes=[osq])
            S.mm(sso, sso[:], ones, ones[:], osq, osq[:], start=True, stop=True)
            S.op("act", lambda: nc.scalar.activation(out=lnt[:], in_=sso[:], func=AF.Ln, bias=eps_t[:, 0:1], scale=1.0 / 128),
                 reads=[sso, eps_t], writes=[lnt])
            S.op("act", lambda: nc.scalar.activation(out=rstd[:], in_=lnt[:], func=AF.Exp, scale=-0.5), reads=[lnt], writes=[rstd])
            S.op("act", lambda: nc.scalar.activation(out=sg[:], in_=g[:], func=AF.Silu), reads=[g], writes=[sg])
            r_ = res[bi]
            S.op("dve", lambda: nc.vector.scalar_tensor_tensor(out=r_[:], in0=o_sb[:], scalar=par_sb[:, 2:3], in1=rstd[:], op0=ALU.mult, op1=ALU.mult),
                 reads=[o_sb, par_sb, rstd], writes=[r_])
            S.op("dve", lambda: nc.vector.tensor_tensor(out=r_[:], in0=r_[:], in1=sg[:], op=ALU.mult), reads=[r_, sg], writes=[r_])
            S.dma("sp", mo[0, :, c0:c0 + 512], r_[:], reads=[r_], sem=res_sem[bi])
            u = ub[bi]; un = ub[1 - bi]
            S.op("dve", lambda: nc.vector.tensor_tensor(out=u[:, 2:514], in0=sc[:], in1=sx[:], op=ALU.mult), reads=[sc, sx], writes=[u])
            S.op("dve", lambda: nc.vector.tensor_scalar(out=t1[:], in0=u[:, 2:514], scalar1=par_sb[:, 5:6], scalar2=None, op0=ALU.mult),
                 reads=[u, par_sb], writes=[t1])
            S.op("dve", lambda: nc.vector.scalar_tensor_tensor(out=t1[:], in0=u[:, 1:513], scalar=par_sb[:, 4:5], in1=t1[:], op0=ALU.mult, op1=ALU.add),
                 reads=[u, par_sb, t1], writes=[t1])
            S.op("dve", lambda: nc.vector.scalar_tensor_tensor(out=t1[:], in0=u[:, 0:512], scalar=par_sb[:, 3:4], in1=t1[:], op0=ALU.mult, op1=ALU.add),
                 reads=[u, par_sb, t1], writes=[t1])
            y_ = yb[bi]
            S.op("dve", lambda: nc.vector.tensor_tensor(out=y_[:], in0=t1[:], in1=sbb[:], op=ALU.mult), reads=[t1, sbb], writes=[y_])
            S.op("dve", lambda: nc.vector.tensor_copy(out=un[:, 0:2], in_=u[:, 512:514]), reads=[u], writes=[un])
            S.dma("sp", mo[1, :, c0:c0 + 512], y_[:], reads=[y_], sem=yb_sem[bi])
        S.finish(res + yb)
        print("mix0 instructions", S.ninst)
    return nc


KSEL = 256
NBIS = 13


def build_rglru(rg_tiles=17):
    TPR = rg_tiles * 512
    nc = bass.Bass("TRN2", target_bir_lowering=False)
    rxy = nc.dram_tensor("rxy", [2, 128, TPR], F32, kind="ExternalInput").ap()
    rgp = nc.dram_tensor("rgp", [128, 8], F32, kind="ExternalInput").ap()
    rgw = nc.dram_tensor("rgw", [2, 128, 128], F32, kind="ExternalInput").ap()
    hc_out = nc.dram_tensor("hc_out", [128, TPR], F32, kind="ExternalOutput").ap()
    with ExitStack() as st:
        S = Sched(nc, st)
        f32t = lambda n, w=512, p=128: S.sb(n, [p, w], F32)
        bft = lambda n, w=512, p=128: S.sb(n, [p, w], BF16)
        one_t = f32t("one_t", 1)
        S.op("dve", lambda: nc.vector.memset(one_t[:], 1.0), writes=[one_t])
        STp = [S.ps("ST0"), S.ps("ST1")]
        rgp_sb = f32t("rgp_sb", 8); rgw_f = S.sb("rgw_f", [128, 2, 128], F32); rgw_b = S.sb("rgw_b", [128, 2, 128], BF16)
        sem_r = [S.dsem("semr0"), S.dsem("semr1")]
        S.dma("sp", rgp_sb[:], rgp[:, :], writes=[rgp_sb], sem=sem_r[0])
        for i in range(2):
            S.dma("sp", rgw_f[:, i, :], rgw[i, :, :], writes=[rgw_f], sem=sem_r[1])
        S.op("dve", lambda: nc.vector.tensor_copy(out=rgw_b[:], in_=rgw_f[:]), reads=[rgw_f], writes=[rgw_b])
        c8 = f32t("c8", 1); c16 = f32t("c16", 1); tsm = f32t("tsm", 1)
        S.op("act", lambda: nc.scalar.activation(out=tsm[:], in_=rgp_sb[:, 7:8], func=AF.Exp, scale=-1.0), reads=[rgp_sb], writes=[tsm])
        S.op("act", lambda: nc.scalar.activation(out=tsm[:], in_=tsm[:], func=AF.Ln, bias=one_t[:, 0:1], scale=1.0), reads=[tsm, one_t], writes=[tsm])
        S.op("dve", lambda: nc.vector.tensor_scalar(out=c8[:], in0=tsm[:], scalar1=-8.0, scalar2=None, op0=ALU.mult), reads=[tsm], writes=[c8])
        S.op("dve", lambda: nc.vector.tensor_scalar(out=c16[:], in0=tsm[:], scalar1=-16.0, scalar2=None, op0=ALU.mult), reads=[tsm], writes=[c16])
        rxb = [f32t(f"rxb{i}", 515) for i in range(2)]
        ryb = [f32t(f"ryb{i}") for i in range(2)]
        sem_x = [S.dsem("semx0"), S.dsem("semx1")]; sem_y = [S.dsem("semy0"), S.dsem("semy1")]
        u_ = f32t("u_"); ub16 = bft("ub16"); r_ = f32t("r_"); ig = f32t("ig"); a_ = f32t("a_"); a2 = f32t("a2"); gx = f32t("gx")
        xin = f32t("xin"); hst = [f32t(f"hst{i}") for i in range(2)]; gl = f32t("gl"); gl2 = f32t("gl2")
        hco = [f32t(f"hco{i}") for i in range(2)]; sem_h = [S.dsem("semh0"), S.dsem("semh1")]
        S.op("dve", lambda: nc.vector.memset(rxb[0][:], 0.0), writes=[rxb[0]])
        S.op("dve", lambda: nc.vector.memset(hst[1][:], 0.0), writes=[hst[1]])
        for tt in range(rg_tiles):
            c0 = tt * 512; bi = tt % 2
            xb_ = rxb[bi]; xn = rxb[1 - bi]; yb_ = ryb[bi]
            S.dma("sp", xb_[:, 3:515], rxy[0, :, c0:c0 + 512], writes=[xb_], sem=sem_x[bi])
            S.dma("sp", yb_[:], rxy[1, :, c0:c0 + 512], writes=[yb_], sem=sem_y[bi])
            S.op("dve", lambda: nc.vector.tensor_scalar(out=u_[:], in0=xb_[:, 3:515], scalar1=rgp_sb[:, 3:4], scalar2=rgp_sb[:, 4:5],
                                                        op0=ALU.mult, op1=ALU.add), reads=[xb_, rgp_sb], writes=[u_])
            for jtap in range(3):
                S.op("dve", lambda: nc.vector.scalar_tensor_tensor(out=u_[:], in0=xb_[:, jtap:jtap + 512], scalar=rgp_sb[:, jtap:jtap + 1],
                                                                   in1=u_[:], op0=ALU.mult, op1=ALU.add), reads=[xb_, rgp_sb, u_], writes=[u_])
            S.op("dve", lambda: nc.vector.tensor_copy(out=xn[:, 0:3], in_=xb_[:, 512:515]), reads=[xb_], writes=[xn])
            S.op("dve", lambda: nc.vector.tensor_copy(out=ub16[:], in_=u_[:]), reads=[u_], writes=[ub16])
            S.mm(STp[0], STp[0][:], rgw_b, rgw_b[:, 0, :], ub16, ub16[:], start=True, stop=True)
            S.mm(STp[1], STp[1][:], rgw_b, rgw_b[:, 1, :], ub16, ub16[:], start=True, stop=True)
            S.op("act", lambda: nc.scalar.activation(out=r_[:], in_=STp[0][:], func=AF.Sigmoid, bias=rgp_sb[:, 5:6], scale=1.0),
                 reads=[STp[0], rgp_sb], writes=[r_])
            S.op("act", lambda: nc.scalar.activation(out=ig[:], in_=STp[1][:], func=AF.Sigmoid, bias=rgp_sb[:, 6:7], scale=1.0),
                 reads=[STp[1], rgp_sb], writes=[ig])
            S.op("act", lambda: nc.scalar.activation(out=a_[:], in_=r_[:], func=AF.Exp, scale=c8[:, 0:1]), reads=[r_, c8], writes=[a_])
            S.op("act", lambda: nc.scalar.activation(out=a2[:], in_=r_[:], func=AF.Exp, scale=c16[:, 0:1]), reads=[r_, c16], writes=[a2])
            S.op("dve", lambda: nc.vector.tensor_scalar(out=a2[:], in0=a2[:], scalar1=-1.0, scalar2=1.0, op0=ALU.mult, op1=ALU.add),
                 reads=[a2], writes=[a2])
            S.op("act", lambda: nc.scalar.activation(out=gx[:], in_=a2[:], func=AF.Sqrt), reads=[a2], writes=[gx])
            S.op("dve", lambda: nc.vector.tensor_tensor(out=xin[:], in0=ig[:], in1=u_[:], op=ALU.mult), reads=[ig, u_], writes=[xin])
            S.op("dve", lambda: nc.vector.tensor_tensor(out=xin[:], in0=xin[:], in1=gx[:], op=ALU.mult), reads=[xin, gx], writes=[xin])
            hprev = hst[1 - bi]; hcur = hst[bi]
            S.op("dve", lambda: nc.vector.tensor_tensor_scan(out=hcur[:], data0=a_[:], data1=xin[:], initial=hprev[:, 511:512],
                                                             op0=ALU.mult, op1=ALU.add), reads=[a_, xin, hprev], writes=[hcur])
            S.op("dve", lambda: nc.vector.tensor_tensor(out=gl[:], in0=yb_[:], in1=yb_[:], op=ALU.mult), reads=[yb_], writes=[gl])
            S.op("dve", lambda: nc.vector.tensor_scalar(out=gl[:], in0=gl[:], scalar1=0.044715, scalar2=1.0, op0=ALU.mult, op1=ALU.add),
                 reads=[gl], writes=[gl])
            S.op("dve", lambda: nc.vector.tensor_tensor(out=gl[:], in0=gl[:], in1=yb_[:], op=ALU.mult), reads=[gl, yb_], writes=[gl])
            S.op("act", lambda: nc.scalar.activation(out=gl2[:], in_=gl[:], func=AF.Sigmoid, scale=1.5957691216), reads=[gl], writes=[gl2])
            S.op("dve", lambda: nc.vector.tensor_tensor(out=gl2[:], in0=gl2[:], in1=yb_[:], op=ALU.mult), reads=[gl2, yb_], writes=[gl2])
            ho = hco[bi]
            S.op("dve", lambda: nc.vector.tensor_tensor(out=ho[:], in0=hcur[:], in1=gl2[:], op=ALU.mult), reads=[hcur, gl2], writes=[ho])
            S.dma("sp", hc_out[:, c0:c0 + 512], ho[:], reads=[ho], sem=sem_h[bi])

        S.finish(hco)
        print("rglru instructions", S.ninst)
    return nc


def build_attn(nblk=8):
    NKEY = 1024 * nblk
    NSB = NKEY // 128
    nc = bass.Bass("TRN2", target_bir_lowering=False)
    ckv_tok = nc.dram_tensor("ckv_tok", [NSB + 1, 128, 256], F32, kind="ExternalInput").ap()
    ikT = nc.dram_tensor("ikT", [64, NKEY], F32, kind="ExternalInput").ap()
    gkv_bc = nc.dram_tensor("gkv_bc", [128, 256], F32, kind="ExternalInput").ap()
    wukT = nc.dram_tensor("wukT", [128, 8, 256], F32, kind="ExternalInput").ap()
    wuv = nc.dram_tensor("wuv", [2, 128, 8, 128], F32, kind="ExternalInput").ap()
    qT = nc.dram_tensor("qT", [nblk, 128, 8, 128], F32, kind="ExternalInput").ap()
    iqT = nc.dram_tensor("iqT", [nblk, 64, 16, 128], F32, kind="ExternalInput").ap()
    iw_bc = nc.dram_tensor("iw_bc", [nblk, 64, 16, 128], F32, kind="ExternalInput").ap()
    iw_tok = nc.dram_tensor("iw_tok", [nblk, 128, 16], F32, kind="ExternalInput").ap()
    trel = nc.dram_tensor("trel", [128, nblk], F32, kind="ExternalInput").ap()
    cst = nc.dram_tensor("cst", [128, 128 + 1024], F32, kind="ExternalInput").ap()
    att_out = nc.dram_tensor("att_out", [nblk, 128, 8, 128], F32, kind="ExternalOutput").ap()

    with ExitStack() as st:
        S = Sched(nc, st)
        f32t = lambda n, w=512, p=128: S.sb(n, [p, w], F32)
        bft = lambda n, w=512, p=128: S.sb(n, [p, w], BF16)
        cst_sb = f32t("cst_sb", 1152)
        ident = bft("ident", 128); ones = bft("ones", 128)
        eps_t = f32t("eps_t", 1); one_t = f32t("one_t", 1)
        sem_c = S.dsem("semc")
        S.dma("sp", cst_sb[:], cst[:, :], writes=[cst_sb], sem=sem_c)
        S.op("dve", lambda: nc.vector.tensor_copy(out=ident[:], in_=cst_sb[:, 0:128]), reads=[cst_sb], writes=[ident])
        S.op("dve", lambda: nc.vector.memset(ones[:], 1.0), writes=[ones])
        S.op("dve", lambda: nc.vector.memset(eps_t[:], EPS), writes=[eps_t])
        S.op("dve", lambda: nc.vector.memset(one_t[:], 1.0), writes=[one_t])
        oacc = [S.ps(f"oacc{i}") for i in range(4)]
        den = S.ps("den")
        STp = [S.ps("ST0"), S.ps("ST1")]
        tpb = S.ps("tpb", (128, 1024), BF16)

        gkv = f32t("gkv", 256); sem_g = S.dsem("semg")
        S.dma("sp", gkv[:], gkv_bc[:, :], writes=[gkv], sem=sem_g)
        cT = [S.sb(f"cT{rc}", [128, NKEY + 128], BF16) for rc in range(2)]
        ctok = S.sb("ctok", [128, NSB + 1, 256], BF16)
        ikb = S.sb("ikb", [64, NKEY], BF16)
        sc = f32t("sc", NKEY)
        kst = [f32t(f"kst{i}", 256) for i in range(2)]; sem_k = [S.dsem("semk0"), S.dsem("semk1")]
        ksq_l = [f32t(f"ksq{i}", 256) for i in range(2)]; kss_l = [f32t(f"kss{i}", 1) for i in range(2)]; krs_l = [f32t(f"krs{i}", 1) for i in range(2)]
        S.op("dve", lambda: nc.vector.memset(kst[0][:], 0.0), writes=[kst[0]])
        for sb_ in range(NSB + 1):
            ks = kst[sb_ % 2]
            ksq = ksq_l[sb_ % 2]; kss = kss_l[sb_ % 2]; krs = krs_l[sb_ % 2]
            rows = 16 if sb_ == 0 else 128
            S.dma("sp", ks[0:rows, :], ckv_tok[sb_, 0:rows, :], writes=[ks], sem=sem_k[sb_ % 2])
            S.op("act", lambda: nc.scalar.activation(out=ksq[:], in_=ks[:], func=AF.Square, accum_out=kss[:, 0:1]), reads=[ks], writes=[ksq, kss])
            S.op("act", lambda: nc.scalar.activation(out=krs[:], in_=kss[:], func=AF.Ln, bias=eps_t[:, 0:1], scale=1.0 / 256), reads=[kss, eps_t], writes=[krs])
            S.op("act", lambda: nc.scalar.activation(out=krs[:], in_=krs[:], func=AF.Exp, scale=-0.5), reads=[krs], writes=[krs])
            S.op("dve", lambda: nc.vector.scalar_tensor_tensor(out=ctok[:, sb_, :], in0=ks[:], scalar=krs[:, 0:1], in1=gkv[:], op0=ALU.mult, op1=ALU.mult),
                 reads=[ks, krs, gkv], writes=[ctok])
            if sb_ == 0:
                S.op("dve", lambda: nc.vector.memset(kst[0][:], 0.0), reads=[ctok], writes=[kst[0]])
            for rc in range(2):
                S.op("pe", lambda: nc.tensor.transpose(out=tpb[:, rc * 128:(rc + 1) * 128], in_=ctok[:, sb_, rc * 128:(rc + 1) * 128], identity=ident[:]),
                     reads=[ctok, ident], writes=[tpb])
            for rc in range(2):
                S.op("act", lambda: nc.scalar.copy(out=cT[rc][:, sb_ * 128:(sb_ + 1) * 128], in_=tpb[:, rc * 128:(rc + 1) * 128]), reads=[tpb], writes=[cT[rc]])
        sem_i = S.dsem("semi0")
        for kc_ in range(NKEY // 1024):
            S.dma("sp", sc[0:64, 0:1024], ikT[:, kc_ * 1024:(kc_ + 1) * 1024], writes=[sc], sem=sem_i)
            S.op("dve", lambda: nc.vector.tensor_copy(out=ikb[:, kc_ * 1024:(kc_ + 1) * 1024], in_=sc[0:64, 0:1024]), reads=[sc], writes=[ikb])
        wukb = S.sb("wukb", [128, 8, 256], BF16); wuvb = S.sb("wuvb", [128, 2, 8, 128], BF16)
        sem_w = [S.dsem("semw0"), S.dsem("semw1")]
        S.dma("sp", sc[:, 0:2048], wukT.rearrange("p h r -> p (h r)"), writes=[sc], sem=sem_w[0])
        S.op("dve", lambda: nc.vector.tensor_copy(out=wukb[:].rearrange("p h r -> p (h r)"), in_=sc[:, 0:2048]), reads=[sc], writes=[wukb])
        for rc in range(2):
            S.dma("sp", sc[:, 0:1024], wuv[rc, :, :, :].rearrange("p h d -> p (h d)"), writes=[sc], sem=sem_w[1])
            S.op("dve", lambda: nc.vector.tensor_copy(out=wuvb[:, rc, :, :].rearrange("p h d -> p (h d)"), in_=sc[:, 0:1024]), reads=[sc], writes=[wuvb])
        cmax = f32t("cmax", 1)
        S.op("dve", lambda: nc.vector.tensor_reduce(out=cmax[:], in_=gkv[:], axis=AX.X, op=ALU.max, apply_absolute_value=True), reads=[gkv], writes=[cmax])
        S.op("dve", lambda: nc.vector.tensor_scalar(out=cmax[:], in0=cmax[:], scalar1=-16.0, scalar2=None, op0=ALU.mult), reads=[cmax], writes=[cmax])
        trel_sb = f32t("trel_sb", nblk); sem_t = S.dsem("semt")
        S.dma("sp", trel_sb[:], trel[:, :], writes=[trel_sb], sem=sem_t)

        qst = S.sb("qst", [128, 8, 128], F32); qb16 = S.sb("qb16", [128, 8, 128], BF16); sem_q = S.dsem("semq")
        iqst = S.sb("iqst", [64, 16, 128], F32); iwst = S.sb("iwst", [64, 16, 128], F32); sem_iq = S.dsem("semiq"); sem_iw = S.dsem("semiw")
        iqs = S.sb("iqs", [64, 16, 128], BF16)
        iwt = f32t("iwt", 16); sgn = f32t("sgn", 16); sem_it = S.dsem("semit")
        qlat = [S.sb(f"qlat{rc}", [128, 1024], BF16) for rc in range(2)]
        qsq = bft("qsq", 1024); negm = S.sb("negm", [1, 1024], BF16); nrm = S.sb("nrm", [1, 1024], F32)
        Rb = [bft(f"Rb{i}") for i in range(4)]
        dg = S.sb("dg", [128, 16, 128], BF16)
        junk = bft("junk", 2048)
        lo = f32t("lo", 1); hi = f32t("hi", 1); mid = f32t("mid", 1); cnt = f32t("cnt", 1); prd = f32t("prd", 1); dd = f32t("dd", 1)
        pen = f32t("pen", 1024)
        maskb = bft("maskb"); maskT = S.sb("maskT", [128, 4, 128], BF16)
        NPT = 3
        PT = [bft(f"PT{i}") for i in range(NPT)]; PTm = [bft(f"PTm{i}") for i in range(NPT)]
        den_sb = f32t("den_sb", 8); rec = f32t("rec", 8)
        o_sb = S.sb("o_sb", [128, 8, 256], BF16); oT = [S.sb(f"oT{rc}", [128, 8, 128], BF16) for rc in range(2)]
        ao = [S.sb(f"ao{i}", [128, 4, 128], F32) for i in range(2)]; sem_ao = [S.dsem("semao0"), S.dsem("semao1")]
        onesrow = S.sb("onesrow", [1, 128], BF16)
        S.op("dve", lambda: nc.vector.memset(onesrow[:], 1.0), writes=[onesrow])
        IDX_SCALE = (64 ** -0.5) * (16 ** -0.5)
        rri = 0; pti = 0
        for j in range(nblk):
            nkt = 2 * (j + 1)
            nsb = 8 * (j + 1)
            S.dma("sp", qst[:], qT[j, :, :, :], writes=[qst], sem=sem_q)
            S.dma("sp", iqst[:], iqT[j, :, :, :], writes=[iqst], sem=sem_iq)
            S.dma("sp", iwst[:], iw_bc[j, :, :, :], writes=[iwst], sem=sem_iw)
            S.dma("sp", iwt[:], iw_tok[j, :, :], writes=[iwt], sem=sem_it)
            S.op("dve", lambda: nc.vector.tensor_copy(out=qb16[:], in_=qst[:]), reads=[qst], writes=[qb16])
            S.op("act", lambda: nc.scalar.activation(out=iwst[:], in_=iwst[:], func=AF.Abs), reads=[iwst], writes=[iwst])
            S.op("dve", lambda: nc.vector.scalar_tensor_tensor(out=iqs[:], in0=iqst[:], scalar=IDX_SCALE, in1=iwst[:], op0=ALU.mult, op1=ALU.mult),
                 reads=[iqst, iwst], writes=[iqs])
            S.op("act", lambda: nc.scalar.activation(out=sgn[:], in_=iwt[:], func=AF.Sign), reads=[iwt], writes=[sgn])
            for rc in range(2):
                for hg in range(2):
                    for hh in range(4):
                        h = hg * 4 + hh
                        S.mm(STp[hg], STp[hg][:, hh * 128:(hh + 1) * 128], wukb, wukb[:, h, rc * 128:(rc + 1) * 128], qb16, qb16[:, h, :], start=True, stop=True)
                    S.op("act", lambda: nc.scalar.activation(out=qlat[rc][:, hg * 512:(hg + 1) * 512], in_=STp[hg][:], func=AF.Identity, scale=128 ** -0.5),
                         reads=[STp[hg]], writes=[qlat[rc]])
            for hg in range(2):
                for rc in range(2):
                    S.op("dve", lambda: nc.vector.tensor_tensor(out=qsq[:, 0:512], in0=qlat[rc][:, hg * 512:(hg + 1) * 512], in1=qlat[rc][:, hg * 512:(hg + 1) * 512], op=ALU.mult),
                         reads=[qlat[rc]], writes=[qsq])
                    S.mm(STp[hg], STp[hg][:], ones, ones[:], qsq, qsq[:, 0:512], start=(rc == 0), stop=(rc == 1))
                S.op("act", lambda: nc.scalar.activation(out=nrm[:, hg * 512:(hg + 1) * 512], in_=STp[hg][0:1, :], func=AF.Sqrt), reads=[STp[hg]], writes=[nrm])
            S.op("dve", lambda: nc.vector.tensor_scalar(out=negm[:], in0=nrm[:], scalar1=cmax[0:1, 0:1], scalar2=None, op0=ALU.mult), reads=[nrm, cmax], writes=[negm])
            for h in range(16):
                S.op("dve", lambda: nc.vector.tensor_scalar(out=dg[:, h, :], in0=ident[:], scalar1=sgn[:, h:h + 1], scalar2=None, op0=ALU.mult),
                     reads=[ident, sgn], writes=[dg])
            scp = oacc[0]
            for kt in range(nkt):
                S.mm(STp[0], STp[0][:], iqs, iqs[:, 0, :], ikb, ikb[:, kt * 512:(kt + 1) * 512], start=True, stop=True)
                for h in range(16):
                    pb = STp[h % 2]
                    if h + 1 < 16:
                        pn = STp[(h + 1) % 2]
                        S.mm(pn, pn[:], iqs, iqs[:, h + 1, :], ikb, ikb[:, kt * 512:(kt + 1) * 512], start=True, stop=True)
                    R = Rb[rri % 4]; rri += 1
                    S.op("act", lambda: nc.scalar.activation(out=R[:], in_=pb[:], func=AF.Relu), reads=[pb], writes=[R])
                    S.mm(scp, scp[:], dg, dg[:, h, :], R, R[:], start=(h == 0), stop=(h == 15))
                S.op("dve", lambda: nc.vector.tensor_copy(out=sc[:, kt * 512:(kt + 1) * 512], in_=scp[:]), reads=[scp], writes=[sc])
            nk = 1024 * (j + 1)
            S.op("dve", lambda: nc.vector.tensor_reduce(out=hi[:], in_=sc[:, 0:nk], axis=AX.X, op=ALU.max, apply_absolute_value=True), reads=[sc], writes=[hi])
            S.op("dve", lambda: nc.vector.tensor_scalar(out=lo[:], in0=hi[:], scalar1=-1.0, scalar2=-1.0, op0=ALU.mult, op1=ALU.add), reads=[hi], writes=[lo])
            S.op("dve", lambda: nc.vector.tensor_scalar(out=hi[:], in0=hi[:], scalar1=1.0, scalar2=None, op0=ALU.add), reads=[hi], writes=[hi])
            S.op("dve", lambda: nc.vector.tensor_scalar(out=pen[:], in0=cst_sb[:, 128:1152], scalar1=trel_sb[:, j:j + 1], scalar2=-1e30, op0=ALU.is_gt, op1=ALU.mult),
                 reads=[cst_sb, trel_sb], writes=[pen])
            S.op("dve", lambda: nc.vector.tensor_tensor(out=sc[:, nk - 1024:nk], in0=sc[:, nk - 1024:nk], in1=pen[:], op=ALU.add), reads=[sc, pen], writes=[sc])
            for itn in range(NBIS):
                S.op("dve", lambda: nc.vector.tensor_tensor(out=mid[:], in0=lo[:], in1=hi[:], op=ALU.add), reads=[lo, hi], writes=[mid])
                S.op("dve", lambda: nc.vector.tensor_scalar(out=mid[:], in0=mid[:], scalar1=0.5, scalar2=None, op0=ALU.mult), reads=[mid], writes=[mid])
                first = True
                for c0 in range(0, nk, 2048):
                    w = min(2048, nk - c0)
                    if first:
                        S.op("dve", lambda: nc.vector.tensor_scalar(out=junk[:, 0:w], in0=sc[:, c0:c0 + w], scalar1=mid[:, 0:1], scalar2=None, op0=ALU.is_ge,
                                                                    op1=ALU.add, accum_out=cnt[:, 0:1]), reads=[sc, mid], writes=[junk, cnt])
                    else:
                        S.op("dve", lambda: nc.vector.tensor_scalar(out=junk[:, 0:w], in0=sc[:, c0:c0 + w], scalar1=mid[:, 0:1], scalar2=cnt[:, 0:1], op0=ALU.is_ge,
                                                                    op1=ALU.add, accum_out=cnt[:, 0:1]), reads=[sc, mid, cnt], writes=[junk, cnt])
                    first = False
                S.op("dve", lambda: nc.vector.tensor_scalar(out=prd[:], in0=cnt[:], scalar1=KSEL - 0.5, scalar2=None, op0=ALU.is_ge), reads=[cnt], writes=[prd])
                S.op("dve", lambda: nc.vector.tensor_tensor(out=dd[:], in0=mid[:], in1=lo[:], op=ALU.subtract), reads=[mid, lo], writes=[dd])
                S.op("dve", lambda: nc.vector.scalar_tensor_tensor(out=lo[:], in0=dd[:], scalar=prd[:, 0:1], in1=lo[:], op0=ALU.mult, op1=ALU.add),
                     reads=[dd, prd, lo], writes=[lo])
                S.op("dve", lambda: nc.vector.tensor_tensor(out=dd[:], in0=hi[:], in1=mid[:], op=ALU.subtract), reads=[hi, mid], writes=[dd])
                S.op("dve", lambda: nc.vector.scalar_tensor_tensor(out=hi[:], in0=dd[:], scalar=prd[:, 0:1], in1=mid[:], op0=ALU.mult, op1=ALU.add),
                     reads=[dd, prd, mid], writes=[hi])
            started = [False] * 4
            den_started = False
            for sbk in range(nsb + 1):
                rows = 16 if sbk == 0 else 128
                kb = sbk - 1
                if sbk >= 1 and kb % 4 == 0:
                    ktile = kb // 4
                    S.op("dve", lambda: nc.vector.tensor_scalar(out=maskb[:], in0=sc[:, ktile * 512:(ktile + 1) * 512], scalar1=lo[:, 0:1], scalar2=None, op0=ALU.is_ge),
                         reads=[sc, lo], writes=[maskb])
                    for q4 in range(4):
                        S.op("pe", lambda: nc.tensor.transpose(out=tpb[:, q4 * 128:(q4 + 1) * 128], in_=maskb[:, q4 * 128:(q4 + 1) * 128], identity=ident[:]),
                             reads=[maskb, ident], writes=[tpb])
                    S.op("act", lambda: nc.scalar.copy(out=maskT[:, :, :], in_=tpb[:, 0:512]), reads=[tpb], writes=[maskT])
                for hg in range(2):
                    stp = STp[hg]
                    for rc in range(2):
                        S.mm(stp, stp[0:rows, :], cT[rc], cT[rc][:, sbk * 128:sbk * 128 + rows], qlat[rc], qlat[rc][:, hg * 512:(hg + 1) * 512],
                             start=(rc == 0), stop=False)
                    S.mm(stp, stp[0:rows, :], onesrow, onesrow[0:1, 0:rows], negm, negm[0:1, hg * 512:(hg + 1) * 512], start=False, stop=True)
                    P = PT[pti % NPT]; Pm = PTm[pti % NPT]; pti += 1
                    S.op("act", lambda: nc.scalar.activation(out=P[0:rows, :], in_=stp[0:rows, :], func=AF.Exp), reads=[stp], writes=[P])
                    if sbk == 0:
                        Pm = P
                    else:
                        q4 = kb % 4
                        S.op("dve", lambda: nc.vector.tensor_tensor(out=Pm[:].rearrange("p (h t) -> p h t", h=4), in0=P[:].rearrange("p (h t) -> p h t", h=4),
                                                                     in1=maskT[:, q4:q4 + 1, :].to_broadcast([128, 4, 128]), op=ALU.mult),
                             reads=[P, maskT], writes=[Pm])
                    for hh in range(4):
                        h = hg * 4 + hh
                        bank = oacc[h // 2]
                        S.mm(bank, bank[:, (h % 2) * 256:(h % 2 + 1) * 256], Pm, Pm[0:rows, hh * 128:(hh + 1) * 128], ctok, ctok[0:rows, sbk, :],
                             start=(not started[h // 2]), stop=(sbk == nsb), skip_group_check=True)
                        started[h // 2] = True
                        S.mm(den, den[:, h:h + 1], Pm, Pm[0:rows, hh * 128:(hh + 1) * 128], ones, ones[0:rows, 0:1],
                             start=(not den_started), stop=(sbk == nsb), skip_group_check=True)
                        den_started = True
            S.op("dve", lambda: nc.vector.tensor_copy(out=den_sb[:], in_=den[:, 0:8]), reads=[den], writes=[den_sb])
            S.op("dve", lambda: nc.vector.reciprocal(out=rec[:], in_=den_sb[:]), reads=[den_sb], writes=[rec])
            for h in range(8):
                bank = oacc[h // 2]
                S.op("dve", lambda: nc.vector.tensor_scalar(out=o_sb[:, h, :], in0=bank[:, (h % 2) * 256:(h % 2 + 1) * 256], scalar1=rec[:, h:h + 1], scalar2=None, op0=ALU.mult),
                     reads=[bank, rec], writes=[o_sb])
            for rc in range(2):
                for h in range(8):
                    S.op("pe", lambda: nc.tensor.transpose(out=tpb[:, h * 128:(h + 1) * 128], in_=o_sb[:, h, rc * 128:(rc + 1) * 128], identity=ident[:]),
                         reads=[o_sb, ident], writes=[tpb])
                S.op("act", lambda: nc.scalar.copy(out=oT[rc][:, :, :], in_=tpb[:, :]), reads=[tpb], writes=[oT[rc]])
            for hg in range(2):
                stp = STp[hg]
                for hh in range(4):
                    h = hg * 4 + hh
                    for rc in range(2):
                        S.mm(stp, stp[:, hh * 128:(hh + 1) * 128], wuvb, wuvb[:, rc, h, :], oT[rc], oT[rc][:, h, :], start=(rc == 0), stop=(rc == 1))
                a = ao[hg]
                S.op("act", lambda: nc.scalar.copy(out=a[:, :, :], in_=stp[:]), reads=[stp], writes=[a])
                S.dma("sp", att_out[j, :, hg * 4:(hg + 1) * 4, :], a[:, :, :], reads=[a], sem=sem_ao[hg])
        S.finish(ao)
        print("attn instructions", S.ninst)
    return nc


def attn_consts():
    c = np.zeros((128, 1152), np.float32)
    c[:, 0:128] = np.eye(128, dtype=np.float32)
    c[:, 128:1152] = np.arange(1024, dtype=np.float32)[None, :]
    return c


def attn_inputs(core, nblk, q, ckv, iq, ik, iw, kvg, w_uk, w_uv):
    NKEY = 1024 * nblk
    NSB = NKEY // 128
    ckv_tok = np.zeros((NSB + 1, 128, 256), np.float32)
    ckv_tok[0, :NMETA] = ckv[:NMETA]
    ckv_tok[1:] = ckv[NMETA:NMETA + NKEY].reshape(NSB, 128, 256)
    d = {"ckv_tok": ckv_tok,
         "ikT": np.ascontiguousarray(ik[NMETA:NMETA + NKEY].T),
         "gkv_bc": np.ascontiguousarray(np.broadcast_to(kvg[None, :], (128, 256))).astype(np.float32),
         "wukT": np.ascontiguousarray(w_uk.transpose(2, 1, 0)),
         "wuv": np.ascontiguousarray(w_uv.reshape(2, 128, 8, 128)),
         "cst": attn_consts()}
    qT = np.zeros((nblk, 128, 8, 128), np.float32)
    iqT = np.zeros((nblk, 64, 16, 128), np.float32)
    iwb = np.zeros((nblk, 64, 16, 128), np.float32)
    iwt = np.zeros((nblk, 128, 16), np.float32)
    trel = np.zeros((128, nblk), np.float32)
    toks = []
    for j in range(nblk):
        qb = 8 * j + core
        tok = NMETA + 128 * qb + np.arange(128)
        toks.append(tok)
        qT[j] = q[tok].reshape(128, 8, 128).transpose(2, 1, 0)
        iqT[j] = iq[tok].reshape(128, 16, 64).transpose(2, 1, 0)
        iwb[j] = np.broadcast_to(iw[tok].T[None, :, :], (64, 16, 128))
        iwt[j] = iw[tok]
        trel[:, j] = (128 * qb + np.arange(128)) - 1024 * j
    d.update({"qT": qT, "iqT": iqT, "iw_bc": iwb, "iw_tok": iwt, "trel": trel})
    return d, toks


def attn_unpack(att_out, toks, att_full):
    for j, tok in enumerate(toks):
        att_full[tok] = att_out[j].transpose(2, 1, 0).reshape(128, 1024)


def rglru_inputs(core, ntiles, rx, ry, conv_w, conv_b, w_a, b_a, w_i, b_i, lam):
    TPR = ntiles * 512
    T = rx.shape[0]
    sl = slice(core * 128, (core + 1) * 128)
    rxy = np.zeros((2, 128, TPR), np.float32)
    n = min(T, TPR)
    rxy[0, :, :n] = rx[:n, sl].T
    rxy[1, :, :n] = ry[:n, sl].T
    rgp = np.zeros((128, 8), np.float32)
    for jt in range(4):
        rgp[:, jt] = conv_w[jt, sl]
    rgp[:, 4] = conv_b[sl]; rgp[:, 5] = b_a[sl]; rgp[:, 6] = b_i[sl]; rgp[:, 7] = lam[sl]
    rgw = np.ascontiguousarray(np.stack([w_a[core], w_i[core]], 0))
    return {"rxy": rxy, "rgp": rgp, "rgw": rgw}


def tok_idx_l0(c):
    r0 = NMETA + c * 1024
    return np.concatenate([np.arange(r0, r0 + 512), np.arange(0, NMETA), np.arange(r0 + 512, r0 + 1024)])


def run_inproj(h_tok, w_in, gain):
    nmb = w_in.shape[0]
    M = nmb * 128
    key = ("ip", nmb)
    if key not in _NC_CACHE:
        _NC_CACHE[key] = build_rowlocal(0, inproj_mb=nmb)
    nc = _NC_CACHE[key]
    gains = np.ascontiguousarray(np.stack([gvec(gain)] * 4, axis=1)).astype(np.float32)
    wt = w_in
    in_maps = []
    for c in range(NCORES):
        in_maps.append({"hT_in": fm(h_tok[tok_idx_l0(c)]), "gains": gains, "cd_t": wt})
    res = run_bass_kernel_spmd(nc, in_maps, core_ids=list(range(NCORES)))
    proj = np.zeros((h_tok.shape[0], M), np.float32)
    for c in range(NCORES):
        p = np.asarray(res.results[c]["proj_out"])
        proj[tok_idx_l0(c)] = p.transpose(2, 0, 1).reshape(p.shape[2], nmb * 128)
    return proj


def run_mix0(proj0, lb_logits, out_norm_g, sconv_w):
    if "m0" not in _NC_CACHE:
        _NC_CACHE["m0"] = build_mix0(17)
    nc = _NC_CACHE["m0"]
    T = proj0.shape[0]
    P = np.zeros((TP0, 7168), np.float32)
    P[48:48 + T] = proj0
    cst = mix0_consts()
    in_maps = []
    for c in range(NCORES):
        pj = np.ascontiguousarray(np.stack([P[:, i * 1024 + c * 128: i * 1024 + (c + 1) * 128].T for i in range(7)], 0))
        par = np.zeros((128, 8), np.float32)
        par[:, 0] = lb_logits[0, c * 128:(c + 1) * 128]
        par[:, 1] = lb_logits[1, c * 128:(c + 1) * 128]
        par[:, 2] = out_norm_g
        for jt in range(3):
            par[:, 3 + jt] = sconv_w[jt, c * 128:(c + 1) * 128]
        in_maps.append({"pj": pj, "par": par, "cst": cst})
    res = run_bass_kernel_spmd(nc, in_maps, core_ids=list(range(NCORES)))
    mix = np.zeros((T, 2048), np.float32)
    for c in range(NCORES):
        mo = np.asarray(res.results[c]["mo"])
        mix[:, c * 128:(c + 1) * 128] = mo[0].T[48:48 + T]
        mix[:, 1024 + c * 128:1024 + (c + 1) * 128] = mo[1].T[48:48 + T]
    return mix


def run_mix1(proj1, inp):
    T = proj1.shape[0]
    sizes = [1024, 1024, 1024, 256, 1024, 64, 16]
    rx, ry, q, ckv, iq, ik, iw = np.split(proj1, np.cumsum(sizes)[:-1], axis=-1)
    if "rg" not in _NC_CACHE:
        _NC_CACHE["rg"] = build_rglru(17)
    nc = _NC_CACHE["rg"]
    in_maps = [rglru_inputs(c, 17, rx, ry, inp["rg_conv_w"][0], inp["rg_conv_b"][0], inp["rg_w_a"][0], inp["rg_b_a"][0],
                            inp["rg_w_i"][0], inp["rg_b_i"][0], inp["rg_lambda"][0]) for c in range(NCORES)]
    res = run_bass_kernel_spmd(nc, in_maps, core_ids=list(range(NCORES)))
    mix = np.zeros((T, 2048), np.float32)
    for c in range(NCORES):
        mix[:, c * 128:(c + 1) * 128] = np.asarray(res.results[c]["hc_out"]).T[:T]
    if "at" not in _NC_CACHE:
        _NC_CACHE["at"] = build_attn(8)
    nc = _NC_CACHE["at"]
    in_maps = []
    toks_all = []
    for c in range(NCORES):
        d, toks = attn_inputs(c, 8, q, ckv, iq, ik, iw, inp["mla_kv_norm"][0], inp["mla_w_uk"][0], inp["mla_w_uv"][0])
        in_maps.append(d)
        toks_all.append(toks)
    res = run_bass_kernel_spmd(nc, in_maps, core_ids=list(range(NCORES)))
    att = np.zeros((T, 1024), np.float32)
    for c in range(NCORES):
        attn_unpack(np.asarray(res.results[c]["att_out"]), toks_all[c], att)
    mix[:, 1024:] = att
    return mix


WC_CH = 4096


def build_wcast(nch):
    ncol = nch * WC_CH
    nc = bass.Bass("TRN2", target_bir_lowering=False)
    x = nc.dram_tensor("wx", [128, ncol], F32, kind="ExternalInput").ap()
    y = nc.dram_tensor("wy", [128, ncol], BF16, kind="ExternalOutput").ap()
    with ExitStack() as st:
        S = Sched(nc, st)
        NB = 4
        ib = [S.sb(f"ib{i}", [128, WC_CH], F32) for i in range(NB)]
        ob = [S.sb(f"ob{i}", [128, WC_CH], BF16) for i in range(NB)]
        isem = [S.dsem(f"isem{i}") for i in range(NB)]
        osem = [S.dsem(f"osem{i}") for i in range(NB)]
        LOOK = 2
        for i in range(min(LOOK, nch)):
            S.dma("sp", ib[i % NB][:], x[:, i * WC_CH:(i + 1) * WC_CH], writes=[ib[i % NB]], sem=isem[i % NB])
        for i in range(nch):
            if i + LOOK < nch:
                k = (i + LOOK) % NB
                S.dma("sp", ib[k][:], x[:, (i + LOOK) * WC_CH:(i + LOOK + 1) * WC_CH], writes=[ib[k]], sem=isem[k])
            b = i % NB
            h = WC_CH // 2
            S.op("act", lambda: nc.scalar.copy(out=ob[b][:, 0:h], in_=ib[b][:, 0:h]), reads=[ib[b]], writes=[ob[b]])
            S.op("dve", lambda: nc.vector.tensor_copy(out=ob[b][:, h:WC_CH], in_=ib[b][:, h:WC_CH]), reads=[ib[b]], writes=[ob[b]])
            S.dma("sp", y[:, i * WC_CH:(i + 1) * WC_CH], ob[b][:], reads=[ob[b]], sem=osem[b])
        S.finish(ob)
    return nc


def device_cast_bf16(arrs):
    flat = np.concatenate([a.reshape(-1) for a in arrs])
    n = flat.size
    per = NCORES * 128 * WC_CH
    nch = -(-n // per)
    pad = nch * per - n
    if pad:
        flat = np.concatenate([flat, np.zeros(pad, np.float32)])
    parts = flat.reshape(NCORES, 128, nch * WC_CH)
    key = ("wc", nch)
    if key not in _NC_CACHE:
        _NC_CACHE[key] = build_wcast(nch)
    res = run_bass_kernel_spmd(_NC_CACHE[key], [{"wx": np.ascontiguousarray(parts[c])} for c in range(NCORES)], core_ids=list(range(NCORES)))
    out = np.concatenate([np.asarray(res.results[c]["wy"]).reshape(-1) for c in range(NCORES)])[:n]
    outs = []
    o = 0
    for a in arrs:
        outs.append(out[o:o + a.size].reshape(a.shape))
        o += a.size
    return outs


def kernel(**inp):
    inp = {k: np.asarray(v) for k, v in inp.items()}
    x = inp["x"]
    h0 = np.concatenate([inp["meta_tokens"].astype(np.float32), x[0]], axis=0)
    names = [("ab_w_in", 0), ("ab_w_out", 0), ("ffn_w1", 0), ("ffn_w3", 0), ("ffn_w2", 0), ("cd_w_in", 0),
             ("cd_w_out", 0), ("ffn_w1", 1), ("ffn_w3", 1), ("ffn_w2", 1)]
    tiled = [tile_w(inp[n][l]) for n, l in names]
    wb = dict(zip(names, device_cast_bf16(tiled)))
    proj0 = run_inproj(h0, wb[("ab_w_in", 0)], inp["ln_mix_pre"][0])
    mix0 = run_mix0(proj0, inp["hgrn_lb_logits"], inp["hgrn_out_norm"][0], inp["sconv_w"][0])
    h1, proj1 = run_rowlocal(0, h0, mix0, wb[("ab_w_out", 0)], wb[("ffn_w1", 0)], wb[("ffn_w3", 0)], wb[("ffn_w2", 0)],
                             [inp["ln_mix_post"][0], inp["ln_ffn_pre"][0], inp["ln_ffn_post"][0], inp["ln_mix_pre"][1]],
                             wb[("cd_w_in", 0)])
    mix1 = run_mix1(proj1, inp)
    h2, _ = run_rowlocal(1, h1[NMETA:], mix1[NMETA:], wb[("cd_w_out", 0)], wb[("ffn_w1", 1)], wb[("ffn_w3", 1)], wb[("ffn_w2", 1)],
                         [inp["ln_mix_post"][1], inp["ln_ffn_pre"][1], inp["ln_ffn_post"][1], inp["ln_mix_pre"][1]])
    return h2[None].astype(np.float32)
```

```python
import numpy as np
from contextlib import ExitStack
import concourse.bass as bass
import concourse.mybir as mybir
from concourse.bass_utils import run_bass_kernel_spmd

F32 = mybir.dt.float32
BF16 = mybir.dt.bfloat16
AF = mybir.ActivationFunctionType
ALU = mybir.AluOpType
AX = mybir.AxisListType

D = 2048
DFF = 5632
NMETA = 16
SEQ = 8192
EPS = 1e-6
NCORES = 8


class T:
    def __init__(self, ap, name=""):
        self.ap = ap
        self.name = name
        self.w = None
        self.r = []

    def __getitem__(self, k):
        return self.ap[k]


class View(T):
    pass


class Sched:
    def __init__(self, nc, stack):
        self.nc = nc
        self.stack = stack
        self.eng = {"pe": nc.tensor, "act": nc.scalar, "dve": nc.vector, "pool": nc.gpsimd, "sp": nc.sync}
        self.sem = {k: stack.enter_context(nc.semaphore("s_" + k)) for k in self.eng}
        self.cnt = {k: 0 for k in self.eng}
        self.seen = {k: {} for k in self.eng}
        self.nsem = 0
        self.ninst = 0

    def sb(self, name, shape, dt):
        t = self.stack.enter_context(self.nc.sbuf_tensor(name, shape, dt))
        return T(t, name)

    def ps(self, name, shape=(128, 512), dt=F32):
        t = self.stack.enter_context(self.nc.psum_tensor(name, list(shape), dt))
        return T(t, name)

    def dsem(self, name):
        self.nsem += 1
        return [self.stack.enter_context(self.nc.semaphore(name)), 0]

    def _wait(self, e, tok):
        if tok is None:
            return
        sem, val, owner = tok
        if owner == e and e == "pe":
            return
        seen = self.seen[e]
        key = id(sem)
        if seen.get(key, 0) >= val:
            return
        self.eng[e].wait_ge(sem, val)
        seen[key] = val

    def deps(self, e, reads, writes):
        toks = []
        for t in reads:
            toks.append(t.w)
        for t in writes:
            toks.append(t.w)
            toks.extend(t.r)
        best = {}
        for tok in toks:
            if tok is None:
                continue
            k = id(tok[0])
            if k not in best or best[k][1] < tok[1]:
                best[k] = tok
        for tok in best.values():
            self._wait(e, tok)

    def done(self, tok, reads, writes):
        for t in reads:
            t.r.append(tok)
            if len(t.r) > 64:
                t.r = t.r[-48:]
        for t in writes:
            t.w = tok
            t.r = []

    def op(self, e, fn, reads=(), writes=()):
        self.deps(e, reads, writes)
        ins = fn()
        self.cnt[e] += 1
        self.ninst += 1
        ins.then_inc(self.sem[e], 1)
        tok = (self.sem[e], self.cnt[e], e)
        self.done(tok, reads, writes)

    def dma(self, q, out, in_, reads=(), writes=(), sem=None, **kw):
        self.deps(q, reads, writes)
        ins = self.eng[q].dma_start(out=out, in_=in_, **kw)
        sem[1] += 16
        self.ninst += 1
        ins.then_inc(sem[0], 16)
        tok = (sem[0], sem[1], "dma")
        self.done(tok, reads, writes)

    def finish(self, tiles):
        self.deps("sp", [], tiles)

    def mm(self, out_t, out_ap, lhsT_t, lhsT_ap, rhs_t, rhs_ap, start, stop, **kw):
        nc = self.nc
        self.op("pe", lambda: nc.tensor.matmul(out_ap, lhsT=lhsT_ap, rhs=rhs_ap, start=start, stop=stop, **kw),
                reads=[lhsT_t, rhs_t], writes=[out_t])


def trim_reads(t):
    pass


CD_M = 4432
CD_MB = 35


DBG = {'ss': True, 'norm': True, 'castpool': True}


def build_rowlocal(layer, stop=99, inproj_mb=None):
    with_cd = (layer == 0)
    CDMB = inproj_mb if inproj_mb else CD_MB
    if layer == 0:
        NT = 1040
        halves = [(0, 528, [(0, 512), (512, 16)]), (528, 512, [(0, 512)])]
    else:
        NT = 1024
        halves = [(0, 512, [(0, 512)]), (512, 512, [(0, 512)])]
    HN = 528
    nc = bass.Bass("TRN2", target_bir_lowering=False)
    hT_in = nc.dram_tensor("hT_in", [128, 16, NT], F32, kind="ExternalInput").ap()
    if not inproj_mb:
        mixT_in = nc.dram_tensor("mixT_in", [128, 16, NT], F32, kind="ExternalInput").ap()
        w_out_t = nc.dram_tensor("w_out_t", [16, 128, 16, 128], BF16, kind="ExternalInput").ap()
        w1_t = nc.dram_tensor("w1_t", [44, 128, 16, 128], BF16, kind="ExternalInput").ap()
        w3_t = nc.dram_tensor("w3_t", [44, 128, 16, 128], BF16, kind="ExternalInput").ap()
        w2_t = nc.dram_tensor("w2_t", [16, 128, 44, 128], BF16, kind="ExternalInput").ap()
        hT_out = nc.dram_tensor("hT_out", [128, 16, NT], F32, kind="ExternalOutput").ap()
    NG = 4
    gains = nc.dram_tensor("gains", [128, NG, 16], F32, kind="ExternalInput").ap()
    if with_cd:
        cd_t = nc.dram_tensor("cd_t", [CDMB, 128, 16, 128], BF16, kind="ExternalInput").ap()
        proj_out = nc.dram_tensor("proj_out", [CDMB, 128, NT], F32, kind="ExternalOutput").ap()

    with ExitStack() as st:
        S = Sched(nc, st)
        hT = S.sb("hT", [128, 16, HN], F32)
        xb = S.sb("xb", [128, 16, HN], BF16)
        y = S.sb("y", [128, 16, HN], F32)
        m = S.sb("m", [128, 44, HN], BF16)
        g_sb = S.sb("g_sb", [128, NG, 16], F32)
        ones = S.sb("ones", [128, 128], BF16)
        rstd = S.sb("rstd", [128, HN], F32)
        lnt = S.sb("lnt", [128, HN], F32)
        NWB = 6
        wbf = [S.sb(f"wbf{i}", [128, 16, 128], BF16) for i in range(NWB)]
        wbf_sem = [S.dsem(f"wbfsem{i}") for i in range(NWB)]
        sq = [S.sb(f"sq{i}", [128, HN], BF16) for i in range(2)]
        tmp = [S.sb(f"tmp{i}", [128, HN], F32) for i in range(2)]
        ostg = [S.sb(f"ostg{i}", [128, HN], F32) for i in range(2)]
        ostg_sem = [S.dsem(f"ostgsem{i}") for i in range(2)]
        accb = [[S.ps(f"acc{w}{b}") for b in range(2)] for w in range(2)]
        small = [S.ps("small0"), S.ps("small1")]
        smallv = [[small[w] for b in range(2)] for w in range(2)]
        ssb = S.ps("ssb")
        sss = S.ps("sss")
        io_sem = [S.dsem("io0"), S.dsem("io1"), S.dsem("io2"), S.dsem("io3")]

        S.dma("sp", g_sb[:], gains[:, :, :], writes=[g_sb], sem=io_sem[2])
        S.op("dve", lambda: nc.vector.memset(ones[:], 1.0), writes=[ones])

        state = {"fill": 0, "wb": 0, "blk": 0, "sq": 0, "tmp": 0, "ostg": 0, "cast": 0}

        def load_w(src_ap, kn):
            j = state["wb"] % NWB
            state["wb"] += 1
            S.dma("sp", wbf[j][:, 0:kn, :], src_ap, writes=[wbf[j]], sem=wbf_sem[j])
            return wbf[j]

        def acc_aps(w, b, ntiles):
            res = []
            for (n0, nsz) in ntiles:
                if nsz == 512:
                    res.append((accb[w][b], accb[w][b][:, 0:512]))
                else:
                    off = (w * 2 + b) * 16
                    res.append((smallv[w][b], smallv[w][b][:, off:off + nsz]))
            return res

        def linear(x_t, KC, wsrcs, nmb, ntiles, consume, fills):
            seq = [(mb, wi, fi) for mb in range(nmb) for wi in range(len(wsrcs)) for fi in range(len(fills))]
            loaded = {}
            nxt = [0]
            LOOK = 4

            def ensure(upto):
                while nxt[0] <= min(upto, len(seq) - 1):
                    mb_, wi_, fi_ = seq[nxt[0]]
                    k0_, kn_ = fills[fi_]
                    loaded[nxt[0]] = load_w(wsrcs[wi_][mb_, :, k0_:k0_ + kn_, :], kn_)
                    nxt[0] += 1
            b = 0
            accs = []
            aps = None
            for i, (mb, wi, fi) in enumerate(seq):
                ensure(i + LOOK)
                if wi == 0 and fi == 0:
                    b = state["blk"] % 2
                    state["blk"] += 1
                    accs = []
                if fi == 0:
                    aps = acc_aps(wi, b, ntiles)
                wt = loaded.pop(i)
                k0, kn = fills[fi]
                for kk in range(kn):
                    kc = k0 + kk
                    for ti, (n0, nsz) in enumerate(ntiles):
                        at, aap = aps[ti]
                        S.mm(at, aap, wt, wt[:, kk, :], x_t, x_t[:, kc, n0:n0 + nsz],
                             start=(kc == 0), stop=(kc == KC - 1))
                if fi == len(fills) - 1:
                    accs.append(aps)
                    if wi == len(wsrcs) - 1:
                        consume(mb, accs)

        def rstd_from(ss_list, ntiles):
            for (sst, ssap), (n0, nsz) in zip(ss_list, ntiles):
                S.op("act", lambda: nc.scalar.activation(out=lnt[:, n0:n0 + nsz], in_=ssap, func=AF.Ln,
                                                         bias=eps_t[:, 0:1], scale=1.0 / D),
                     reads=[sst, eps_t], writes=[lnt])
                S.op("act", lambda: nc.scalar.activation(out=rstd[:, n0:n0 + nsz], in_=lnt[:, n0:n0 + nsz],
                                                         func=AF.Exp, scale=-0.5),
                     reads=[lnt], writes=[rstd])

        eps_t = S.sb("eps_t", [128, 1], F32)
        S.op("dve", lambda: nc.vector.memset(eps_t[:], EPS), writes=[eps_t])

        def ss_aps(ntiles):
            res = []
            for (n0, nsz) in ntiles:
                if nsz == 512:
                    res.append((ssb, ssb[:, 0:512]))
                else:
                    res.append((sss, sss[:, 0:nsz]))
            return res

        def ss_accum(src_t, src_ap_fn, kc, ntiles, from_psum_aps=None):
            ssl = ss_aps(ntiles)
            for ti, (n0, nsz) in enumerate(ntiles):
                i = state["sq"] % 2
                state["sq"] += 1
                if from_psum_aps is not None:
                    st_, sap = from_psum_aps[ti]
                else:
                    st_, sap = src_t, src_ap_fn(n0, nsz)
                S.op("act", lambda: nc.scalar.activation(out=sq[i][:, 0:nsz], in_=sap, func=AF.Square),
                     reads=[st_], writes=[sq[i]])
                S.mm(ssl[ti][0], ssl[ti][1], ones, ones[:], sq[i], sq[i][:, 0:nsz], start=(kc == 0), stop=(kc == 15))

        def post_norm_residual(gidx, ntiles, hw):
            for kc in range(16):
                i = state["tmp"] % 2
                state["tmp"] += 1
                S.op("dve", lambda: nc.vector.scalar_tensor_tensor(
                    out=tmp[i][:, 0:hw], in0=y[:, kc, 0:hw], scalar=g_sb[:, gidx, kc:kc + 1], in1=rstd[:, 0:hw],
                    op0=ALU.mult, op1=ALU.mult), reads=[y, g_sb, rstd], writes=[tmp[i]])
                S.op("dve", lambda: nc.vector.tensor_tensor(out=hT[:, kc, 0:hw], in0=hT[:, kc, 0:hw],
                                                             in1=tmp[i][:, 0:hw], op=ALU.add),
                     reads=[tmp[i], hT], writes=[hT])

        def pre_norm(gidx, ntiles, hw):
            for kc in range(16):
                ss_accum(hT, lambda n0, nsz: hT[:, kc, n0:n0 + nsz], kc, ntiles)
            rstd_from(ss_aps(ntiles), ntiles)
            for kc in range(16):
                S.op("dve", lambda: nc.vector.scalar_tensor_tensor(
                    out=xb[:, kc, 0:hw], in0=hT[:, kc, 0:hw], scalar=g_sb[:, gidx, kc:kc + 1], in1=rstd[:, 0:hw],
                    op0=ALU.mult, op1=ALU.mult), reads=[hT, g_sb, rstd], writes=[xb])

        def consume_y(ntiles):
            def f(mb, accs):
                aps = accs[0]
                for ti, (n0, nsz) in enumerate(ntiles):
                    at, aap = aps[ti]
                    S.op("dve", lambda: nc.vector.tensor_copy(out=y[:, mb, n0:n0 + nsz], in_=aap),
                         reads=[at], writes=[y])
                if DBG['ss']:
                    ss_accum(y, lambda n0, nsz: y[:, mb, n0:n0 + nsz], mb, ntiles)
            return f

        for (h0, hw, ntiles) in halves:
            if inproj_mb:
                for k4 in range(0, 16, 2):
                    S.dma("sp", hT[:, k4:k4 + 2, 0:hw], hT_in[:, k4:k4 + 2, h0:h0 + hw], writes=[hT], sem=io_sem[0])
                pre_norm(0, ntiles, hw)

                def consume_ip(mb, accs):
                    i = state["ostg"] % 2
                    state["ostg"] += 1
                    for ti, (n0, nsz) in enumerate(ntiles):
                        at, aap = accs[0][ti]
                        S.op("dve", lambda: nc.vector.tensor_copy(out=ostg[i][:, n0:n0 + nsz], in_=aap),
                             reads=[at], writes=[ostg[i]])
                    S.dma("sp", proj_out[mb, :, h0:h0 + hw], ostg[i][:, 0:hw], reads=[ostg[i]], sem=ostg_sem[i])
                linear(xb, 16, [cd_t], CDMB, ntiles, consume_ip, [(0, 16)])
                continue
            for k4 in range(0, 16, 2):
                S.dma("sp", hT[:, k4:k4 + 2, 0:hw], hT_in[:, k4:k4 + 2, h0:h0 + hw], writes=[hT], sem=io_sem[0])
                S.dma("sp", y[:, k4:k4 + 2, 0:hw], mixT_in[:, k4:k4 + 2, h0:h0 + hw], writes=[y], sem=io_sem[1])
            for kc in range(16):
                eng = "dve"
                if eng == "dve":
                    S.op("dve", lambda: nc.vector.tensor_copy(out=xb[:, kc, 0:hw], in_=y[:, kc, 0:hw]),
                         reads=[y], writes=[xb])
                else:
                    S.op("pool", lambda: nc.gpsimd.tensor_copy(out=xb[:, kc, 0:hw], in_=y[:, kc, 0:hw]),
                         reads=[y], writes=[xb])
            if stop >= 1:
                linear(xb, 16, [w_out_t], 16, ntiles, consume_y(ntiles), [(0, 16)])
                if DBG['norm']:
                    rstd_from(ss_aps(ntiles), ntiles)
                    post_norm_residual(0, ntiles, hw)
                else:
                    for kc in range(16):
                        S.op("dve", lambda: nc.vector.tensor_copy(out=hT[:, kc, 0:hw], in_=y[:, kc, 0:hw]), reads=[y], writes=[hT])
            if stop == 0:
                for kc in range(16):
                    S.op("dve", lambda: nc.vector.tensor_copy(out=hT[:, kc, 0:hw], in_=y[:, kc, 0:hw]), reads=[y], writes=[hT])
            if stop <= 1:
                for k4 in range(0, 16, 2):
                    S.dma("sp", hT_out[:, k4:k4 + 2, h0:h0 + hw], hT[:, k4:k4 + 2, 0:hw], reads=[hT], sem=io_sem[3])
                continue
            pre_norm(1, ntiles, hw)

            def consume_ab(mb, accs):
                for ti, (n0, nsz) in enumerate(ntiles):
                    i = state["tmp"] % 2
                    state["tmp"] += 1
                    at, aap = accs[0][ti]
                    bt, bap = accs[1][ti]
                    S.op("act", lambda: nc.scalar.activation(out=tmp[i][:, 0:nsz], in_=aap, func=AF.Silu),
                         reads=[at], writes=[tmp[i]])
                    S.op("dve", lambda: nc.vector.tensor_tensor(out=m[:, mb, n0:n0 + nsz], in0=tmp[i][:, 0:nsz],
                                                                 in1=bap, op=ALU.mult),
                         reads=[tmp[i], bt], writes=[m])
            linear(xb, 16, [w1_t, w3_t], 44, ntiles, consume_ab, [(0, 16)])
            linear(m, 44, [w2_t], 16, ntiles, consume_y(ntiles), [(0, 16), (16, 16), (32, 12)])
            rstd_from(ss_aps(ntiles), ntiles)
            post_norm_residual(2, ntiles, hw)
            for k4 in range(0, 16, 2):
                S.dma("sp", hT_out[:, k4:k4 + 2, h0:h0 + hw], hT[:, k4:k4 + 2, 0:hw], reads=[hT], sem=io_sem[3])
            if with_cd:
                pre_norm(3, ntiles, hw)

                def consume_cd(mb, accs):
                    i = state["ostg"] % 2
                    state["ostg"] += 1
                    for ti, (n0, nsz) in enumerate(ntiles):
                        at, aap = accs[0][ti]
                        S.op("dve", lambda: nc.vector.tensor_copy(out=ostg[i][:, n0:n0 + nsz], in_=aap),
                             reads=[at], writes=[ostg[i]])
                    S.dma("sp", proj_out[mb, :, h0:h0 + hw], ostg[i][:, 0:hw], reads=[ostg[i]], sem=ostg_sem[i])
                linear(xb, 16, [cd_t], CDMB, ntiles, consume_cd, [(0, 16)])
        S.finish([hT, ostg[0], ostg[1]] if (with_cd or inproj_mb) else [hT])
        print("rowlocal layer", layer, "instructions", S.ninst)
    return nc


def tile_w(w, mw=128):
    K, M = w.shape
    Mp = -(-M // mw) * mw
    if Mp != M:
        w = np.concatenate([w, np.zeros((K, Mp - M), w.dtype)], axis=1)
    return np.ascontiguousarray(w.reshape(K // 128, 128, Mp // mw, mw).transpose(2, 1, 0, 3))


def fm(a):
    Tn, Fn = a.shape
    return np.ascontiguousarray(a.reshape(Tn, Fn // 128, 128).transpose(2, 1, 0))


def unfm(a):
    p, kc, Tn = a.shape
    return np.ascontiguousarray(a.transpose(2, 1, 0).reshape(Tn, kc * 128))


def gvec(g):
    return np.ascontiguousarray(g.reshape(16, 128).T)


_NC_CACHE = {}


def run_rowlocal(layer, h_tok, mix_tok, w_out, w1, w3, w2, gain_list, cd_w=None):
    if ("rl", layer) not in _NC_CACHE:
        _NC_CACHE[("rl", layer)] = build_rowlocal(layer)
    nc = _NC_CACHE[("rl", layer)]
    gains = np.ascontiguousarray(np.stack([gvec(g) for g in gain_list], axis=1)).astype(np.float32)
    common = {"w_out_t": w_out, "w1_t": w1, "w3_t": w3, "w2_t": w2, "gains": gains}
    if layer == 0:
        common["cd_t"] = cd_w
    in_maps = []
    idxs = []
    for c in range(NCORES):
        if layer == 0:
            r0 = NMETA + c * 1024
            idx = np.concatenate([np.arange(r0, r0 + 512), np.arange(0, NMETA), np.arange(r0 + 512, r0 + 1024)])
        else:
            idx = np.arange(c * 1024, (c + 1) * 1024)
        idxs.append(idx)
        d = dict(common)
        d["hT_in"] = fm(h_tok[idx])
        d["mixT_in"] = fm(mix_tok[idx])
        in_maps.append(d)
    res = run_bass_kernel_spmd(nc, in_maps, core_ids=list(range(NCORES)))
    h_out = np.zeros_like(h_tok)
    proj = np.zeros((h_tok.shape[0], CD_M), np.float32) if layer == 0 else None
    for c in range(NCORES):
        r = res.results[c]
        h_out[idxs[c]] = unfm(np.asarray(r["hT_out"]))
        if layer == 0:
            p = np.asarray(r["proj_out"])
            pt = p.transpose(2, 0, 1).reshape(p.shape[2], CD_MB * 128)[:, :CD_M]
            proj[idxs[c]] = pt
    return h_out, proj


TP0 = 8704
NCONST = 128 + 512 + 512


def mix0_consts():
    c = np.zeros((128, NCONST), np.float32)
    c[:, 0:128] = np.eye(128, dtype=np.float32)
    tri = (np.arange(64)[None, :] >= np.arange(64)[:, None]).astype(np.float32)
    c[0:64, 128:640] = np.tile(tri, (1, 8))
    r = np.ones(512, np.float32)
    r[0::64] = 0.0
    c[:, 640:1152] = r[None, :]
    return c


def build_mix0(ntile=17):
    TP = ntile * 512
    nc = bass.Bass("TRN2", target_bir_lowering=False)
    pj = nc.dram_tensor("pj", [7, 128, TP], F32, kind="ExternalInput").ap()
    par = nc.dram_tensor("par", [128, 8], F32, kind="ExternalInput").ap()
    cst = nc.dram_tensor("cst", [128, NCONST], F32, kind="ExternalInput").ap()
    mo = nc.dram_tensor("mo", [2, 128, TP], F32, kind="ExternalOutput").ap()
    with ExitStack() as st:
        S = Sched(nc, st)
        f32t = lambda n, w=512: S.sb(n, [128, w], F32)
        bft = lambda n, w=512: S.sb(n, [128, w], BF16)
        par_sb = f32t("par_sb", 8)
        cst_sb = f32t("cst_sb", NCONST)
        ident = bft("ident", 128)
        ones = bft("ones", 128)
        lb = f32t("lb", 1); oml = f32t("oml", 1); eps_t = f32t("eps_t", 1)
        inp = [[f32t(f"in{b}_{i}") for i in range(7)] for b in range(2)]
        in_sem = [[S.dsem(f"insem{b}_{i}") for i in range(7)] for b in range(2)]
        sig = f32t("sig"); f_ = f32t("f_"); logf = f32t("logf"); b_ = f32t("b_"); k_ = f32t("k_")
        eb = f32t("eb"); enb = f32t("enb"); e2 = f32t("e2")
        Qt = bft("Qt"); Kt = bft("Kt"); Kp = bft("Kp"); Vb = bft("Vb")
        Vtok = S.sb("Vtok", [64, 8, 128], BF16); Ktok = S.sb("Ktok", [64, 8, 128], BF16)
        attm = S.sb("attm", [64, 512], BF16)
        Sf = f32t("Sf", 128)
        NSB = 4
        Sb = [bft(f"Sb{i}", 128) for i in range(NSB)]
        o_sb = f32t("o_sb"); osq = bft("osq"); lnt = f32t("lnt"); rstd = f32t("rstd"); sg = f32t("sg")
        res = [f32t(f"res{i}") for i in range(2)]; res_sem = [S.dsem(f"ressem{i}") for i in range(2)]
        ub = [f32t(f"ub{i}", 514) for i in range(2)]
        t1 = f32t("t1")
        yb = [f32t(f"yb{i}") for i in range(2)]; yb_sem = [S.dsem(f"ybsem{i}") for i in range(2)]
        tpv = S.ps("tpv", (128, 1024), BF16); tpk = S.ps("tpk", (128, 1024), BF16)
        att = S.ps("att"); o_ps = S.ps("o_ps"); U = [S.ps("U0"), S.ps("U1")]; sso = S.ps("sso")
        csem = S.dsem("csem")
        csem2 = S.dsem("csem2")
        S.dma("sp", par_sb[:], par[:, :], writes=[par_sb], sem=csem)
        S.dma("sp", cst_sb[:], cst[:, :], writes=[cst_sb], sem=csem2)
        tri = View(cst_sb.ap, "tri"); rmask = View(cst_sb.ap, "rmask")
        S.op("dve", lambda: nc.vector.tensor_copy(out=ident[:], in_=cst_sb[:, 0:128]), reads=[cst_sb], writes=[ident])
        S.op("dve", lambda: nc.vector.memset(ones[:], 1.0), writes=[ones])
        S.op("dve", lambda: nc.vector.memset(eps_t[:], EPS), writes=[eps_t])
        S.op("dve", lambda: nc.vector.memset(Sf[:], 0.0), writes=[Sf])
        S.op("dve", lambda: nc.vector.memset(Sb[0][:], 0.0), writes=[Sb[0]])
        S.op("dve", lambda: nc.vector.memset(ub[0][:], 0.0), writes=[ub[0]])
        S.op("dve", lambda: nc.vector.tensor_tensor(out=oml[:], in0=par_sb[:, 0:1], in1=par_sb[:, 1:2], op=ALU.subtract),
             reads=[par_sb], writes=[oml])
        S.op("act", lambda: nc.scalar.activation(out=lb[:], in_=oml[:], func=AF.Sigmoid), reads=[oml], writes=[lb])
        S.op("dve", lambda: nc.vector.tensor_scalar(out=oml[:], in0=lb[:], scalar1=-1.0, scalar2=1.0, op0=ALU.mult, op1=ALU.add),
             reads=[lb], writes=[oml])
        sbi = 0
        for tt in range(ntile):
            c0 = tt * 512
            bi = tt % 2
            I = inp[bi]
            for i in range(7):
                S.dma("sp", I[i][:], pj[i, :, c0:c0 + 512], writes=[I[i]], sem=in_sem[bi][i])
            q, fl, v, g, sx, sbb, sc = I
            S.op("act", lambda: nc.scalar.activation(out=sig[:], in_=fl[:], func=AF.Sigmoid), reads=[fl], writes=[sig])
            S.op("dve", lambda: nc.vector.tensor_scalar(out=f_[:], in0=sig[:], scalar1=oml[:, 0:1], scalar2=lb[:, 0:1],
                                                        op0=ALU.mult, op1=ALU.add), reads=[sig, oml, lb], writes=[f_])
            S.op("act", lambda: nc.scalar.activation(out=logf[:], in_=f_[:], func=AF.Ln), reads=[f_], writes=[logf])
            S.op("dve", lambda: nc.vector.tensor_tensor_scan(out=b_[:], data0=cst_sb[:, 640:1152], data1=logf[:], initial=0.0,
                                                             op0=ALU.mult, op1=ALU.add), reads=[cst_sb, logf], writes=[b_])
            S.op("dve", lambda: nc.vector.tensor_scalar(out=k_[:], in0=f_[:], scalar1=-1.0, scalar2=1.0, op0=ALU.mult, op1=ALU.add),
                 reads=[f_], writes=[k_])
            S.op("act", lambda: nc.scalar.activation(out=eb[:], in_=b_[:], func=AF.Exp), reads=[b_], writes=[eb])
            S.op("act", lambda: nc.scalar.activation(out=enb[:], in_=b_[:], func=AF.Exp, scale=-1.0), reads=[b_], writes=[enb])
            for j in range(8):
                S.op("act", lambda: nc.scalar.activation(out=e2[:, j * 64:(j + 1) * 64], in_=b_[:, j * 64:(j + 1) * 64], func=AF.Exp,
                                                         scale=-1.0, bias=b_[:, j * 64 + 63:j * 64 + 64]), reads=[b_], writes=[e2])
            S.op("dve", lambda: nc.vector.tensor_tensor(out=Qt[:], in0=q[:], in1=eb[:], op=ALU.mult), reads=[q, eb], writes=[Qt])
            S.op("dve", lambda: nc.vector.tensor_tensor(out=Kt[:], in0=k_[:], in1=enb[:], op=ALU.mult), reads=[k_, enb], writes=[Kt])
            S.op("dve", lambda: nc.vector.tensor_tensor(out=Kp[:], in0=k_[:], in1=e2[:], op=ALU.mult), reads=[k_, e2], writes=[Kp])
            S.op("dve", lambda: nc.vector.tensor_copy(out=Vb[:], in_=v[:]), reads=[v], writes=[Vb])
            for j in range(8):
                S.op("pe", lambda: nc.tensor.transpose(out=tpv[0:64, j * 128:(j + 1) * 128], in_=Vb[:, j * 64:(j + 1) * 64], identity=ident[:]),
                     reads=[Vb, ident], writes=[tpv])
            for j in range(8):
                S.op("pe", lambda: nc.tensor.transpose(out=tpk[0:64, j * 128:(j + 1) * 128], in_=Kp[:, j * 64:(j + 1) * 64], identity=ident[:]),
                     reads=[Kp, ident], writes=[tpk])
            S.op("act", lambda: nc.scalar.copy(out=Vtok[:, :, :], in_=tpv[0:64, :]), reads=[tpv], writes=[Vtok])
            S.op("act", lambda: nc.scalar.copy(out=Ktok[:, :, :], in_=tpk[0:64, :]), reads=[tpk], writes=[Ktok])
            for j in range(8):
                S.mm(att, att[0:64, j * 64:(j + 1) * 64], Kt, Kt[:, j * 64:(j + 1) * 64], Qt, Qt[:, j * 64:(j + 1) * 64], start=True, stop=True)
            S.op("dve", lambda: nc.vector.tensor_tensor(out=attm[:], in0=att[0:64, :], in1=cst_sb[0:64, 128:640], op=ALU.mult),
                 reads=[att, cst_sb], writes=[attm])
            for j in range(8):
                Uj = U[j // 4]
                S.mm(Uj, Uj[:, (j % 4) * 128:(j % 4 + 1) * 128], Ktok, Ktok[:, j, :], Vtok, Vtok[:, j, :], start=True, stop=True)
            for j in range(8):
                cur = Sb[sbi % NSB]
                nxt = Sb[(sbi + 1) % NSB]
                sbi += 1
                S.mm(o_ps, o_ps[:, j * 64:(j + 1) * 64], Vtok, Vtok[:, j, :], attm, attm[:, j * 64:(j + 1) * 64], start=True, stop=False)
                S.mm(o_ps, o_ps[:, j * 64:(j + 1) * 64], cur, cur[:], Qt, Qt[:, j * 64:(j + 1) * 64], start=False, stop=True)
                Uj = U[j // 4]
                uap = Uj[:, (j % 4) * 128:(j % 4 + 1) * 128]
                dcol = eb[:, j * 64 + 63:j * 64 + 64]
                S.op("dve", lambda: nc.vector.scalar_tensor_tensor(out=nxt[:], in0=Sf[:], scalar=dcol, in1=uap, op0=ALU.mult, op1=ALU.add),
                     reads=[Sf, eb, Uj], writes=[nxt])
                S.op("dve", lambda: nc.vector.scalar_tensor_tensor(out=Sf[:], in0=Sf[:], scalar=dcol, in1=uap, op0=ALU.mult, op1=ALU.add),
                     reads=[Sf, eb, Uj], writes=[Sf])
            S.op("act", lambda: nc.scalar.copy(out=o_sb[:], in_=o_ps[:]), reads=[o_ps], writes=[o_sb])
            S.op("act", lambda: nc.scalar.activation(out=osq[:], in_=o_sb[:], func=AF.Square), reads=[o_sb], writes=[osq])
            S.mm(sso, sso[:], ones, ones[:], osq, osq[:], start=True, stop=True)
            S.op("act", lambda: nc.scalar.activation(out=lnt[:], in_=sso[:], func=AF.Ln, bias=eps_t[:, 0:1], scale=1.0 / 128),
                 reads=[sso, eps_t], writes=[lnt])
            S.op("act", lambda: nc.scalar.activation(out=rstd[:], in_=lnt[:], func=AF.Exp, scale=-0.5), reads=[lnt], writes=[rstd])
            S.op("act", lambda: nc.scalar.activation(out=sg[:], in_=g[:], func=AF.Silu), reads=[g], writes=[sg])
            r_ = res[bi]
            S.op("dve", lambda: nc.vector.scalar_tensor_tensor(out=r_[:], in0=o_sb[:], scalar=par_sb[:, 2:3], in1=rstd[:], op0=ALU.mult, op1=ALU.mult),
                 reads=[o_sb, par_sb, rstd], writes=[r_])
            S.op("dve", lambda: nc.vector.tensor_tensor(out=r_[:], in0=r_[:], in1=sg[:], op=ALU.mult), reads=[r_, sg], writes=[r_])
            S.dma("sp", mo[0, :, c0:c0 + 512], r_[:], reads=[r_], sem=res_sem[bi])
            u = ub[bi]; un = ub[1 - bi]
            S.op("dve", lambda: nc.vector.tensor_tensor(out=u[:, 2:514], in0=sc[:], in1=sx[:], op=ALU.mult), reads=[sc, sx], writes=[u])
            S.op("dve", lambda: nc.vector.tensor_scalar(out=t1[:], in0=u[:, 2:514], scalar1=par_sb[:, 5:6], scalar2=None, op0=ALU.mult),
                 reads=[u, par_sb], writes=[t1])
            S.op("dve", lambda: nc.vector.scalar_tensor_tensor(out=t1[:], in0=u[:, 1:513], scalar=par_sb[:, 4:5], in1=t1[:], op0=ALU.mult, op1=ALU.add),
                 reads=[u, par_sb, t1], writes=[t1])
            S.op("dve", lambda: nc.vector.scalar_tensor_tensor(out=t1[:], in0=u[:, 0:512], scalar=par_sb[:, 3:4], in1=t1[:], op0=ALU.mult, op1=ALU.add),
                 reads=[u, par_sb, t1], writes=[t1])
            y_ = yb[bi]
            S.op("dve", lambda: nc.vector.tensor_tensor(out=y_[:], in0=t1[:], in1=sbb[:], op=ALU.mult), reads=[t1, sbb], writes=[y_])
            S.op("dve", lambda: nc.vector.tensor_copy(out=un[:, 0:2], in_=u[:, 512:514]), reads=[u], writes=[un])
            S.dma("sp", mo[1, :, c0:c0 + 512], y_[:], reads=[y_], sem=yb_sem[bi])
        S.finish(res + yb)
        print("mix0 instructions", S.ninst)
    return nc


KSEL = 256
NBIS = 13


def build_rglru(rg_tiles=17):
    TPR = rg_tiles * 512
    nc = bass.Bass("TRN2", target_bir_lowering=False)
    rxy = nc.dram_tensor("rxy", [2, 128, TPR], F32, kind="ExternalInput").ap()
    rgp = nc.dram_tensor("rgp", [128, 8], F32, kind="ExternalInput").ap()
    rgw = nc.dram_tensor("rgw", [2, 128, 128], F32, kind="ExternalInput").ap()
    hc_out = nc.dram_tensor("hc_out", [128, TPR], F32, kind="ExternalOutput").ap()
    with ExitStack() as st:
        S = Sched(nc, st)
        f32t = lambda n, w=512, p=128: S.sb(n, [p, w], F32)
        bft = lambda n, w=512, p=128: S.sb(n, [p, w], BF16)
        one_t = f32t("one_t", 1)
        S.op("dve", lambda: nc.vector.memset(one_t[:], 1.0), writes=[one_t])
        STp = [S.ps("ST0"), S.ps("ST1")]
        rgp_sb = f32t("rgp_sb", 8); rgw_f = S.sb("rgw_f", [128, 2, 128], F32); rgw_b = S.sb("rgw_b", [128, 2, 128], BF16)
        sem_r = [S.dsem("semr0"), S.dsem("semr1")]
        S.dma("sp", rgp_sb[:], rgp[:, :], writes=[rgp_sb], sem=sem_r[0])
        for i in range(2):
            S.dma("sp", rgw_f[:, i, :], rgw[i, :, :], writes=[rgw_f], sem=sem_r[1])
        S.op("dve", lambda: nc.vector.tensor_copy(out=rgw_b[:], in_=rgw_f[:]), reads=[rgw_f], writes=[rgw_b])
        c8 = f32t("c8", 1); c16 = f32t("c16", 1); tsm = f32t("tsm", 1)
        S.op("act", lambda: nc.scalar.activation(out=tsm[:], in_=rgp_sb[:, 7:8], func=AF.Exp, scale=-1.0), reads=[rgp_sb], writes=[tsm])
        S.op("act", lambda: nc.scalar.activation(out=tsm[:], in_=tsm[:], func=AF.Ln, bias=one_t[:, 0:1], scale=1.0), reads=[tsm, one_t], writes=[tsm])
        S.op("dve", lambda: nc.vector.tensor_scalar(out=c8[:], in0=tsm[:], scalar1=-8.0, scalar2=None, op0=ALU.mult), reads=[tsm], writes=[c8])
        S.op("dve", lambda: nc.vector.tensor_scalar(out=c16[:], in0=tsm[:], scalar1=-16.0, scalar2=None, op0=ALU.mult), reads=[tsm], writes=[c16])
        rxb = [f32t(f"rxb{i}", 515) for i in range(2)]
        ryb = [f32t(f"ryb{i}") for i in range(2)]
        sem_x = [S.dsem("semx0"), S.dsem("semx1")]; sem_y = [S.dsem("semy0"), S.dsem("semy1")]
        u_ = f32t("u_"); ub16 = bft("ub16"); r_ = f32t("r_"); ig = f32t("ig"); a_ = f32t("a_"); a2 = f32t("a2"); gx = f32t("gx")
        xin = f32t("xin"); hst = [f32t(f"hst{i}") for i in range(2)]; gl = f32t("gl"); gl2 = f32t("gl2")
        hco = [f32t(f"hco{i}") for i in range(2)]; sem_h = [S.dsem("semh0"), S.dsem("semh1")]
        S.op("dve", lambda: nc.vector.memset(rxb[0][:], 0.0), writes=[rxb[0]])
        S.op("dve", lambda: nc.vector.memset(hst[1][:], 0.0), writes=[hst[1]])
        for tt in range(rg_tiles):
            c0 = tt * 512; bi = tt % 2
            xb_ = rxb[bi]; xn = rxb[1 - bi]; yb_ = ryb[bi]
            S.dma("sp", xb_[:, 3:515], rxy[0, :, c0:c0 + 512], writes=[xb_], sem=sem_x[bi])
            S.dma("sp", yb_[:], rxy[1, :, c0:c0 + 512], writes=[yb_], sem=sem_y[bi])
            S.op("dve", lambda: nc.vector.tensor_scalar(out=u_[:], in0=xb_[:, 3:515], scalar1=rgp_sb[:, 3:4], scalar2=rgp_sb[:, 4:5],
                                                        op0=ALU.mult, op1=ALU.add), reads=[xb_, rgp_sb], writes=[u_])
            for jtap in range(3):
                S.op("dve", lambda: nc.vector.scalar_tensor_tensor(out=u_[:], in0=xb_[:, jtap:jtap + 512], scalar=rgp_sb[:, jtap:jtap + 1],
                                                                   in1=u_[:], op0=ALU.mult, op1=ALU.add), reads=[xb_, rgp_sb, u_], writes=[u_])
            S.op("dve", lambda: nc.vector.tensor_copy(out=xn[:, 0:3], in_=xb_[:, 512:515]), reads=[xb_], writes=[xn])
            S.op("dve", lambda: nc.vector.tensor_copy(out=ub16[:], in_=u_[:]), reads=[u_], writes=[ub16])
            S.mm(STp[0], STp[0][:], rgw_b, rgw_b[:, 0, :], ub16, ub16[:], start=True, stop=True)
            S.mm(STp[1], STp[1][:], rgw_b, rgw_b[:, 1, :], ub16, ub16[:], start=True, stop=True)
            S.op("act", lambda: nc.scalar.activation(out=r_[:], in_=STp[0][:], func=AF.Sigmoid, bias=rgp_sb[:, 5:6], scale=1.0),
                 reads=[STp[0], rgp_sb], writes=[r_])
            S.op("act", lambda: nc.scalar.activation(out=ig[:], in_=STp[1][:], func=AF.Sigmoid, bias=rgp_sb[:, 6:7], scale=1.0),
                 reads=[STp[1], rgp_sb], writes=[ig])
            S.op("act", lambda: nc.scalar.activation(out=a_[:], in_=r_[:], func=AF.Exp, scale=c8[:, 0:1]), reads=[r_, c8], writes=[a_])
            S.op("act", lambda: nc.scalar.activation(out=a2[:], in_=r_[:], func=AF.Exp, scale=c16[:, 0:1]), reads=[r_, c16], writes=[a2])
            S.op("dve", lambda: nc.vector.tensor_scalar(out=a2[:], in0=a2[:], scalar1=-1.0, scalar2=1.0, op0=ALU.mult, op1=ALU.add),
                 reads=[a2], writes=[a2])
            S.op("dve", lambda: nc.vector.tensor_scalar(out=a2[:], in0=a2[:], scalar1=0.0, scalar2=None, op0=ALU.max), reads=[a2], writes=[a2])
            S.op("act", lambda: nc.scalar.activation(out=gx[:], in_=a2[:], func=AF.Sqrt), reads=[a2], writes=[gx])
            S.op("dve", lambda: nc.vector.tensor_tensor(out=xin[:], in0=ig[:], in1=u_[:], op=ALU.mult), reads=[ig, u_], writes=[xin])
            S.op("dve", lambda: nc.vector.tensor_tensor(out=xin[:], in0=xin[:], in1=gx[:], op=ALU.mult), reads=[xin, gx], writes=[xin])
            hprev = hst[1 - bi]; hcur = hst[bi]
            S.op("dve", lambda: nc.vector.tensor_tensor_scan(out=hcur[:], data0=a_[:], data1=xin[:], initial=hprev[:, 511:512],
                                                             op0=ALU.mult, op1=ALU.add), reads=[a_, xin, hprev], writes=[hcur])
            S.op("dve", lambda: nc.vector.tensor_tensor(out=gl[:], in0=yb_[:], in1=yb_[:], op=ALU.mult), reads=[yb_], writes=[gl])
            S.op("dve", lambda: nc.vector.tensor_scalar(out=gl[:], in0=gl[:], scalar1=0.044715, scalar2=1.0, op0=ALU.mult, op1=ALU.add),
                 reads=[gl], writes=[gl])
            S.op("dve", lambda: nc.vector.tensor_tensor(out=gl[:], in0=gl[:], in1=yb_[:], op=ALU.mult), reads=[gl, yb_], writes=[gl])
            S.op("act", lambda: nc.scalar.activation(out=gl2[:], in_=gl[:], func=AF.Sigmoid, scale=1.5957691216), reads=[gl], writes=[gl2])
            S.op("dve", lambda: nc.vector.tensor_tensor(out=gl2[:], in0=gl2[:], in1=yb_[:], op=ALU.mult), reads=[gl2, yb_], writes=[gl2])
            ho = hco[bi]
            S.op("dve", lambda: nc.vector.tensor_tensor(out=ho[:], in0=hcur[:], in1=gl2[:], op=ALU.mult), reads=[hcur, gl2], writes=[ho])
            S.dma("sp", hc_out[:, c0:c0 + 512], ho[:], reads=[ho], sem=sem_h[bi])

        S.finish(hco)
        print("rglru instructions", S.ninst)
    return nc


def build_attn(nblk=8):
    NKEY = 1024 * nblk
    NSB = NKEY // 128
    nc = bass.Bass("TRN2", target_bir_lowering=False)
    ckv_tok = nc.dram_tensor("ckv_tok", [NSB + 1, 128, 256], F32, kind="ExternalInput").ap()
    ikT = nc.dram_tensor("ikT", [64, NKEY], F32, kind="ExternalInput").ap()
    gkv_bc = nc.dram_tensor("gkv_bc", [128, 256], F32, kind="ExternalInput").ap()
    wukT = nc.dram_tensor("wukT", [128, 8, 256], F32, kind="ExternalInput").ap()
    wuv = nc.dram_tensor("wuv", [2, 128, 8, 128], F32, kind="ExternalInput").ap()
    qT = nc.dram_tensor("qT", [nblk, 128, 8, 128], F32, kind="ExternalInput").ap()
    iqT = nc.dram_tensor("iqT", [nblk, 64, 16, 128], F32, kind="ExternalInput").ap()
    iw_bc = nc.dram_tensor("iw_bc", [nblk, 64, 16, 128], F32, kind="ExternalInput").ap()
    iw_tok = nc.dram_tensor("iw_tok", [nblk, 128, 16], F32, kind="ExternalInput").ap()
    trel = nc.dram_tensor("trel", [128, nblk], F32, kind="ExternalInput").ap()
    cst = nc.dram_tensor("cst", [128, 128 + 1024], F32, kind="ExternalInput").ap()
    att_out = nc.dram_tensor("att_out", [nblk, 128, 8, 128], F32, kind="ExternalOutput").ap()

    with ExitStack() as st:
        S = Sched(nc, st)
        f32t = lambda n, w=512, p=128: S.sb(n, [p, w], F32)
        bft = lambda n, w=512, p=128: S.sb(n, [p, w], BF16)
        cst_sb = f32t("cst_sb", 1152)
        ident = bft("ident", 128); ones = bft("ones", 128)
        eps_t = f32t("eps_t", 1); one_t = f32t("one_t", 1)
        sem_c = S.dsem("semc")
        S.dma("sp", cst_sb[:], cst[:, :], writes=[cst_sb], sem=sem_c)
        S.op("dve", lambda: nc.vector.tensor_copy(out=ident[:], in_=cst_sb[:, 0:128]), reads=[cst_sb], writes=[ident])
        S.op("dve", lambda: nc.vector.memset(ones[:], 1.0), writes=[ones])
        S.op("dve", lambda: nc.vector.memset(eps_t[:], EPS), writes=[eps_t])
        S.op("dve", lambda: nc.vector.memset(one_t[:], 1.0), writes=[one_t])
        oacc = [S.ps(f"oacc{i}") for i in range(4)]
        den = S.ps("den")
        STp = [S.ps("ST0"), S.ps("ST1")]
        tpb = S.ps("tpb", (128, 1024), BF16)

        gkv = f32t("gkv", 256); sem_g = S.dsem("semg")
        S.dma("sp", gkv[:], gkv_bc[:, :], writes=[gkv], sem=sem_g)
        cT = [S.sb(f"cT{rc}", [128, NKEY + 128], BF16) for rc in range(2)]
        ctok = S.sb("ctok", [128, NSB + 1, 256], BF16)
        ikb = S.sb("ikb", [64, NKEY], BF16)
        sc = f32t("sc", NKEY)
        kst = [f32t(f"kst{i}", 256) for i in range(2)]; sem_k = [S.dsem("semk0"), S.dsem("semk1")]
        ksq_l = [f32t(f"ksq{i}", 256) for i in range(2)]; kss_l = [f32t(f"kss{i}", 1) for i in range(2)]; krs_l = [f32t(f"krs{i}", 1) for i in range(2)]
        S.op("dve", lambda: nc.vector.memset(kst[0][:], 0.0), writes=[kst[0]])
        for sb_ in range(NSB + 1):
            ks = kst[sb_ % 2]
            ksq = ksq_l[sb_ % 2]; kss = kss_l[sb_ % 2]; krs = krs_l[sb_ % 2]
            rows = 16 if sb_ == 0 else 128
            S.dma("sp", ks[0:rows, :], ckv_tok[sb_, 0:rows, :], writes=[ks], sem=sem_k[sb_ % 2])
            S.op("act", lambda: nc.scalar.activation(out=ksq[:], in_=ks[:], func=AF.Square, accum_out=kss[:, 0:1]), reads=[ks], writes=[ksq, kss])
            S.op("act", lambda: nc.scalar.activation(out=krs[:], in_=kss[:], func=AF.Ln, bias=eps_t[:, 0:1], scale=1.0 / 256), reads=[kss, eps_t], writes=[krs])
            S.op("act", lambda: nc.scalar.activation(out=krs[:], in_=krs[:], func=AF.Exp, scale=-0.5), reads=[krs], writes=[krs])
            S.op("dve", lambda: nc.vector.scalar_tensor_tensor(out=ctok[:, sb_, :], in0=ks[:], scalar=krs[:, 0:1], in1=gkv[:], op0=ALU.mult, op1=ALU.mult),
                 reads=[ks, krs, gkv], writes=[ctok])
            if sb_ == 0:
                S.op("dve", lambda: nc.vector.memset(kst[0][:], 0.0), reads=[ctok], writes=[kst[0]])
            for rc in range(2):
                S.op("pe", lambda: nc.tensor.transpose(out=tpb[:, rc * 128:(rc + 1) * 128], in_=ctok[:, sb_, rc * 128:(rc + 1) * 128], identity=ident[:]),
                     reads=[ctok, ident], writes=[tpb])
            for rc in range(2):
                S.op("act", lambda: nc.scalar.copy(out=cT[rc][:, sb_ * 128:(sb_ + 1) * 128], in_=tpb[:, rc * 128:(rc + 1) * 128]), reads=[tpb], writes=[cT[rc]])
        sem_i = S.dsem("semi0")
        for kc_ in range(NKEY // 1024):
            S.dma("sp", sc[0:64, 0:1024], ikT[:, kc_ * 1024:(kc_ + 1) * 1024], writes=[sc], sem=sem_i)
            S.op("dve", lambda: nc.vector.tensor_copy(out=ikb[:, kc_ * 1024:(kc_ + 1) * 1024], in_=sc[0:64, 0:1024]), reads=[sc], writes=[ikb])
        wukb = S.sb("wukb", [128, 8, 256], BF16); wuvb = S.sb("wuvb", [128, 2, 8, 128], BF16)
        sem_w = [S.dsem("semw0"), S.dsem("semw1")]
        S.dma("sp", sc[:, 0:2048], wukT.rearrange("p h r -> p (h r)"), writes=[sc], sem=sem_w[0])
        S.op("dve", lambda: nc.vector.tensor_copy(out=wukb[:].rearrange("p h r -> p (h r)"), in_=sc[:, 0:2048]), reads=[sc], writes=[wukb])
        for rc in range(2):
            S.dma("sp", sc[:, 0:1024], wuv[rc, :, :, :].rearrange("p h d -> p (h d)"), writes=[sc], sem=sem_w[1])
            S.op("dve", lambda: nc.vector.tensor_copy(out=wuvb[:, rc, :, :].rearrange("p h d -> p (h d)"), in_=sc[:, 0:1024]), reads=[sc], writes=[wuvb])
        cmax = f32t("cmax", 1)
        S.op("dve", lambda: nc.vector.tensor_reduce(out=cmax[:], in_=gkv[:], axis=AX.X, op=ALU.max, apply_absolute_value=True), reads=[gkv], writes=[cmax])
        S.op("dve", lambda: nc.vector.tensor_scalar(out=cmax[:], in0=cmax[:], scalar1=-16.0, scalar2=None, op0=ALU.mult), reads=[cmax], writes=[cmax])
        trel_sb = f32t("trel_sb", nblk); sem_t = S.dsem("semt")
        S.dma("sp", trel_sb[:], trel[:, :], writes=[trel_sb], sem=sem_t)

        qst = S.sb("qst", [128, 8, 128], F32); qb16 = S.sb("qb16", [128, 8, 128], BF16); sem_q = S.dsem("semq")
        iqst = S.sb("iqst", [64, 16, 128], F32); iwst = S.sb("iwst", [64, 16, 128], F32); sem_iq = S.dsem("semiq"); sem_iw = S.dsem("semiw")
        iqs = S.sb("iqs", [64, 16, 128], BF16)
        iwt = f32t("iwt", 16); sgn = f32t("sgn", 16); sem_it = S.dsem("semit")
        qlat = [S.sb(f"qlat{rc}", [128, 1024], BF16) for rc in range(2)]
        qsq = bft("qsq", 1024); negm = S.sb("negm", [1, 1024], BF16); nrm = S.sb("nrm", [1, 1024], F32)
        Rb = [bft(f"Rb{i}") for i in range(4)]
        dg = S.sb("dg", [128, 16, 128], BF16)
        junk = bft("junk", 2048)
        lo = f32t("lo", 1); hi = f32t("hi", 1); mid = f32t("mid", 1); cnt = f32t("cnt", 1); prd = f32t("prd", 1); dd = f32t("dd", 1)
        pen = f32t("pen", 1024)
        maskb = bft("maskb"); maskT = S.sb("maskT", [128, 4, 128], BF16)
        NPT = 3
        PT = [bft(f"PT{i}") for i in range(NPT)]; PTm = [bft(f"PTm{i}") for i in range(NPT)]
        den_sb = f32t("den_sb", 8); rec = f32t("rec", 8)
        o_sb = S.sb("o_sb", [128, 8, 256], BF16); oT = [S.sb(f"oT{rc}", [128, 8, 128], BF16) for rc in range(2)]
        ao = [S.sb(f"ao{i}", [128, 4, 128], F32) for i in range(2)]; sem_ao = [S.dsem("semao0"), S.dsem("semao1")]
        onesrow = S.sb("onesrow", [1, 128], BF16)
        S.op("dve", lambda: nc.vector.memset(onesrow[:], 1.0), writes=[onesrow])
        IDX_SCALE = (64 ** -0.5) * (16 ** -0.5)
        rri = 0; pti = 0
        for j in range(nblk):
            nkt = 2 * (j + 1)
            nsb = 8 * (j + 1)
            S.dma("sp", qst[:], qT[j, :, :, :], writes=[qst], sem=sem_q)
            S.dma("sp", iqst[:], iqT[j, :, :, :], writes=[iqst], sem=sem_iq)
            S.dma("sp", iwst[:], iw_bc[j, :, :, :], writes=[iwst], sem=sem_iw)
            S.dma("sp", iwt[:], iw_tok[j, :, :], writes=[iwt], sem=sem_it)
            S.op("dve", lambda: nc.vector.tensor_copy(out=qb16[:], in_=qst[:]), reads=[qst], writes=[qb16])
            S.op("act", lambda: nc.scalar.activation(out=iwst[:], in_=iwst[:], func=AF.Abs), reads=[iwst], writes=[iwst])
            S.op("dve", lambda: nc.vector.scalar_tensor_tensor(out=iqs[:], in0=iqst[:], scalar=IDX_SCALE, in1=iwst[:], op0=ALU.mult, op1=ALU.mult),
                 reads=[iqst, iwst], writes=[iqs])
            S.op("act", lambda: nc.scalar.activation(out=sgn[:], in_=iwt[:], func=AF.Sign), reads=[iwt], writes=[sgn])
            for rc in range(2):
                for hg in range(2):
                    for hh in range(4):
                        h = hg * 4 + hh
                        S.mm(STp[hg], STp[hg][:, hh * 128:(hh + 1) * 128], wukb, wukb[:, h, rc * 128:(rc + 1) * 128], qb16, qb16[:, h, :], start=True, stop=True)
                    S.op("act", lambda: nc.scalar.activation(out=qlat[rc][:, hg * 512:(hg + 1) * 512], in_=STp[hg][:], func=AF.Identity, scale=128 ** -0.5),
                         reads=[STp[hg]], writes=[qlat[rc]])
            for hg in range(2):
                for rc in range(2):
                    S.op("dve", lambda: nc.vector.tensor_tensor(out=qsq[:, 0:512], in0=qlat[rc][:, hg * 512:(hg + 1) * 512], in1=qlat[rc][:, hg * 512:(hg + 1) * 512], op=ALU.mult),
                         reads=[qlat[rc]], writes=[qsq])
                    S.mm(STp[hg], STp[hg][:], ones, ones[:], qsq, qsq[:, 0:512], start=(rc == 0), stop=(rc == 1))
                S.op("act", lambda: nc.scalar.activation(out=nrm[:, hg * 512:(hg + 1) * 512], in_=STp[hg][0:1, :], func=AF.Sqrt), reads=[STp[hg]], writes=[nrm])
            S.op("dve", lambda: nc.vector.tensor_scalar(out=negm[:], in0=nrm[:], scalar1=cmax[0:1, 0:1], scalar2=None, op0=ALU.mult), reads=[nrm, cmax], writes=[negm])
            for h in range(16):
                S.op("dve", lambda: nc.vector.tensor_scalar(out=dg[:, h, :], in0=ident[:], scalar1=sgn[:, h:h + 1], scalar2=None, op0=ALU.mult),
                     reads=[ident, sgn], writes=[dg])
            scp = oacc[0]
            for kt in range(nkt):
                S.mm(STp[0], STp[0][:], iqs, iqs[:, 0, :], ikb, ikb[:, kt * 512:(kt + 1) * 512], start=True, stop=True)
                for h in range(16):
                    pb = STp[h % 2]
                    if h + 1 < 16:
                        pn = STp[(h + 1) % 2]
                        S.mm(pn, pn[:], iqs, iqs[:, h + 1, :], ikb, ikb[:, kt * 512:(kt + 1) * 512], start=True, stop=True)
                    R = Rb[rri % 4]; rri += 1
                    S.op("act", lambda: nc.scalar.activation(out=R[:], in_=pb[:], func=AF.Relu), reads=[pb], writes=[R])
                    S.mm(scp, scp[:], dg, dg[:, h, :], R, R[:], start=(h == 0), stop=(h == 15))
                S.op("dve", lambda: nc.vector.tensor_copy(out=sc[:, kt * 512:(kt + 1) * 512], in_=scp[:]), reads=[scp], writes=[sc])
            nk = 1024 * (j + 1)
            S.op("dve", lambda: nc.vector.tensor_reduce(out=hi[:], in_=sc[:, 0:nk], axis=AX.X, op=ALU.max, apply_absolute_value=True), reads=[sc], writes=[hi])
            S.op("dve", lambda: nc.vector.tensor_scalar(out=lo[:], in0=hi[:], scalar1=-1.0, scalar2=-1.0, op0=ALU.mult, op1=ALU.add), reads=[hi], writes=[lo])
            S.op("dve", lambda: nc.vector.tensor_scalar(out=hi[:], in0=hi[:], scalar1=1.0, scalar2=None, op0=ALU.add), reads=[hi], writes=[hi])
            S.op("dve", lambda: nc.vector.tensor_scalar(out=pen[:], in0=cst_sb[:, 128:1152], scalar1=trel_sb[:, j:j + 1], scalar2=-1e30, op0=ALU.is_gt, op1=ALU.mult),
                 reads=[cst_sb, trel_sb], writes=[pen])
            S.op("dve", lambda: nc.vector.tensor_tensor(out=sc[:, nk - 1024:nk], in0=sc[:, nk - 1024:nk], in1=pen[:], op=ALU.add), reads=[sc, pen], writes=[sc])
            for itn in range(NBIS):
                S.op("dve", lambda: nc.vector.tensor_tensor(out=mid[:], in0=lo[:], in1=hi[:], op=ALU.add), reads=[lo, hi], writes=[mid])
                S.op("dve", lambda: nc.vector.tensor_scalar(out=mid[:], in0=mid[:], scalar1=0.5, scalar2=None, op0=ALU.mult), reads=[mid], writes=[mid])
                first = True
                for c0 in range(0, nk, 2048):
                    w = min(2048, nk - c0)
                    if first:
                        S.op("dve", lambda: nc.vector.tensor_scalar(out=junk[:, 0:w], in0=sc[:, c0:c0 + w], scalar1=mid[:, 0:1], scalar2=None, op0=ALU.is_ge,
                                                                    op1=ALU.add, accum_out=cnt[:, 0:1]), reads=[sc, mid], writes=[junk, cnt])
                    else:
                        S.op("dve", lambda: nc.vector.tensor_scalar(out=junk[:, 0:w], in0=sc[:, c0:c0 + w], scalar1=mid[:, 0:1], scalar2=cnt[:, 0:1], op0=ALU.is_ge,
                                                                    op1=ALU.add, accum_out=cnt[:, 0:1]), reads=[sc, mid, cnt], writes=[junk, cnt])
                    first = False
                S.op("dve", lambda: nc.vector.tensor_scalar(out=prd[:], in0=cnt[:], scalar1=KSEL - 0.5, scalar2=None, op0=ALU.is_ge), reads=[cnt], writes=[prd])
                S.op("dve", lambda: nc.vector.tensor_tensor(out=dd[:], in0=mid[:], in1=lo[:], op=ALU.subtract), reads=[mid, lo], writes=[dd])
                S.op("dve", lambda: nc.vector.scalar_tensor_tensor(out=lo[:], in0=dd[:], scalar=prd[:, 0:1], in1=lo[:], op0=ALU.mult, op1=ALU.add),
                     reads=[dd, prd, lo], writes=[lo])
                S.op("dve", lambda: nc.vector.tensor_tensor(out=dd[:], in0=hi[:], in1=mid[:], op=ALU.subtract), reads=[hi, mid], writes=[dd])
                S.op("dve", lambda: nc.vector.scalar_tensor_tensor(out=hi[:], in0=dd[:], scalar=prd[:, 0:1], in1=mid[:], op0=ALU.mult, op1=ALU.add),
                     reads=[dd, prd, mid], writes=[hi])
            started = [False] * 4
            den_started = False
            for sbk in range(nsb + 1):
                rows = 16 if sbk == 0 else 128
                kb = sbk - 1
                if sbk >= 1 and kb % 4 == 0:
                    ktile = kb // 4
                    S.op("dve", lambda: nc.vector.tensor_scalar(out=maskb[:], in0=sc[:, ktile * 512:(ktile + 1) * 512], scalar1=lo[:, 0:1], scalar2=None, op0=ALU.is_ge),
                         reads=[sc, lo], writes=[maskb])
                    for q4 in range(4):
                        S.op("pe", lambda: nc.tensor.transpose(out=tpb[:, q4 * 128:(q4 + 1) * 128], in_=maskb[:, q4 * 128:(q4 + 1) * 128], identity=ident[:]),
                             reads=[maskb, ident], writes=[tpb])
                    S.op("act", lambda: nc.scalar.copy(out=maskT[:, :, :], in_=tpb[:, 0:512]), reads=[tpb], writes=[maskT])
                for hg in range(2):
                    stp = STp[hg]
                    for rc in range(2):
                        S.mm(stp, stp[0:rows, :], cT[rc], cT[rc][:, sbk * 128:sbk * 128 + rows], qlat[rc], qlat[rc][:, hg * 512:(hg + 1) * 512],
                             start=(rc == 0), stop=False)
                    S.mm(stp, stp[0:rows, :], onesrow, onesrow[0:1, 0:rows], negm, negm[0:1, hg * 512:(hg + 1) * 512], start=False, stop=True)
                    P = PT[pti % NPT]; Pm = PTm[pti % NPT]; pti += 1
                    S.op("act", lambda: nc.scalar.activation(out=P[0:rows, :], in_=stp[0:rows, :], func=AF.Exp), reads=[stp], writes=[P])
                    if sbk == 0:
                        Pm = P
                    else:
                        q4 = kb % 4
                        S.op("dve", lambda: nc.vector.tensor_tensor(out=Pm[:].rearrange("p (h t) -> p h t", h=4), in0=P[:].rearrange("p (h t) -> p h t", h=4),
                                                                     in1=maskT[:, q4:q4 + 1, :].to_broadcast([128, 4, 128]), op=ALU.mult),
                             reads=[P, maskT], writes=[Pm])
                    for hh in range(4):
                        h = hg * 4 + hh
                        bank = oacc[h // 2]
                        S.mm(bank, bank[:, (h % 2) * 256:(h % 2 + 1) * 256], Pm, Pm[0:rows, hh * 128:(hh + 1) * 128], ctok, ctok[0:rows, sbk, :],
                             start=(not started[h // 2]), stop=(sbk == nsb), skip_group_check=True)
                        started[h // 2] = True
                        S.mm(den, den[:, h:h + 1], Pm, Pm[0:rows, hh * 128:(hh + 1) * 128], ones, ones[0:rows, 0:1],
                             start=(not den_started), stop=(sbk == nsb), skip_group_check=True)
                        den_started = True
            S.op("dve", lambda: nc.vector.tensor_copy(out=den_sb[:], in_=den[:, 0:8]), reads=[den], writes=[den_sb])
            S.op("dve", lambda: nc.vector.reciprocal(out=rec[:], in_=den_sb[:]), reads=[den_sb], writes=[rec])
            for h in range(8):
                bank = oacc[h // 2]
                S.op("dve", lambda: nc.vector.tensor_scalar(out=o_sb[:, h, :], in0=bank[:, (h % 2) * 256:(h % 2 + 1) * 256], scalar1=rec[:, h:h + 1], scalar2=None, op0=ALU.mult),
                     reads=[bank, rec], writes=[o_sb])
            for rc in range(2):
                for h in range(8):
                    S.op("pe", lambda: nc.tensor.transpose(out=tpb[:, h * 128:(h + 1) * 128], in_=o_sb[:, h, rc * 128:(rc + 1) * 128], identity=ident[:]),
                         reads=[o_sb, ident], writes=[tpb])
                S.op("act", lambda: nc.scalar.copy(out=oT[rc][:, :, :], in_=tpb[:, :]), reads=[tpb], writes=[oT[rc]])
            for hg in range(2):
                stp = STp[hg]
                for hh in range(4):
                    h = hg * 4 + hh
                    for rc in range(2):
                        S.mm(stp, stp[:, hh * 128:(hh + 1) * 128], wuvb, wuvb[:, rc, h, :], oT[rc], oT[rc][:, h, :], start=(rc == 0), stop=(rc == 1))
                a = ao[hg]
                S.op("act", lambda: nc.scalar.copy(out=a[:, :, :], in_=stp[:]), reads=[stp], writes=[a])
                S.dma("sp", att_out[j, :, hg * 4:(hg + 1) * 4, :], a[:, :, :], reads=[a], sem=sem_ao[hg])
        S.finish(ao)
        print("attn instructions", S.ninst)
    return nc


def attn_consts():
    c = np.zeros((128, 1152), np.float32)
    c[:, 0:128] = np.eye(128, dtype=np.float32)
    c[:, 128:1152] = np.arange(1024, dtype=np.float32)[None, :]
    return c


def attn_inputs(core, nblk, q, ckv, iq, ik, iw, kvg, w_uk, w_uv):
    NKEY = 1024 * nblk
    NSB = NKEY // 128
    ckv_tok = np.zeros((NSB + 1, 128, 256), np.float32)
    ckv_tok[0, :NMETA] = ckv[:NMETA]
    ckv_tok[1:] = ckv[NMETA:NMETA + NKEY].reshape(NSB, 128, 256)
    d = {"ckv_tok": ckv_tok,
         "ikT": np.ascontiguousarray(ik[NMETA:NMETA + NKEY].T),
         "gkv_bc": np.ascontiguousarray(np.broadcast_to(kvg[None, :], (128, 256))).astype(np.float32),
         "wukT": np.ascontiguousarray(w_uk.transpose(2, 1, 0)),
         "wuv": np.ascontiguousarray(w_uv.reshape(2, 128, 8, 128)),
         "cst": attn_consts()}
    qT = np.zeros((nblk, 128, 8, 128), np.float32)
    iqT = np.zeros((nblk, 64, 16, 128), np.float32)
    iwb = np.zeros((nblk, 64, 16, 128), np.float32)
    iwt = np.zeros((nblk, 128, 16), np.float32)
    trel = np.zeros((128, nblk), np.float32)
    toks = []
    for j in range(nblk):
        qb = 8 * j + core
        tok = NMETA + 128 * qb + np.arange(128)
        toks.append(tok)
        qT[j] = q[tok].reshape(128, 8, 128).transpose(2, 1, 0)
        iqT[j] = iq[tok].reshape(128, 16, 64).transpose(2, 1, 0)
        iwb[j] = np.broadcast_to(iw[tok].T[None, :, :], (64, 16, 128))
        iwt[j] = iw[tok]
        trel[:, j] = (128 * qb + np.arange(128)) - 1024 * j
    d.update({"qT": qT, "iqT": iqT, "iw_bc": iwb, "iw_tok": iwt, "trel": trel})
    return d, toks


def attn_unpack(att_out, toks, att_full):
    for j, tok in enumerate(toks):
        att_full[tok] = att_out[j].transpose(2, 1, 0).reshape(128, 1024)


def rglru_inputs(core, ntiles, rx, ry, conv_w, conv_b, w_a, b_a, w_i, b_i, lam):
    TPR = ntiles * 512
    T = rx.shape[0]
    sl = slice(core * 128, (core + 1) * 128)
    rxy = np.zeros((2, 128, TPR), np.float32)
    n = min(T, TPR)
    rxy[0, :, :n] = rx[:n, sl].T
    rxy[1, :, :n] = ry[:n, sl].T
    rgp = np.zeros((128, 8), np.float32)
    for jt in range(4):
        rgp[:, jt] = conv_w[jt, sl]
    rgp[:, 4] = conv_b[sl]; rgp[:, 5] = b_a[sl]; rgp[:, 6] = b_i[sl]; rgp[:, 7] = lam[sl]
    rgw = np.ascontiguousarray(np.stack([w_a[core], w_i[core]], 0))
    return {"rxy": rxy, "rgp": rgp, "rgw": rgw}


def tok_idx_l0(c):
    r0 = NMETA + c * 1024
    return np.concatenate([np.arange(r0, r0 + 512), np.arange(0, NMETA), np.arange(r0 + 512, r0 + 1024)])


def run_inproj(h_tok, w_in, gain):
    nmb = w_in.shape[0]
    M = nmb * 128
    key = ("ip", nmb)
    if key not in _NC_CACHE:
        _NC_CACHE[key] = build_rowlocal(0, inproj_mb=nmb)
    nc = _NC_CACHE[key]
    gains = np.ascontiguousarray(np.stack([gvec(gain)] * 4, axis=1)).astype(np.float32)
    wt = w_in
    in_maps = []
    for c in range(NCORES):
        in_maps.append({"hT_in": fm(h_tok[tok_idx_l0(c)]), "gains": gains, "cd_t": wt})
    res = run_bass_kernel_spmd(nc, in_maps, core_ids=list(range(NCORES)))
    proj = np.zeros((h_tok.shape[0], M), np.float32)
    for c in range(NCORES):
        p = np.asarray(res.results[c]["proj_out"])
        proj[tok_idx_l0(c)] = p.transpose(2, 0, 1).reshape(p.shape[2], nmb * 128)
    return proj


def run_mix0(proj0, lb_logits, out_norm_g, sconv_w):
    if "m0" not in _NC_CACHE:
        _NC_CACHE["m0"] = build_mix0(17)
    nc = _NC_CACHE["m0"]
    T = proj0.shape[0]
    P = np.zeros((TP0, 7168), np.float32)
    P[48:48 + T] = proj0
    cst = mix0_consts()
    in_maps = []
    for c in range(NCORES):
        pj = np.ascontiguousarray(np.stack([P[:, i * 1024 + c * 128: i * 1024 + (c + 1) * 128].T for i in range(7)], 0))
        par = np.zeros((128, 8), np.float32)
        par[:, 0] = lb_logits[0, c * 128:(c + 1) * 128]
        par[:, 1] = lb_logits[1, c * 128:(c + 1) * 128]
        par[:, 2] = out_norm_g
        for jt in range(3):
            par[:, 3 + jt] = sconv_w[jt, c * 128:(c + 1) * 128]
        in_maps.append({"pj": pj, "par": par, "cst": cst})
    res = run_bass_kernel_spmd(nc, in_maps, core_ids=list(range(NCORES)))
    mix = np.zeros((T, 2048), np.float32)
    for c in range(NCORES):
        mo = np.asarray(res.results[c]["mo"])
        mix[:, c * 128:(c + 1) * 128] = mo[0].T[48:48 + T]
        mix[:, 1024 + c * 128:1024 + (c + 1) * 128] = mo[1].T[48:48 + T]
    return mix


def run_mix1(proj1, inp):
    T = proj1.shape[0]
    sizes = [1024, 1024, 1024, 256, 1024, 64, 16]
    rx, ry, q, ckv, iq, ik, iw = np.split(proj1, np.cumsum(sizes)[:-1], axis=-1)
    if "rg" not in _NC_CACHE:
        _NC_CACHE["rg"] = build_rglru(17)
    nc = _NC_CACHE["rg"]
    in_maps = [rglru_inputs(c, 17, rx, ry, inp["rg_conv_w"][0], inp["rg_conv_b"][0], inp["rg_w_a"][0], inp["rg_b_a"][0],
                            inp["rg_w_i"][0], inp["rg_b_i"][0], inp["rg_lambda"][0]) for c in range(NCORES)]
    res = run_bass_kernel_spmd(nc, in_maps, core_ids=list(range(NCORES)))
    mix = np.zeros((T, 2048), np.float32)
    for c in range(NCORES):
        mix[:, c * 128:(c + 1) * 128] = np.asarray(res.results[c]["hc_out"]).T[:T]
    if "at" not in _NC_CACHE:
        _NC_CACHE["at"] = build_attn(8)
    nc = _NC_CACHE["at"]
    in_maps = []
    toks_all = []
    for c in range(NCORES):
        d, toks = attn_inputs(c, 8, q, ckv, iq, ik, iw, inp["mla_kv_norm"][0], inp["mla_w_uk"][0], inp["mla_w_uv"][0])
        in_maps.append(d)
        toks_all.append(toks)
    res = run_bass_kernel_spmd(nc, in_maps, core_ids=list(range(NCORES)))
    att = np.zeros((T, 1024), np.float32)
    for c in range(NCORES):
        attn_unpack(np.asarray(res.results[c]["att_out"]), toks_all[c], att)
    mix[:, 1024:] = att
    return mix


WC_CH = 4096


def build_wcast(nch):
    ncol = nch * WC_CH
    nc = bass.Bass("TRN2", target_bir_lowering=False)
    x = nc.dram_tensor("wx", [128, ncol], F32, kind="ExternalInput").ap()
    y = nc.dram_tensor("wy", [128, ncol], BF16, kind="ExternalOutput").ap()
    with ExitStack() as st:
        S = Sched(nc, st)
        NB = 4
        ib = [S.sb(f"ib{i}", [128, WC_CH], F32) for i in range(NB)]
        ob = [S.sb(f"ob{i}", [128, WC_CH], BF16) for i in range(NB)]
        isem = [S.dsem(f"isem{i}") for i in range(NB)]
        osem = [S.dsem(f"osem{i}") for i in range(NB)]
        LOOK = 2
        for i in range(min(LOOK, nch)):
            S.dma("sp", ib[i % NB][:], x[:, i * WC_CH:(i + 1) * WC_CH], writes=[ib[i % NB]], sem=isem[i % NB])
        for i in range(nch):
            if i + LOOK < nch:
                k = (i + LOOK) % NB
                S.dma("sp", ib[k][:], x[:, (i + LOOK) * WC_CH:(i + LOOK + 1) * WC_CH], writes=[ib[k]], sem=isem[k])
            b = i % NB
            h = WC_CH // 2
            S.op("act", lambda: nc.scalar.copy(out=ob[b][:, 0:h], in_=ib[b][:, 0:h]), reads=[ib[b]], writes=[ob[b]])
            S.op("dve", lambda: nc.vector.tensor_copy(out=ob[b][:, h:WC_CH], in_=ib[b][:, h:WC_CH]), reads=[ib[b]], writes=[ob[b]])
            S.dma("sp", y[:, i * WC_CH:(i + 1) * WC_CH], ob[b][:], reads=[ob[b]], sem=osem[b])
        S.finish(ob)
    return nc


def device_cast_bf16(arrs):
    flat = np.concatenate([a.reshape(-1) for a in arrs])
    n = flat.size
    per = NCORES * 128 * WC_CH
    nch = -(-n // per)
    pad = nch * per - n
    if pad:
        flat = np.concatenate([flat, np.zeros(pad, np.float32)])
    parts = flat.reshape(NCORES, 128, nch * WC_CH)
    key = ("wc", nch)
    if key not in _NC_CACHE:
        _NC_CACHE[key] = build_wcast(nch)
    res = run_bass_kernel_spmd(_NC_CACHE[key], [{"wx": np.ascontiguousarray(parts[c])} for c in range(NCORES)], core_ids=list(range(NCORES)))
    out = np.concatenate([np.asarray(res.results[c]["wy"]).reshape(-1) for c in range(NCORES)])[:n]
    outs = []
    o = 0
    for a in arrs:
        outs.append(out[o:o + a.size].reshape(a.shape))
        o += a.size
    return outs


def kernel(**inp):
    inp = {k: np.asarray(v) for k, v in inp.items()}
    x = inp["x"]
    h0 = np.concatenate([inp["meta_tokens"].astype(np.float32), x[0]], axis=0)
    names = [("ab_w_in", 0), ("ab_w_out", 0), ("ffn_w1", 0), ("ffn_w3", 0), ("ffn_w2", 0), ("cd_w_in", 0),
             ("cd_w_out", 0), ("ffn_w1", 1), ("ffn_w3", 1), ("ffn_w2", 1)]
    tiled = [tile_w(inp[n][l]) for n, l in names]
    wb = dict(zip(names, device_cast_bf16(tiled)))
    proj0 = run_inproj(h0, wb[("ab_w_in", 0)], inp["ln_mix_pre"][0])
    mix0 = run_mix0(proj0, inp["hgrn_lb_logits"], inp["hgrn_out_norm"][0], inp["sconv_w"][0])
    h1, proj1 = run_rowlocal(0, h0, mix0, wb[("ab_w_out", 0)], wb[("ffn_w1", 0)], wb[("ffn_w3", 0)], wb[("ffn_w2", 0)],
                             [inp["ln_mix_post"][0], inp["ln_ffn_pre"][0], inp["ln_ffn_post"][0], inp["ln_mix_pre"][1]],
                             wb[("cd_w_in", 0)])
    mix1 = run_mix1(proj1, inp)
    h2, _ = run_rowlocal(1, h1[NMETA:], mix1[NMETA:], wb[("cd_w_out", 0)], wb[("ffn_w1", 1)], wb[("ffn_w3", 1)], wb[("ffn_w2", 1)],
                         [inp["ln_mix_post"][1], inp["ln_ffn_pre"][1], inp["ln_ffn_post"][1], inp["ln_mix_pre"][1]])
    return h2[None].astype(np.float32)
```
